# Optimizing a Trainium2 kernel written in Bass

```python
import jax, jax.numpy as jnp
from jax import lax
import numpy as np

D_MODEL = 1024
BATCH = 4
SEQ = 4096
DEPTH = 4

HEAD_DIM = 64
A_HEADS = 8
A_KV_HEADS = 2
A_WIDTH = A_HEADS * HEAD_DIM
A_KV_WIDTH = A_KV_HEADS * HEAD_DIM
IDX_HEADS = 4
IDX_DIM = 64
IDX_SCALE = (IDX_HEADS * IDX_DIM) ** -0.5
TOPK_MAX = 256
POOL_WINDOWS = (2, 4, 8, 16)
POOL_GROUP = 128
POOL_WIDTH = POOL_GROUP * len(POOL_WINDOWS)
EVEN_MIX_WIDTH = A_WIDTH + POOL_WIDTH
EVEN_IN_COLS = A_WIDTH + 2 * A_KV_WIDTH + IDX_HEADS * IDX_DIM + IDX_DIM + IDX_HEADS + POOL_WIDTH
C_HEADS = 16
C_WIDTH = C_HEADS * HEAD_DIM
D_FF = -(-8 * D_MODEL // (3 * 256)) * 256
N_EVEN = (DEPTH + 1) // 2
N_ODD = DEPTH // 2
Q_BLOCK = 128
EPS = 1e-6

kernel_name = "hybrid_dsa_pool_stickbreak_trunk"


def rms_norm(x, g):
    x32 = x.astype(jnp.float32)
    y = x32 * lax.rsqrt(jnp.mean(x32 * x32, axis=-1, keepdims=True) + EPS)
    return (y * g.astype(jnp.float32)).astype(x.dtype)


def to_blocks(a):
    b, t = a.shape[:2]
    a = a.reshape((b, t // Q_BLOCK, Q_BLOCK) + a.shape[2:])
    return jnp.moveaxis(a, 1, 0)


def from_blocks(a):
    a = jnp.moveaxis(a, 0, 1)
    return a.reshape((a.shape[0], a.shape[1] * a.shape[2]) + a.shape[3:])


def dsa_attention(q, k, v, q_idx, k_idx, w_idx):
    b, t = q.shape[:2]
    n_sel = min(TOPK_MAX, t // 4)
    rep = A_HEADS // A_KV_HEADS
    key_pos = jnp.arange(t)
    gather = jax.vmap(lambda kv, ids: kv[ids])

    def block(args):
        qb, qib, wb, start = args
        q_pos = start + jnp.arange(Q_BLOCK)
        causal = key_pos[None, :] <= q_pos[:, None]
        rel = jax.nn.relu(jnp.einsum('bthd,bsd->bths', qib, k_idx).astype(jnp.float32))
        score = jnp.einsum('bths,bth->bts', rel, wb.astype(jnp.float32)) * IDX_SCALE
        score = jnp.where(causal[None], score, -jnp.inf)
        _, sel = lax.top_k(score, n_sel)
        k_sel = gather(k, sel)
        v_sel = gather(v, sel)
        valid = sel <= q_pos[None, :, None]
        qg = qb.reshape(b, Q_BLOCK, A_KV_HEADS, rep, HEAD_DIM)
        logits = jnp.einsum('btgrd,btkgd->btgrk', qg, k_sel).astype(jnp.float32) * (HEAD_DIM ** -0.5)
        logits = jnp.where(valid[:, :, None, None, :], logits, -jnp.inf)
        p = jax.nn.softmax(logits, axis=-1).astype(v.dtype)
        o = jnp.einsum('btgrk,btkgd->btgrd', p, v_sel)
        return o.reshape(b, Q_BLOCK, A_WIDTH)

    starts = jnp.arange(t // Q_BLOCK) * Q_BLOCK
    out = lax.map(block, (to_blocks(q), to_blocks(q_idx), to_blocks(w_idx), starts))
    return from_blocks(out)


def multiscale_pool(u, pool_w, pool_scale):
    b, t, _ = u.shape
    groups = u.astype(jnp.float32).reshape(b, t, len(POOL_WINDOWS), POOL_GROUP)
    c0 = jnp.pad(jnp.cumsum(groups, axis=1), ((0, 0), (1, 0), (0, 0), (0, 0)))
    pos = jnp.arange(1, t + 1, dtype=jnp.float32)
    outs = []
    for g, w in enumerate(POOL_WINDOWS):
        cg = c0[:, :, g]
        hi = cg[:, 1:]
        lo = jnp.pad(cg[:, :t + 1 - w], ((0, 0), (w - 1, 0), (0, 0)))
        count = jnp.minimum(pos, float(w))[None, :, None]
        outs.append((hi - lo) / count - groups[:, :, g])
    mixed = jnp.stack(outs, axis=2)
    y = jnp.einsum('btgc,gcd->btgd', mixed, pool_w.astype(jnp.float32))
    return (y.reshape(b, t, POOL_WIDTH) * pool_scale.astype(jnp.float32)).astype(u.dtype)


def stick_breaking_attention(q, k, v):
    b, t = q.shape[:2]
    key_pos = jnp.arange(t)

    def block(args):
        qb, start = args
        q_pos = start + jnp.arange(Q_BLOCK)
        strict = key_pos[None, :] < q_pos[:, None]
        z = jnp.einsum('bthd,bshd->bhts', qb, k).astype(jnp.float32) * (HEAD_DIM ** -0.5)
        brk = jnp.where(strict, jax.nn.softplus(z), 0.0)
        later = jnp.pad(brk[..., 1:], ((0, 0), (0, 0), (0, 0), (0, 1)))
        remain = lax.cumsum(later, axis=3, reverse=True)
        log_a = jax.nn.log_sigmoid(z) - remain
        a = jnp.where(strict, jnp.exp(log_a), 0.0).astype(v.dtype)
        o = jnp.einsum('bhts,bshd->bthd', a, v)
        return o.reshape(b, Q_BLOCK, C_WIDTH)

    starts = jnp.arange(t // Q_BLOCK) * Q_BLOCK
    out = lax.map(block, (to_blocks(q), starts))
    return from_blocks(out)


def even_mixer(h, w_in, w_out, q_g, k_g, pool_w, pool_scale):
    b, t, _ = h.shape
    proj = h @ w_in
    sizes = [A_WIDTH, A_KV_WIDTH, A_KV_WIDTH, IDX_HEADS * IDX_DIM, IDX_DIM, IDX_HEADS]
    offsets = np.cumsum(sizes).tolist()
    q, k, v, qi, ki, wi, u = jnp.split(proj, offsets, axis=-1)
    q = rms_norm(q.reshape(b, t, A_HEADS, HEAD_DIM), q_g)
    k = rms_norm(k.reshape(b, t, A_KV_HEADS, HEAD_DIM), k_g)
    v = v.reshape(b, t, A_KV_HEADS, HEAD_DIM)
    attn = dsa_attention(q, k, v, qi.reshape(b, t, IDX_HEADS, IDX_DIM), ki, wi)
    pooled = multiscale_pool(u, pool_w, pool_scale)
    return jnp.concatenate([attn, pooled], axis=-1) @ w_out


def odd_mixer(h, w_qkv, w_out):
    b, t, _ = h.shape
    q, k, v = jnp.split((h @ w_qkv).reshape(b, t, 3, C_HEADS, HEAD_DIM), 3, axis=2)
    o = stick_breaking_attention(q[:, :, 0], k[:, :, 0], v[:, :, 0])
    return o @ w_out


def swiglu(h, w1, w3, w2):
    return (jax.nn.silu(h @ w1) * (h @ w3)) @ w2


def setup_inputs(seed: int = 0) -> dict:
    key = jax.random.key(seed)
    ks = jax.random.split(key, 16)
    f32 = jnp.float32

    def dense(k, shape):
        return jax.random.normal(k, shape, f32) * (shape[-2] ** -0.5)

    def gain(k, shape, s):
        return 1.0 + s * jax.random.normal(k, shape, f32)

    return {
        'x': jax.random.normal(ks[0], (BATCH, SEQ, D_MODEL), f32),
        'norm_mix_g': gain(ks[1], (DEPTH, D_MODEL), 0.05),
        'norm_ffn_g': gain(ks[2], (DEPTH, D_MODEL), 0.05),
        'ev_w_in': dense(ks[3], (N_EVEN, D_MODEL, EVEN_IN_COLS)),
        'ev_w_out': dense(ks[4], (N_EVEN, EVEN_MIX_WIDTH, D_MODEL)),
        'ev_q_norm_g': gain(ks[5], (N_EVEN, HEAD_DIM), 0.05),
        'ev_k_norm_g': gain(ks[6], (N_EVEN, HEAD_DIM), 0.05),
        'ev_pool_w': dense(ks[7], (N_EVEN, len(POOL_WINDOWS), POOL_GROUP, POOL_GROUP)),
        'ev_pool_scale': gain(ks[8], (N_EVEN, POOL_WIDTH), 0.1),
        'od_w_qkv': dense(ks[9], (N_ODD, D_MODEL, 3 * C_WIDTH)),
        'od_w_out': dense(ks[10], (N_ODD, C_WIDTH, D_MODEL)),
        'ffn_w1': dense(ks[11], (DEPTH, D_MODEL, D_FF)),
        'ffn_w3': dense(ks[12], (DEPTH, D_MODEL, D_FF)),
        'ffn_w2': dense(ks[13], (DEPTH, D_FF, D_MODEL)),
    }


def reference(x, norm_mix_g, norm_ffn_g, ev_w_in, ev_w_out, ev_q_norm_g, ev_k_norm_g,
              ev_pool_w, ev_pool_scale, od_w_qkv, od_w_out, ffn_w1, ffn_w3, ffn_w2):
    for layer in range(DEPTH):
        i = layer // 2
        h = rms_norm(x, norm_mix_g[layer])
        if layer % 2 == 0:
            x = x + even_mixer(h, ev_w_in[i], ev_w_out[i], ev_q_norm_g[i], ev_k_norm_g[i],
                               ev_pool_w[i], ev_pool_scale[i])
        else:
            x = x + odd_mixer(h, od_w_qkv[i], od_w_out[i])
        h = rms_norm(x, norm_ffn_g[layer])
        x = x + swiglu(h, ffn_w1[layer], ffn_w3[layer], ffn_w2[layer])
    return x
```

```python
import contextlib
import numpy as np
import concourse.bass as bass
import concourse.mybir as mybir
from concourse.bass_utils import run_bass_kernel_spmd

F32 = mybir.dt.float32
BF16 = mybir.dt.bfloat16
ALU = mybir.AluOpType
AF = mybir.ActivationFunctionType
AX = mybir.AxisListType

D = 1024
DFF = 2816
NFB = 22
EV_COLS = 1604
NIT = 18
NEG = -30000.0
IDX_SCALE = 256 ** -0.5
WINS = (2, 4, 8, 16)
EPS = 1e-6


class Sched:
    def __init__(self, nc):
        self.nc = nc
        self.ops = []
        self.lastw = {}
        self.readers = {}
        self.pending = {}
        self.last_on = {}
        self.last_dma = {}
        self.epoch = 0

    def add(self, eng, fn, reads=(), writes=(), dma=None):
        idx = len(self.ops)
        hard, soft = set(), set()
        for k in reads:
            w = self.lastw.get(k)
            if w is not None:
                hard.add(w)
        for k in writes:
            w = self.lastw.get(k)
            if w is not None:
                hard.add(w)
            for r in self.readers.get(k, ()):
                soft.add(r)
        pb = self.pending.pop(eng, None)
        if pb:
            hard |= pb
        for k in reads:
            self.readers.setdefault(k, []).append(idx)
        for k in writes:
            self.lastw[k] = idx
            self.readers[k] = []
        self.ops.append(dict(eng=eng, fn=fn, hard=hard, soft=soft, dma=dma, sig=False, cnt=0, ep=self.epoch))
        if dma is None:
            self.last_on[eng] = idx
        else:
            self.last_dma[dma] = idx
        return idx

    def barrier(self):
        b = set(self.last_on.values()) | set(self.last_dma.values())
        self.epoch += 1
        for e in ("pe", "act", "dve", "pool", "sp"):
            self.pending[e] = set(b) | self.pending.get(e, set())

    def emit(self, final_dma):
        nc = self.nc
        ops = self.ops
        need = [None] * len(ops)
        for i, op in enumerate(ops):
            deps = set()
            for d in op["hard"]:
                od = ops[d]
                if od["dma"] is None and op["dma"] is None and od["eng"] == op["eng"] == "pe":
                    continue
                deps.add(d)
            for d in op["soft"]:
                od = ops[d]
                if od["dma"] is None and op["dma"] is None and od["eng"] == op["eng"]:
                    continue
                deps.add(d)
            need[i] = deps
            for d in deps:
                ops[d]["sig"] = True
        ecount = {}
        dcount = {}
        for op in ops:
            if op["dma"] is not None:
                dcount[op["dma"]] = dcount.get(op["dma"], 0) + 16
                op["cnt"] = dcount[op["dma"]]
            elif op["sig"]:
                ek = (op["eng"], op["ep"])
                ecount[ek] = ecount.get(ek, 0) + 1
                op["cnt"] = ecount[ek]
        with contextlib.ExitStack() as st:
            esem = {ek: st.enter_context(nc.semaphore("es_%s_%d" % ek)) for ek in sorted(ecount)}
            dsem = {k: st.enter_context(nc.semaphore("ds_%d" % n)) for n, k in enumerate(sorted(dcount, key=str))}
            block = st.enter_context(nc.Block())
            streams = {e: [] for e in ("pe", "act", "dve", "pool", "sp")}
            for i, op in enumerate(ops):
                streams[op["eng"]].append(i)

            def run(e, ename):
                waited = {}
                for i in streams[ename]:
                    op = ops[i]
                    tgt = {}
                    for d in need[i]:
                        od = ops[d]
                        s = dsem[od["dma"]] if od["dma"] is not None else esem[(od["eng"], od["ep"])]
                        key = id(s)
                        if od["cnt"] > tgt.get(key, (None, 0))[1]:
                            tgt[key] = (s, od["cnt"])
                    for key, (s, v) in tgt.items():
                        if waited.get(key, 0) < v:
                            e.wait_ge(s, v)
                            waited[key] = v
                    ins = op["fn"](e)
                    if op["dma"] is not None:
                        ins.then_inc(dsem[op["dma"]], 16)
                    elif op["sig"]:
                        ins.then_inc(esem[(ename, op["ep"])], 1)
                if ename == "sp":
                    for k in final_dma:
                        e.wait_ge(dsem[k], dcount[k])

            block.tensor(lambda e: run(e, "pe"))
            block.scalar(lambda e: run(e, "act"))
            block.vector(lambda e: run(e, "dve"))
            block.gpsimd(lambda e: run(e, "pool"))
            block.sync(lambda e: run(e, "sp"))


def build(T, layers):
    NCH = T // 512
    NKB = T // 128
    nc = bass.Bass("TRN2", target_bir_lowering=False)

    def din(name, shape, dt=F32):
        return nc.dram_tensor(name, list(shape), dt, kind="ExternalInput").ap()

    xT_in = din("xT", [128, 8, T])
    gmix = din("gmix", [128, 4, 8])
    gffn = din("gffn", [128, 4, 8])
    ev_w_in = din("ev_w_in", [2, D, EV_COLS])
    ev_w_out = din("ev_w_out", [2, D, D])
    od_w_qkv = din("od_w_qkv", [2, D, 3 * D])
    od_w_out = din("od_w_out", [2, D, D])
    w1 = din("ffn_w1", [4, D, DFF])
    w3 = din("ffn_w3", [4, D, DFF])
    w2 = din("ffn_w2", [4, DFF, D])
    qg_in = din("qg", [128, 2])
    kg_in = din("kg", [128, 2])
    poolw_in = din("pool_w", [2, 4, 128, 128])
    pscale_in = din("pscale", [128, 2, 4])
    cbf_in = din("cbf", [128, 2048 + 5 * 128])
    cf_in = din("cf", [128, 2048 + 64 + NIT])
    yT = nc.dram_tensor("yT", [128, 8, T], F32, kind="ExternalOutput").ap()

    XT_d = nc.dram_tensor("XT_d", [128, 8, T], F32).ap()
    QT_d = nc.dram_tensor("QT_d", [D, T], BF16).ap()
    KT_d = nc.dram_tensor("KT_d", [D, T], BF16).ap()
    V_d = nc.dram_tensor("V_d", [T, D], BF16).ap()
    VA_d = nc.dram_tensor("VA_d", [T, 130], BF16).ap()
    QI_d = nc.dram_tensor("QI_d", [256, T], BF16).ap()
    KI_d = nc.dram_tensor("KI_d", [64, T], BF16).ap()
    WI_d = nc.dram_tensor("WI_d", [T, 4], F32).ap()
    OT_d = nc.dram_tensor("OT_d", [D, T], BF16).ap()

    S = Sched(nc)
    with contextlib.ExitStack() as st:
        def sb(name, shape, dt):
            return st.enter_context(nc.sbuf_tensor(name, list(shape), dt))

        xTt = sb("xTt", [128, 8, 512], F32)
        hT = sb("hT", [128, 8, 512], BF16)
        sq = sb("sq", [128, 8, 512], BF16)
        cbf = sb("cbf_s", [128, 2048 + 5 * 128], BF16)
        cf = sb("cf_s", [128, 2048 + 64 + NIT], F32)
        gm = sb("gm", [128, 4, 8], F32)
        gf = sb("gf", [128, 4, 8], F32)
        qg = sb("qg_s", [128, 2], F32)
        kg = sb("kg_s", [128, 2], F32)
        psc = sb("psc", [128, 2, 4], F32)
        poolw = sb("poolw", [128, 2, 4, 128], BF16)
        cst = sb("cst", [128, 4], F32)
        t32a = sb("t32a", [128, 512], F32)
        t32b = sb("t32b", [128, 512], F32)
        t32c = sb("t32c", [128, 512], F32)
        ARENA = 56320
        arena = sb("arena", [128, ARENA], BF16)
        banks = [st.enter_context(nc.psum_tensor("bank%d" % i, [128, 512], F32)) for i in range(8)]

        maskS = cbf[:, 0:2048].rearrange("p (a b) -> p a b", a=4)
        negtri = cbf[:, 2048:2176]
        negones = cbf[:, 2176:2304]
        ident = cbf[:, 2304:2432]
        blockones = cbf[:, 2432:2560]
        ones = cbf[:, 2560:2688]
        maskQ = cf[:, 0:2048].rearrange("p (a b) -> p a b", a=4)
        rc16 = cf[:, 2048:2112].rearrange("p (a b) -> p a b", a=4)
        pow2 = cf[:, 2112:2112 + NIT]

        apos = [0]

        def carve(n_el, dt, shape=None):
            nb = n_el * (4 if dt == F32 else 2)
            nb = (nb + 63) // 64 * 64
            o = apos[0]
            assert o + nb <= ARENA * 2, (o, nb)
            apos[0] = o + nb
            v = arena[:, o // 2:(o + nb) // 2]
            if dt == F32:
                v = v.bitcast(F32)
            v = v[:, 0:n_el]
            if shape is not None:
                names = " ".join("a%d" % i for i in range(len(shape)))
                kw = {"a%d" % i: s for i, s in enumerate(shape[:-1])}
                v = v.rearrange("p (%s) -> p %s" % (names, names), **kw)
            return v

        def mm(out, lhsT, rhs, start, stop, reads, writes):
            S.add("pe", lambda e: e.matmul(out, lhsT, rhs, start=start, stop=stop), reads, writes)

        def act(out, in_, func, reads, writes, bias=None, scale=None):
            kw = {}
            if bias is not None:
                kw["bias"] = bias
            if scale is not None:
                kw["scale"] = scale
            S.add("act", lambda e: e.activation(out=out, in_=in_, func=func, **kw), reads, writes)

        def dma(out, in_, reads, writes, slot, eng="sp"):
            S.add(eng, lambda e: e.dma_start(out=out, in_=in_), reads, writes, dma=slot)

        def tt(out, in0, in1, op, reads, writes, eng="dve"):
            S.add(eng, lambda e: e.tensor_tensor(out=out, in0=in0, in1=in1, op=op), reads, writes)

        def ts(out, in0, s1, s2, op0, op1, reads, writes, accum=None, eng="dve"):
            if accum is None:
                S.add(eng, lambda e: e.tensor_scalar(out=out, in0=in0, scalar1=s1, scalar2=s2, op0=op0, op1=op1),
                      reads, writes)
            else:
                S.add(eng, lambda e: e.tensor_scalar(out=out, in0=in0, scalar1=s1, scalar2=s2, op0=op0, op1=op1,
                                                     accum_out=accum), reads, writes)

        def stt(out, in0, scalar, in1, op0, op1, reads, writes, eng="dve"):
            S.add(eng, lambda e: e.scalar_tensor_tensor(out=out, in0=in0, scalar=scalar, in1=in1, op0=op0, op1=op1),
                  reads, writes)

        def memset(ap, v, writes, eng="pool"):
            S.add(eng, lambda e: e.memset(ap, v), (), writes)

        def wview(w_ap):
            return w_ap.rearrange("(k p) n -> p k n", p=128)

        dma(cbf[:], cbf_in[:, :], (), ["cbf"], "c", eng="pool")
        dma(cf[:], cf_in[:, :], (), ["cf"], "c")
        dma(gm[:], gmix[:, :, :], (), ["gm"], "c")
        dma(gf[:], gffn[:, :, :], (), ["gf"], "c")
        dma(qg[:], qg_in[:, :], (), ["qg"], "c")
        dma(kg[:], kg_in[:, :], (), ["kg"], "c")
        dma(psc[:], pscale_in[:, :, :], (), ["psc"], "c")
        for i in range(2):
            dma(poolw[:, i, :, :], poolw_in[i].rearrange("g c d -> c g d"), (), ["poolw%d" % i], "c", eng="pool")
        memset(cst[:, 0:1], EPS, ["cst"])
        memset(cst[:, 1:2], 1.0, ["cst"])
        memset(cst[:, 2:3], 64 * EPS, ["cst"])
        memset(cst[:, 3:4], 0.0, ["cst"])

        bk = ["ps%d" % i for i in range(8)]

        def norm(gcol_t, l, gkey):
            act(sq[:], xTt[:], AF.Square, ["xT"], ["sq"])
            for k in range(8):
                mm(banks[7][:], ones, sq[:, k, :], k == 0, k == 7, ["sq", "cbf"], [bk[7]])
            act(t32a[:], banks[7][:], AF.Ln, [bk[7], "cst"], ["t32a"], bias=cst[:, 0:1], scale=1.0 / D)
            act(t32b[:], t32a[:], AF.Exp, ["t32a"], ["t32b"], scale=-0.5)
            for k in range(8):
                stt(hT[:, k, :], xTt[:, k, :], gcol_t[:, l, k:k + 1], t32b[:], ALU.mult, ALU.mult,
                    ["xT", "t32b", gkey], [("hT", k)])

        hkeys = [("hT", k) for k in range(8)]

        for l in range(layers):
            i = l // 2
            even = (l % 2 == 0)
            src = xT_in if l == 0 else XT_d
            dst = yT if l == layers - 1 else XT_d
            srck = "xin" if l == 0 else "XT"
            dstk = "yT" if l == layers - 1 else "XT"

            S.barrier()
            apos[0] = 0
            ring = [carve(11520, BF16) for _ in range(3)]
            stg = [carve(512, BF16) for _ in range(4)]
            rct = [0]
            sct = [0]

            def ring_load(src_ap, shape3, key):
                r = rct[0] % 3
                rct[0] += 1
                n = shape3[1] * shape3[2]
                v = ring[r][:, 0:n].rearrange("p (a b) -> p a b", a=shape3[1])
                dma(v, src_ap, [], [("ring", r)], ("ring", r), eng="pool")
                return v, ("ring", r)

            def stage_out(ps_ap, dram_ap, psk, dkey, scale=None, nparts=128):
                s = sct[0] % 4
                sct[0] += 1
                o = stg[s][0:nparts, :]
                if scale is None:
                    act(o, ps_ap, AF.Copy, [psk], [("stg", s)])
                else:
                    act(o, ps_ap, AF.Copy, [psk, "psc"], [("stg", s)], scale=scale)
                dma(dram_ap, o, [("stg", s)], [dkey], ("stgo", s))

            if even:
                uext = carve(4 * 528, F32, [4, 528])
                pa = carve(528, F32)
                pb = carve(528, F32)
                mixb = carve(512, BF16)
                vstage = carve(4 * 130, BF16, [4, 130])
                wist = carve(16, F32, [4, 4])
                t16 = carve(16, F32)
                memset(uext[:, :, 0:16], 0.0, ["uext"])
                memset(vstage[:], 1.0, ["vstage"])

            for j in range(NCH):
                cs = slice(j * 512, (j + 1) * 512)
                dma(xTt[:], src[:, :, cs], [(srck, j)], ["xT"], "xld")
                norm(gm, l, "gm")
                bi = [0]

                def nb_bank():
                    b = bi[0] % 4
                    bi[0] += 1
                    return b

                if not even:
                    wq = wview(od_w_qkv[i])
                    for piece in range(3):
                        W, wk = ring_load(wq[:, :, piece * D:(piece + 1) * D], [128, 8, D], None)
                        if piece < 2:
                            tgt = QT_d if piece == 0 else KT_d
                            tk = "QT" if piece == 0 else "KT"
                            for nb in range(8):
                                b = nb_bank()
                                for k in range(8):
                                    mm(banks[b][:], W[:, k, nb * 128:(nb + 1) * 128], hT[:, k, :], k == 0, k == 7,
                                       [wk, ("hT", k)], [bk[b]])
                                stage_out(banks[b][:], tgt[nb * 128:(nb + 1) * 128, cs], bk[b], (tk, j),
                                          scale=(0.125 if piece == 0 else None))
                        else:
                            for tb in range(4):
                                for nh in range(2):
                                    b = nb_bank()
                                    for k in range(8):
                                        mm(banks[b][:], hT[:, k, tb * 128:(tb + 1) * 128], W[:, k, nh * 512:(nh + 1) * 512],
                                           k == 0, k == 7, [wk, ("hT", k)], [bk[b]])
                                    stage_out(banks[b][:], V_d[j * 512 + tb * 128:j * 512 + (tb + 1) * 128, nh * 512:(nh + 1) * 512],
                                              bk[b], ("V", j))
                else:
                    wv = wview(ev_w_in[i])
                    WA, wak = ring_load(wv[:, :, 0:1092], [128, 8, 1092], None)
                    WB, wbk = ring_load(wv[:, :, 1092:1604], [128, 8, 512], None)

                    def qkproc(c0, gcol, gkey, lnscale, lnbias, dram_ap, dkey):
                        b = nb_bank()
                        for k in range(8):
                            mm(banks[b][:], WA[:, k, c0:c0 + 128], hT[:, k, :], k == 0, k == 7, [wak, ("hT", k)], [bk[b]])
                        act(t32c[:], banks[b][:], AF.Copy, [bk[b]], ["t32c"])
                        act(sq[:, 0, :], banks[b][:], AF.Square, [bk[b]], ["sq"])
                        mm(banks[6][:], blockones, sq[:, 0, :], True, True, ["sq", "cbf"], [bk[6]])
                        act(t32a[:], banks[6][:], AF.Ln, [bk[6], "cst"], ["t32a"], bias=lnbias, scale=lnscale)
                        act(t32b[:], t32a[:], AF.Exp, ["t32a"], ["t32b"], scale=-0.5)
                        s = sct[0] % 4
                        sct[0] += 1
                        stt(stg[s][:], t32c[:], gcol, t32b[:], ALU.mult, ALU.mult, ["t32c", "t32b", gkey], [("stg", s)])
                        dma(dram_ap, stg[s][:], [("stg", s)], [dkey], ("stgo", s))

                    for nb in range(4):
                        qkproc(nb * 128, qg[:, i:i + 1], "qg", 1.0, cst[:, 2:3], QT_d[nb * 128:(nb + 1) * 128, cs], ("QT", j))
                    qkproc(512, kg[:, i:i + 1], "kg", 1.0 / 64, cst[:, 0:1], KT_d[0:128, cs], ("KT", j))
                    for tb in range(4):
                        b = nb_bank()
                        for k in range(8):
                            mm(banks[b][:, 0:128], hT[:, k, tb * 128:(tb + 1) * 128], WA[:, k, 640:768], k == 0, k == 7,
                               [wak, ("hT", k)], [bk[b]])
                        for k in range(8):
                            mm(banks[b][:, 128:132], hT[:, k, tb * 128:(tb + 1) * 128], WA[:, k, 1088:1092], k == 0, k == 7,
                               [wak, ("hT", k)], [bk[b]])
                        for g in range(2):
                            act(vstage[:, tb, g * 65:g * 65 + 64], banks[b][:, g * 64:(g + 1) * 64], AF.Copy, [bk[b]], ["vstage"])
                        act(wist[:, tb, :], banks[b][:, 128:132], AF.Copy, [bk[b]], ["wist"], scale=IDX_SCALE)
                    dma(VA_d[cs, :].rearrange("(tb s) c -> s tb c", s=128), vstage[:], ["vstage"], [("VA", j)], "vao")
                    dma(WI_d[cs, :].rearrange("(tb s) c -> s tb c", s=128), wist[:], ["wist"], [("WI", j)], "wio")
                    for nb in range(2):
                        b = nb_bank()
                        for k in range(8):
                            mm(banks[b][:], WA[:, k, 768 + nb * 128:768 + (nb + 1) * 128], hT[:, k, :], k == 0, k == 7,
                               [wak, ("hT", k)], [bk[b]])
                        stage_out(banks[b][:], QI_d[nb * 128:(nb + 1) * 128, cs], bk[b], ("QI", j))
                    b = nb_bank()
                    for k in range(8):
                        mm(banks[b][0:64, :], WA[:, k, 1024:1088], hT[:, k, :], k == 0, k == 7, [wak, ("hT", k)], [bk[b]])
                    stage_out(banks[b][0:64, :], KI_d[0:64, cs], bk[b], ("KI", j), nparts=64)
                    for g in range(4):
                        b = nb_bank()
                        for k in range(8):
                            mm(banks[b][:], WB[:, k, g * 128:(g + 1) * 128], hT[:, k, :], k == 0, k == 7, [wbk, ("hT", k)], [bk[b]])
                        act(uext[:, g, 16:528], banks[b][:], AF.Copy, [bk[b]], ["uext"])
                        a_ap, a_key = uext[:, g, :], "uext"
                        dsh = 1
                        tgl = 0
                        for _ in range(g + 1):
                            o_ap, o_key = (pa, "pa") if tgl == 0 else (pb, "pb")
                            lo_ = 2 * dsh - 1
                            tt(o_ap[:, lo_:528], a_ap[:, lo_:528], a_ap[:, lo_ - dsh:528 - dsh], ALU.add,
                               [a_key], [o_key], eng="pool")
                            a_ap, a_key = o_ap, o_key
                            dsh *= 2
                            tgl ^= 1
                        w = WINS[g]
                        stt(mixb[:], a_ap[:, 16:528], 1.0 / w, uext[:, g, 16:528], ALU.mult, ALU.subtract,
                            [a_key, "uext"], ["mixb"])
                        if j == 0:
                            tt(t16[:], a_ap[:, 16:32], rc16[:, g, :], ALU.mult, [a_key, "cf"], ["t16"])
                            tt(mixb[:, 0:16], t16[:], uext[:, g, 16:32], ALU.subtract, ["t16", "uext", "mixb"], ["mixb"])
                        b2 = nb_bank()
                        mm(banks[b2][:], poolw[:, i, g, :], mixb[:], True, True, ["mixb", "poolw%d" % i], [bk[b2]])
                        stage_out(banks[b2][:], OT_d[(4 + g) * 128:(5 + g) * 128, cs], bk[b2], ("OT", j),
                                  scale=psc[:, i, g:g + 1])
                    S.add("pool", (lambda o_, i_: (lambda e: e.tensor_copy(out=o_, in_=i_)))(uext[:, :, 0:16], uext[:, :, 512:528]),
                          ["uext", "pa", "pb", "mixb"], ["uext"])

            S.barrier()
            apos[0] = 0
            allk = lambda nm, jj: [(nm, c) for c in range(jj + 1)]
            if not even:
                KTs = carve(T, BF16)
                Vz = [carve(NKB * 128, BF16, [NKB, 128]) for _ in range(2)]
                QTc = [carve(512, BF16) for _ in range(2)]
                Eb = [carve(512, F32) for _ in range(2)]
                spb = [carve(512, BF16) for _ in range(2)]
                Rb = [carve(512, BF16) for _ in range(2)]
                aTb = [carve(512, BF16) for _ in range(2)]
                oTs = [carve(512, BF16) for _ in range(2)]
                memset(Vz[0][:, :, 64:128], 0.0, ["Vz0"])
                memset(Vz[1][:, :, 0:64], 0.0, ["Vz1"])
                Vv = V_d.rearrange("(kb s) c -> s kb c", s=128)
                tile_i = 0
                qi_ = 0
                for hp in range(8):
                    dma(KTs[:, :], KT_d[hp * 128:(hp + 1) * 128, :], allk("KT", NCH - 1), ["KTs"], "ktl")
                    dma(Vz[0][:, :, 0:64], Vv[:, :, hp * 128:hp * 128 + 64], allk("V", NCH - 1), ["Vz0"], "vz0")
                    dma(Vz[1][:, :, 64:128], Vv[:, :, hp * 128 + 64:hp * 128 + 128], allk("V", NCH - 1), ["Vz1"], "vz1")
                    for j in range(NCH):
                        cs = slice(j * 512, (j + 1) * 512)
                        qs = qi_ % 2
                        qi_ += 1
                        dma(QTc[qs][:, :], QT_d[hp * 128:(hp + 1) * 128, cs], [("QT", j)], [("QTc", qs)], ("qtl", qs))
                        bo = 4 + qs
                        first = True
                        for e_ in range(2):
                            ps_ = slice(e_ * 64, (e_ + 1) * 64)
                            Rcur = None
                            Rk = None
                            ri = 0
                            for kb in range(4 * j + 3, -1, -1):
                                diag = kb >= 4 * j
                                kbl = kb - 4 * j
                                x = tile_i % 2
                                tile_i += 1
                                A, Ak = banks[x], bk[x]
                                B, Bk = banks[2 + x], bk[2 + x]
                                ksl = slice(kb * 128, (kb + 1) * 128)
                                mm(A[:], KTs[ps_, ksl], QTc[qs][ps_, :], True, not diag, ["KTs", ("QTc", qs)], [Ak])
                                if diag:
                                    mm(A[:], ident, maskS[:, kbl, :], False, True, ["cbf"], [Ak])
                                act(Eb[x][:], A[:], AF.Exp, [Ak], [("Eb", x)])
                                act(spb[x][:], Eb[x][:], AF.Ln, [("Eb", x), "cst"], [("spb", x)], bias=cst[:, 1:2], scale=1.0)
                                mm(B[:], KTs[ps_, ksl], QTc[qs][ps_, :], True, False, ["KTs", ("QTc", qs)], [Bk])
                                if diag:
                                    mm(B[:], ident, maskS[:, kbl, :], False, False, ["cbf"], [Bk])
                                mm(B[:], negtri, spb[x][:], False, Rcur is None, ["cbf", ("spb", x)], [Bk])
                                if Rcur is not None:
                                    mm(B[:], negones, Rcur[:], False, True, ["cbf", Rk], [Bk])
                                act(aTb[x][:], B[:], AF.Exp, [Bk], [("aTb", x)])
                                mm(banks[bo][:], Vz[e_][:, kb, :], aTb[x][:], first, (e_ == 1 and kb == 0),
                                   ["Vz%d" % e_, ("aTb", x)], [bk[bo]])
                                first = False
                                if kb > 0:
                                    rn = ri % 2
                                    ri += 1
                                    if Rcur is None:
                                        S.add("dve", (lambda o, i_: (lambda e: e.tensor_copy(out=o, in_=i_)))(Rb[rn][:], spb[x][:]),
                                              [("spb", x)], [("Rb", rn)])
                                    else:
                                        tt(Rb[rn][:], Rcur[:], spb[x][:], ALU.add, [Rk, ("spb", x)], [("Rb", rn)])
                                    Rcur, Rk = Rb[rn], ("Rb", rn)
                        act(oTs[qs][:], banks[bo][:], AF.Copy, [bk[bo]], [("oTs", qs)])
                        dma(OT_d[hp * 128:(hp + 1) * 128, cs], oTs[qs][:], [("oTs", qs)], [("OT", j)], ("oto", qs))
            else:
                KTs = carve(T, BF16)
                KI2 = carve(T, BF16)
                VAs = carve(NKB * 130, BF16, [NKB, 130])
                sc = carve(T, F32)
                junk = carve(T, BF16)
                mb = [carve(T, BF16) for _ in range(4)]
                QTc = carve(4 * 512, BF16, [4, 512])
                QIc = carve(2 * 512, BF16, [2, 512])
                wic = carve(16, F32, [4, 4])
                rtmp = [carve(512, F32) for _ in range(2)]
                PT = [carve(512, BF16) for _ in range(2)]
                otok = carve(4 * 512, BF16, [4, 512])
                oTc = carve(4 * 512, BF16, [4, 512])
                sm = carve(8 + NIT, F32)
                rd = carve(4, F32)
                dma(KTs[:, :], KT_d[0:128, :], allk("KT", NCH - 1), ["KTs"], "ktl")
                dma(KI2[0:64, :], KI_d[:, :], allk("KI", NCH - 1), ["KI2"], "kil")
                dma(KI2[64:128, :], KI_d[:, :], allk("KI", NCH - 1), ["KI2"], "kil")
                dma(VAs[:, :, :], VA_d.rearrange("(kb s) c -> s kb c", s=128), allk("VA", NCH - 1), ["VAs"], "val")
                ti = 0
                for j in range(NCH):
                    cs = slice(j * 512, (j + 1) * 512)
                    L = (j + 1) * 512
                    dma(QTc[0:64, :, :], QT_d[0:256, cs].rearrange("(h d) t -> d h t", d=64), [("QT", j)], ["QTc"], "qtl")
                    dma(QTc[64:128, :, :], QT_d[256:512, cs].rearrange("(h d) t -> d h t", d=64), [("QT", j)], ["QTc"], "qtl")
                    dma(QIc[:, :, :], QI_d[:, cs].rearrange("(b p) t -> p b t", p=128), [("QI", j)], ["QIc"], "qil")
                    dma(wic[:, :, :], WI_d[cs, :].rearrange("(tb s) c -> s tb c", s=128), [("WI", j)], ["wic"], "wil")
                    for qb in range(4):
                        for sg in range(j + 1):
                            ssl = slice(sg * 512, (sg + 1) * 512)
                            for hi in range(4):
                                x = ti % 2
                                ti += 1
                                hp_ = slice((hi % 2) * 64, (hi % 2) * 64 + 64)
                                mm(banks[x][:], QIc[hp_, hi // 2, qb * 128:(qb + 1) * 128], KI2[hp_, ssl], True, True,
                                   ["QIc", "KI2"], [bk[x]])
                                act(rtmp[x][:], banks[x][:], AF.Relu, [bk[x]], [("rtmp", x)])
                                if hi == 0:
                                    ts(sc[:, ssl], rtmp[x][:], wic[:, qb, 0:1], None, ALU.mult, ALU.bypass,
                                       [("rtmp", x), "wic"], ["sc"])
                                else:
                                    stt(sc[:, ssl], rtmp[x][:], wic[:, qb, hi:hi + 1], sc[:, ssl], ALU.mult, ALU.add,
                                        [("rtmp", x), "wic", "sc"], ["sc"])
                        S.add("dve", (lambda o_, i_: (lambda e: e.tensor_reduce(out=o_, in_=i_, axis=AX.X, op=ALU.max)))(sm[:, 0:1], sc[:, 0:L]),
                              ["sc"], ["sm"])
                        S.add("dve", (lambda o_, i_: (lambda e: e.tensor_reduce(out=o_, in_=i_, axis=AX.X, op=ALU.min)))(sm[:, 1:2], sc[:, 0:L]),
                              ["sc"], ["sm"])
                        tt(sc[:, j * 512:(j + 1) * 512], sc[:, j * 512:(j + 1) * 512], maskQ[:, qb, :], ALU.add,
                           ["sc", "cf"], ["sc"])
                        if j == 0 and qb < 2:
                            thr = sm[:, 1:2]
                        else:
                            tt(sm[:, 2:3], sm[:, 0:1], sm[:, 1:2], ALU.subtract, ["sm"], ["sm"])
                            ts(sm[:, 8:8 + NIT], pow2, sm[:, 2:3], None, ALU.mult, ALU.bypass, ["sm", "cf"], ["sm"])
                            S.add("dve", (lambda o_, i_: (lambda e: e.tensor_copy(out=o_, in_=i_)))(sm[:, 3:4], sm[:, 1:2]), ["sm"], ["sm"])
                            for it in range(NIT):
                                tt(sm[:, 4:5], sm[:, 3:4], sm[:, 8 + it:9 + it], ALU.add, ["sm"], ["sm"])
                                ts(junk[:, 0:L], sc[:, 0:L], sm[:, 4:5], None, ALU.is_ge, ALU.add, ["sm", "sc"],
                                   ["junk", "sm"], accum=sm[:, 5:6])
                                ts(sm[:, 6:7], sm[:, 5:6], 256.0, sm[:, 8 + it:9 + it], ALU.is_ge, ALU.mult, ["sm"], ["sm"])
                                tt(sm[:, 3:4], sm[:, 3:4], sm[:, 6:7], ALU.add, ["sm"], ["sm"])
                            thr = sm[:, 3:4]
                        ts(mb[qb][:, 0:L], sc[:, 0:L], thr, NEG, ALU.is_lt, ALU.mult, ["sm", "sc"], [("mb", qb)])
                    hb = 0
                    for h in range(8):
                        g = h // 4
                        gp = slice(g * 64, (g + 1) * 64)
                        bo = 4 + (hb % 2)
                        hb += 1
                        for kb in range(4 * j + 4):
                            x = ti % 2
                            ti += 1
                            ksl = slice(kb * 128, (kb + 1) * 128)
                            mm(banks[2 + x][:], KTs[gp, ksl], QTc[gp, h % 4, :], True, False, ["KTs", "QTc"], [bk[2 + x]])
                            for qb in range(4):
                                mm(banks[2 + x][:, qb * 128:(qb + 1) * 128], mb[qb][:, ksl], ident, False, qb == 3,
                                   [("mb", qb), "cbf"], [bk[2 + x]])
                            act(PT[x][:], banks[2 + x][:], AF.Exp, [bk[2 + x]], [("PT", x)])
                            for qb in range(4):
                                if kb <= 4 * j + qb:
                                    mm(banks[bo][:, qb * 65:(qb + 1) * 65], PT[x][:, qb * 128:(qb + 1) * 128],
                                       VAs[:, kb, g * 65:(g + 1) * 65], (kb == 0 and qb == 0), kb == 4 * j + qb,
                                       [("PT", x), "VAs"], [bk[bo]])
                        bov = banks[bo][:, 0:260].rearrange("p (a b) -> p a b", a=4)
                        S.add("dve", (lambda o_, bv: (lambda e: e.reciprocal(out=o_, in_=bv)))(rd[:, :], bov[:, :, 64]), [bk[bo]], ["rd"])
                        for qb in range(4):
                            ts(otok[:, qb, h * 64:(h + 1) * 64], banks[bo][:, qb * 65:qb * 65 + 64], rd[:, qb:qb + 1], None,
                               ALU.mult, ALU.bypass, [bk[bo], "rd"], ["otok"])
                    for cb in range(4):
                        x = ti % 2
                        ti += 1
                        for qb in range(4):
                            mm(banks[x][:, qb * 128:(qb + 1) * 128], otok[:, qb, cb * 128:(cb + 1) * 128], ident, True, True,
                               ["otok", "cbf"], [bk[x]])
                        act(oTc[:, cb, :], banks[x][:], AF.Copy, [bk[x]], ["oTc"])
                    dma(OT_d[0:512, cs].rearrange("(c p) t -> p c t", p=128), oTc[:, :, :], ["oTc"], [("OT", j)], "oto")

            S.barrier()
            apos[0] = 0
            ring = [carve(11520, BF16) for _ in range(3)]
            oTl = carve(8 * 512, BF16, [8, 512])
            gT = carve(NFB * 512, BF16, [NFB, 512])
            rct = [0]
            w_out = wview(od_w_out[i] if not even else ev_w_out[i])
            w1v = wview(w1[l])
            w3v = wview(w3[l])
            w2v = w2[l].rearrange("(f p) n -> p f n", p=128)
            for j in range(NCH):
                cs = slice(j * 512, (j + 1) * 512)
                dma(xTt[:], src[:, :, cs], [(srck, j)], ["xT"], "xld")
                dma(oTl[:, :, :], OT_d[:, cs].rearrange("(c p) t -> p c t", p=128), [("OT", j)], ["oTl"], "otl")
                Wo, wok = ring_load(w_out[:, :, :], [128, 8, D], None)
                pbk = [0]

                def nbank():
                    b = pbk[0] % 6
                    pbk[0] += 1
                    return b

                for nb in range(8):
                    b = nbank()
                    for c in range(8):
                        mm(banks[b][:], Wo[:, c, nb * 128:(nb + 1) * 128], oTl[:, c, :], c == 0, c == 7, [wok, "oTl"], [bk[b]])
                    tt(xTt[:, nb, :], xTt[:, nb, :], banks[b][:], ALU.add, ["xT", bk[b]], ["xT"])
                norm(gf, l, "gf")
                for half in range(2):
                    fs = slice(half * 1408, (half + 1) * 1408)
                    W1p, k1 = ring_load(w1v[:, :, fs], [128, 8, 1408], None)
                    W3p, k3 = ring_load(w3v[:, :, fs], [128, 8, 1408], None)
                    for fb in range(11):
                        f = half * 11 + fb
                        ba = nbank()
                        bb = nbank()
                        for k in range(8):
                            mm(banks[ba][:], W1p[:, k, fb * 128:(fb + 1) * 128], hT[:, k, :], k == 0, k == 7, [k1, ("hT", k)], [bk[ba]])
                        for k in range(8):
                            mm(banks[bb][:], W3p[:, k, fb * 128:(fb + 1) * 128], hT[:, k, :], k == 0, k == 7, [k3, ("hT", k)], [bk[bb]])
                        tsel = (t32a, "t32a") if f % 2 == 0 else (t32c, "t32c")
                        act(tsel[0][:], banks[ba][:], AF.Silu, [bk[ba]], [tsel[1]])
                        tt(gT[:, f, :], tsel[0][:], banks[bb][:], ALU.mult, [tsel[1], bk[bb]], [("gT", f)])
                for nh in range(2):
                    W2p, k2 = ring_load(w2v[:, :, nh * 512:(nh + 1) * 512], [128, NFB, 512], None)
                    for nbl in range(4):
                        nb = nh * 4 + nbl
                        b = nbank()
                        for f in range(NFB):
                            mm(banks[b][:], W2p[:, f, nbl * 128:(nbl + 1) * 128], gT[:, f, :], f == 0, f == NFB - 1,
                               [k2, ("gT", f)], [bk[b]])
                        tt(xTt[:, nb, :], xTt[:, nb, :], banks[b][:], ALU.add, ["xT", bk[b]], ["xT"])
                dma(dst[:, :, cs], xTt[:], ["xT"], [(dstk, j)], "xst")

        S.emit(final_dma=["xst"])
    return nc


def host_consts():
    s = np.arange(128)[:, None]
    t = np.arange(512)[None, :]
    maskS = np.zeros((128, 4, 512), np.float32)
    maskQ = np.zeros((128, 4, 512), np.float32)
    for a in range(4):
        maskS[:, a, :] = np.where(a * 128 + s < t, 0.0, NEG)
        maskQ[:, a, :] = np.where(t <= a * 128 + s, 0.0, -3.0e38)
    jj = np.arange(128)[:, None]
    ss = np.arange(128)[None, :]
    negtri = np.where(jj >= ss, -1.0, 0.0).astype(np.float32)
    negones = -np.ones((128, 128), np.float32)
    ident = np.eye(128, dtype=np.float32)
    blockones = (jj // 64 == ss // 64).astype(np.float32)
    ones = np.ones((128, 128), np.float32)
    cbf = np.concatenate([maskS.reshape(128, 2048), negtri, negones, ident, blockones, ones], axis=1)
    rc16 = np.zeros((128, 4, 16), np.float32)
    for g, w in enumerate(WINS):
        rc16[:, g, :] = 1.0 / np.minimum(np.arange(16) + 1, w)
    pow2 = np.tile((0.5 ** (np.arange(NIT) + 1)).astype(np.float32)[None, :], (128, 1))
    cf = np.concatenate([maskQ.reshape(128, 2048), rc16.reshape(128, 64), pow2], axis=1)
    return np.ascontiguousarray(cbf, np.float32), np.ascontiguousarray(cf, np.float32)


def make_in_maps(inputs, T, nseq):
    cbf, cf = host_consts()
    f = lambda a: np.ascontiguousarray(np.asarray(a, dtype=np.float32))
    x = np.asarray(inputs["x"], dtype=np.float32)

    def gl(g):
        return np.ascontiguousarray(np.asarray(g, np.float32).reshape(4, 8, 128).transpose(2, 0, 1))

    common = {
        "gmix": gl(inputs["norm_mix_g"]), "gffn": gl(inputs["norm_ffn_g"]),
        "ev_w_in": f(inputs["ev_w_in"]), "ev_w_out": f(inputs["ev_w_out"]),
        "od_w_qkv": f(inputs["od_w_qkv"]), "od_w_out": f(inputs["od_w_out"]),
        "ffn_w1": f(inputs["ffn_w1"]), "ffn_w3": f(inputs["ffn_w3"]), "ffn_w2": f(inputs["ffn_w2"]),
        "qg": np.ascontiguousarray(np.tile(np.asarray(inputs["ev_q_norm_g"], np.float32), (1, 2)).T),
        "kg": np.ascontiguousarray(np.tile(np.asarray(inputs["ev_k_norm_g"], np.float32), (1, 2)).T),
        "pool_w": f(inputs["ev_pool_w"]),
        "pscale": np.ascontiguousarray(np.asarray(inputs["ev_pool_scale"], np.float32).reshape(2, 4, 128).transpose(2, 0, 1)),
        "cbf": cbf, "cf": cf,
    }
    maps = []
    for c in range(8):
        b = c % nseq
        xT = np.ascontiguousarray(x[b].T.reshape(8, 128, T).transpose(1, 0, 2))
        m = dict(common)
        m["xT"] = xT
        maps.append(m)
    return maps


_NC_CACHE = {}


def run(inputs, T, layers, nseq):
    key = (T, layers)
    if key not in _NC_CACHE:
        _NC_CACHE[key] = build(T, layers)
    nc = _NC_CACHE[key]
    maps = make_in_maps(inputs, T, nseq)
    res = run_bass_kernel_spmd(nc, maps, core_ids=list(range(8)))
    outs = []
    for b in range(nseq):
        yT = np.asarray(res.results[b]["yT"])
        outs.append(yT.transpose(1, 0, 2).reshape(D, T).T)
    return np.ascontiguousarray(np.stack(outs, 0)).astype(np.float32)


def kernel(**inputs):
    return run(inputs, 4096, 4, 4)
```

```python
import contextlib
import numpy as np
import concourse.bass as bass
import concourse.mybir as mybir
from concourse.bass_utils import run_bass_kernel_spmd

F32 = mybir.dt.float32
BF16 = mybir.dt.bfloat16
ALU = mybir.AluOpType
AF = mybir.ActivationFunctionType
AX = mybir.AxisListType

D = 1024
DFF = 2816
NFB = 22
EV_COLS = 1604
NIT = 18
NEG = -30000.0
IDX_SCALE = 256 ** -0.5
WINS = (2, 4, 8, 16)
EPS = 1e-6


class Sched:
    def __init__(self, nc):
        self.nc = nc
        self.ops = []
        self.lastw = {}
        self.readers = {}
        self.pending = {}
        self.last_on = {}
        self.last_dma = {}
        self.epoch = 0

    def add(self, eng, fn, reads=(), writes=(), dma=None):
        idx = len(self.ops)
        hard, soft = set(), set()
        for k in reads:
            w = self.lastw.get(k)
            if w is not None:
                hard.add(w)
        for k in writes:
            w = self.lastw.get(k)
            if w is not None:
                hard.add(w)
            for r in self.readers.get(k, ()):
                soft.add(r)
        pb = self.pending.pop(eng, None)
        if pb:
            hard |= pb
        for k in reads:
            self.readers.setdefault(k, []).append(idx)
        for k in writes:
            self.lastw[k] = idx
            self.readers[k] = []
        self.ops.append(dict(eng=eng, fn=fn, hard=hard, soft=soft, dma=dma, sig=False, cnt=0, ep=self.epoch))
        if dma is None:
            self.last_on[eng] = idx
        else:
            self.last_dma[dma] = idx
        return idx

    def barrier(self):
        b = set(self.last_on.values()) | set(self.last_dma.values())
        self.epoch += 1
        for e in ("pe", "act", "dve", "pool", "sp"):
            self.pending[e] = set(b) | self.pending.get(e, set())

    def emit(self, final_dma):
        nc = self.nc
        ops = self.ops
        need = [None] * len(ops)
        for i, op in enumerate(ops):
            deps = set()
            for d in op["hard"]:
                od = ops[d]
                if od["dma"] is None and op["dma"] is None and od["eng"] == op["eng"] == "pe":
                    continue
                deps.add(d)
            for d in op["soft"]:
                od = ops[d]
                if od["dma"] is None and op["dma"] is None and od["eng"] == op["eng"]:
                    continue
                deps.add(d)
            need[i] = deps
            for d in deps:
                ops[d]["sig"] = True
        ecount = {}
        dcount = {}
        for op in ops:
            if op["dma"] is not None:
                dcount[op["dma"]] = dcount.get(op["dma"], 0) + 16
                op["cnt"] = dcount[op["dma"]]
            elif op["sig"]:
                ek = (op["eng"], op["ep"])
                ecount[ek] = ecount.get(ek, 0) + 1
                op["cnt"] = ecount[ek]
        with contextlib.ExitStack() as st:
            esem = {ek: st.enter_context(nc.semaphore("es_%s_%d" % ek)) for ek in sorted(ecount)}
            dsem = {k: st.enter_context(nc.semaphore("ds_%d" % n)) for n, k in enumerate(sorted(dcount, key=str))}
            block = st.enter_context(nc.Block())
            streams = {e: [] for e in ("pe", "act", "dve", "pool", "sp")}
            for i, op in enumerate(ops):
                streams[op["eng"]].append(i)

            def run(e, ename):
                waited = {}
                for i in streams[ename]:
                    op = ops[i]
                    tgt = {}
                    for d in need[i]:
                        od = ops[d]
                        s = dsem[od["dma"]] if od["dma"] is not None else esem[(od["eng"], od["ep"])]
                        key = id(s)
                        if od["cnt"] > tgt.get(key, (None, 0))[1]:
                            tgt[key] = (s, od["cnt"])
                    for key, (s, v) in tgt.items():
                        if waited.get(key, 0) < v:
                            e.wait_ge(s, v)
                            waited[key] = v
                    ins = op["fn"](e)
                    if op["dma"] is not None:
                        ins.then_inc(dsem[op["dma"]], 16)
                    elif op["sig"]:
                        ins.then_inc(esem[(ename, op["ep"])], 1)
                if ename == "sp":
                    for k in final_dma:
                        e.wait_ge(dsem[k], dcount[k])

            block.tensor(lambda e: run(e, "pe"))
            block.scalar(lambda e: run(e, "act"))
            block.vector(lambda e: run(e, "dve"))
            block.gpsimd(lambda e: run(e, "pool"))
            block.sync(lambda e: run(e, "sp"))


def build(T, layers):
    NCH = T // 512
    NKB = T // 128
    nc = bass.Bass("TRN2", target_bir_lowering=False)

    def din(name, shape, dt=F32):
        return nc.dram_tensor(name, list(shape), dt, kind="ExternalInput").ap()

    xT_in = din("xT", [128, 8, T])
    gmix = din("gmix", [128, 4, 8])
    gffn = din("gffn", [128, 4, 8])
    ev_w_in = din("ev_w_in", [2, D, EV_COLS])
    ev_w_out = din("ev_w_out", [2, D, D])
    od_w_qkv = din("od_w_qkv", [2, D, 3 * D])
    od_w_out = din("od_w_out", [2, D, D])
    w1 = din("ffn_w1", [4, D, DFF])
    w3 = din("ffn_w3", [4, D, DFF])
    w2 = din("ffn_w2", [4, DFF, D])
    qg_in = din("qg", [128, 2])
    kg_in = din("kg", [128, 2])
    poolw_in = din("pool_w", [2, 4, 128, 128])
    pscale_in = din("pscale", [128, 2, 4])
    cbf_in = din("cbf", [128, 2048 + 5 * 128])
    cf_in = din("cf", [128, 2048 + 64 + NIT])
    yT = nc.dram_tensor("yT", [128, 8, T], F32, kind="ExternalOutput").ap()

    XT_d = nc.dram_tensor("XT_d", [128, 8, T], F32).ap()
    QT_d = nc.dram_tensor("QT_d", [D, T], BF16).ap()
    KT_d = nc.dram_tensor("KT_d", [D, T], BF16).ap()
    V_d = nc.dram_tensor("V_d", [T, D], BF16).ap()
    VA_d = nc.dram_tensor("VA_d", [T, 130], BF16).ap()
    QI_d = nc.dram_tensor("QI_d", [256, T], BF16).ap()
    KI_d = nc.dram_tensor("KI_d", [64, T], BF16).ap()
    WI_d = nc.dram_tensor("WI_d", [T, 4], F32).ap()
    OT_d = nc.dram_tensor("OT_d", [D, T], BF16).ap()

    S = Sched(nc)
    with contextlib.ExitStack() as st:
        def sb(name, shape, dt):
            return st.enter_context(nc.sbuf_tensor(name, list(shape), dt))

        xTt = sb("xTt", [128, 8, 512], F32)
        hT = sb("hT", [128, 8, 512], BF16)
        sq = sb("sq", [128, 8, 512], BF16)
        cbf = sb("cbf_s", [128, 2048 + 5 * 128], BF16)
        cf = sb("cf_s", [128, 2048 + 64 + NIT], F32)
        gm = sb("gm", [128, 4, 8], F32)
        gf = sb("gf", [128, 4, 8], F32)
        qg = sb("qg_s", [128, 2], F32)
        kg = sb("kg_s", [128, 2], F32)
        psc = sb("psc", [128, 2, 4], F32)
        poolw = sb("poolw", [128, 2, 4, 128], BF16)
        cst = sb("cst", [128, 4], F32)
        t32a = sb("t32a", [128, 512], F32)
        t32b = sb("t32b", [128, 512], F32)
        t32c = sb("t32c", [128, 512], F32)
        ARENA = 56320
        arena = sb("arena", [128, ARENA], BF16)
        psall = st.enter_context(nc.psum_tensor("psall", [128, 4096], F32))
        banks = [psall[:, i * 512:(i + 1) * 512] for i in range(8)]

        maskS = cbf[:, 0:2048].rearrange("p (a b) -> p a b", a=4)
        negtri = cbf[:, 2048:2176]
        negones = cbf[:, 2176:2304]
        ident = cbf[:, 2304:2432]
        blockones = cbf[:, 2432:2560]
        ones = cbf[:, 2560:2688]
        maskQ = cf[:, 0:2048].rearrange("p (a b) -> p a b", a=4)
        rc16 = cf[:, 2048:2112].rearrange("p (a b) -> p a b", a=4)
        pow2 = cf[:, 2112:2112 + NIT]

        apos = [0]

        def carve(n_el, dt, shape=None):
            nb = n_el * (4 if dt == F32 else 2)
            nb = (nb + 63) // 64 * 64
            o = apos[0]
            assert o + nb <= ARENA * 2, (o, nb)
            apos[0] = o + nb
            v = arena[:, o // 2:(o + nb) // 2]
            if dt == F32:
                v = v.bitcast(F32)
            v = v[:, 0:n_el]
            if shape is not None:
                names = " ".join("a%d" % i for i in range(len(shape)))
                kw = {"a%d" % i: s for i, s in enumerate(shape[:-1])}
                v = v.rearrange("p (%s) -> p %s" % (names, names), **kw)
            return v

        def mm(out, lhsT, rhs, start, stop, reads, writes):
            S.add("pe", lambda e: e.matmul(out, lhsT, rhs, start=start, stop=stop), reads, writes)

        def act(out, in_, func, reads, writes, bias=None, scale=None):
            kw = {}
            if bias is not None:
                kw["bias"] = bias
            if scale is not None:
                kw["scale"] = scale
            S.add("act", lambda e: e.activation(out=out, in_=in_, func=func, **kw), reads, writes)

        def dma(out, in_, reads, writes, slot, eng="sp"):
            S.add(eng, lambda e: e.dma_start(out=out, in_=in_), reads, writes, dma=slot)

        def tt(out, in0, in1, op, reads, writes, eng="dve"):
            S.add(eng, lambda e: e.tensor_tensor(out=out, in0=in0, in1=in1, op=op), reads, writes)

        def ts(out, in0, s1, s2, op0, op1, reads, writes, accum=None, eng="dve"):
            if accum is None:
                S.add(eng, lambda e: e.tensor_scalar(out=out, in0=in0, scalar1=s1, scalar2=s2, op0=op0, op1=op1),
                      reads, writes)
            else:
                S.add(eng, lambda e: e.tensor_scalar(out=out, in0=in0, scalar1=s1, scalar2=s2, op0=op0, op1=op1,
                                                     accum_out=accum), reads, writes)

        def stt(out, in0, scalar, in1, op0, op1, reads, writes, eng="dve"):
            S.add(eng, lambda e: e.scalar_tensor_tensor(out=out, in0=in0, scalar=scalar, in1=in1, op0=op0, op1=op1),
                  reads, writes)

        def memset(ap, v, writes, eng="pool"):
            S.add(eng, lambda e: e.memset(ap, v), (), writes)

        def wview(w_ap):
            return w_ap.rearrange("(k p) n -> p k n", p=128)

        dma(cbf[:], cbf_in[:, :], (), ["cbf"], "c", eng="pool")
        dma(cf[:], cf_in[:, :], (), ["cf"], "c")
        dma(gm[:], gmix[:, :, :], (), ["gm"], "c")
        dma(gf[:], gffn[:, :, :], (), ["gf"], "c")
        dma(qg[:], qg_in[:, :], (), ["qg"], "c")
        dma(kg[:], kg_in[:, :], (), ["kg"], "c")
        dma(psc[:], pscale_in[:, :, :], (), ["psc"], "c")
        for i in range(2):
            dma(poolw[:, i, :, :], poolw_in[i].rearrange("g c d -> c g d"), (), ["poolw%d" % i], "c", eng="pool")
        memset(cst[:, 0:1], EPS, ["cst"])
        memset(cst[:, 1:2], 1.0, ["cst"])
        memset(cst[:, 2:3], 64 * EPS, ["cst"])
        memset(cst[:, 3:4], 0.0, ["cst"])

        bk = ["ps%d" % i for i in range(8)]

        def norm(gcol_t, l, gkey):
            act(sq[:], xTt[:], AF.Square, ["xT"], ["sq"])
            for k in range(8):
                mm(banks[7][:], ones, sq[:, k, :], k == 0, k == 7, ["sq", "cbf"], [bk[7]])
            act(t32a[:], banks[7][:], AF.Ln, [bk[7], "cst"], ["t32a"], bias=cst[:, 0:1], scale=1.0 / D)
            act(t32b[:], t32a[:], AF.Exp, ["t32a"], ["t32b"], scale=-0.5)
            for k in range(8):
                stt(hT[:, k, :], xTt[:, k, :], gcol_t[:, l, k:k + 1], t32b[:], ALU.mult, ALU.mult,
                    ["xT", "t32b", gkey], [("hT", k)])

        hkeys = [("hT", k) for k in range(8)]

        for l in range(layers):
            i = l // 2
            even = (l % 2 == 0)
            src = xT_in if l == 0 else XT_d
            dst = yT if l == layers - 1 else XT_d
            srck = "xin" if l == 0 else "XT"
            dstk = "yT" if l == layers - 1 else "XT"

            S.barrier()
            apos[0] = 0
            ring = [carve(11520, BF16) for _ in range(3)]
            stg = [carve(512, BF16) for _ in range(4)]
            rct = [0]
            sct = [0]

            def ring_load(src_ap, shape3, key):
                r = rct[0] % 3
                rct[0] += 1
                n = shape3[1] * shape3[2]
                v = ring[r][:, 0:n].rearrange("p (a b) -> p a b", a=shape3[1])
                dma(v, src_ap, [], [("ring", r)], ("ring", r), eng="pool")
                return v, ("ring", r)

            def stage_out(ps_ap, dram_ap, psk, dkey, scale=None, nparts=128):
                s = sct[0] % 4
                sct[0] += 1
                o = stg[s][0:nparts, :]
                if scale is None:
                    act(o, ps_ap, AF.Copy, [psk], [("stg", s)])
                else:
                    act(o, ps_ap, AF.Copy, [psk, "psc"], [("stg", s)], scale=scale)
                dma(dram_ap, o, [("stg", s)], [dkey], ("stgo", s))

            if even:
                uext = carve(4 * 528, F32, [4, 528])
                pa = carve(528, F32)
                pb = carve(528, F32)
                mixb = carve(512, BF16)
                vstage = carve(4 * 130, BF16, [4, 130])
                wist = carve(16, F32, [4, 4])
                t16 = carve(16, F32)
                memset(uext[:, :, 0:16], 0.0, ["uext"])
                memset(vstage[:], 1.0, ["vstage"])

            for j in range(NCH):
                cs = slice(j * 512, (j + 1) * 512)
                dma(xTt[:], src[:, :, cs], [(srck, j)], ["xT"], "xld")
                norm(gm, l, "gm")
                bi = [0]

                def nb_bank():
                    b = bi[0] % 4
                    bi[0] += 1
                    return b

                if not even:
                    wq = wview(od_w_qkv[i])
                    for piece in range(3):
                        W, wk = ring_load(wq[:, :, piece * D:(piece + 1) * D], [128, 8, D], None)
                        if piece < 2:
                            tgt = QT_d if piece == 0 else KT_d
                            tk = "QT" if piece == 0 else "KT"
                            for nb in range(8):
                                b = nb_bank()
                                for k in range(8):
                                    mm(banks[b][:], W[:, k, nb * 128:(nb + 1) * 128], hT[:, k, :], k == 0, k == 7,
                                       [wk, ("hT", k)], [bk[b]])
                                stage_out(banks[b][:], tgt[nb * 128:(nb + 1) * 128, cs], bk[b], (tk, j),
                                          scale=(0.125 if piece == 0 else None))
                        else:
                            for tb in range(4):
                                for nh in range(2):
                                    b = nb_bank()
                                    for k in range(8):
                                        mm(banks[b][:], hT[:, k, tb * 128:(tb + 1) * 128], W[:, k, nh * 512:(nh + 1) * 512],
                                           k == 0, k == 7, [wk, ("hT", k)], [bk[b]])
                                    stage_out(banks[b][:], V_d[j * 512 + tb * 128:j * 512 + (tb + 1) * 128, nh * 512:(nh + 1) * 512],
                                              bk[b], ("V", j))
                else:
                    wv = wview(ev_w_in[i])
                    WA, wak = ring_load(wv[:, :, 0:1092], [128, 8, 1092], None)
                    WB, wbk = ring_load(wv[:, :, 1092:1604], [128, 8, 512], None)

                    def qkproc(c0, gcol, gkey, lnscale, lnbias, dram_ap, dkey):
                        b = nb_bank()
                        for k in range(8):
                            mm(banks[b][:], WA[:, k, c0:c0 + 128], hT[:, k, :], k == 0, k == 7, [wak, ("hT", k)], [bk[b]])
                        act(t32c[:], banks[b][:], AF.Copy, [bk[b]], ["t32c"])
                        act(sq[:, 0, :], banks[b][:], AF.Square, [bk[b]], ["sq"])
                        mm(banks[6][:], blockones, sq[:, 0, :], True, True, ["sq", "cbf"], [bk[6]])
                        act(t32a[:], banks[6][:], AF.Ln, [bk[6], "cst"], ["t32a"], bias=lnbias, scale=lnscale)
                        act(t32b[:], t32a[:], AF.Exp, ["t32a"], ["t32b"], scale=-0.5)
                        s = sct[0] % 4
                        sct[0] += 1
                        stt(stg[s][:], t32c[:], gcol, t32b[:], ALU.mult, ALU.mult, ["t32c", "t32b", gkey], [("stg", s)])
                        dma(dram_ap, stg[s][:], [("stg", s)], [dkey], ("stgo", s))

                    for nb in range(4):
                        qkproc(nb * 128, qg[:, i:i + 1], "qg", 1.0, cst[:, 2:3], QT_d[nb * 128:(nb + 1) * 128, cs], ("QT", j))
                    qkproc(512, kg[:, i:i + 1], "kg", 1.0 / 64, cst[:, 0:1], KT_d[0:128, cs], ("KT", j))
                    for tb in range(4):
                        b = nb_bank()
                        for k in range(8):
                            mm(banks[b][:, 0:128], hT[:, k, tb * 128:(tb + 1) * 128], WA[:, k, 640:768], k == 0, k == 7,
                               [wak, ("hT", k)], [bk[b]])
                        for k in range(8):
                            mm(banks[b][:, 128:132], hT[:, k, tb * 128:(tb + 1) * 128], WA[:, k, 1088:1092], k == 0, k == 7,
                               [wak, ("hT", k)], [bk[b]])
                        for g in range(2):
                            act(vstage[:, tb, g * 65:g * 65 + 64], banks[b][:, g * 64:(g + 1) * 64], AF.Copy, [bk[b]], ["vstage"])
                        act(wist[:, tb, :], banks[b][:, 128:132], AF.Copy, [bk[b]], ["wist"], scale=IDX_SCALE)
                    dma(VA_d[cs, :].rearrange("(tb s) c -> s tb c", s=128), vstage[:], ["vstage"], [("VA", j)], "vao")
                    dma(WI_d[cs, :].rearrange("(tb s) c -> s tb c", s=128), wist[:], ["wist"], [("WI", j)], "wio")
                    for nb in range(2):
                        b = nb_bank()
                        for k in range(8):
                            mm(banks[b][:], WA[:, k, 768 + nb * 128:768 + (nb + 1) * 128], hT[:, k, :], k == 0, k == 7,
                               [wak, ("hT", k)], [bk[b]])
                        stage_out(banks[b][:], QI_d[nb * 128:(nb + 1) * 128, cs], bk[b], ("QI", j))
                    b = nb_bank()
                    for k in range(8):
                        mm(banks[b][0:64, :], WA[:, k, 1024:1088], hT[:, k, :], k == 0, k == 7, [wak, ("hT", k)], [bk[b]])
                    stage_out(banks[b][0:64, :], KI_d[0:64, cs], bk[b], ("KI", j), nparts=64)
                    for g in range(4):
                        b = nb_bank()
                        for k in range(8):
                            mm(banks[b][:], WB[:, k, g * 128:(g + 1) * 128], hT[:, k, :], k == 0, k == 7, [wbk, ("hT", k)], [bk[b]])
                        act(uext[:, g, 16:528], banks[b][:], AF.Copy, [bk[b]], ["uext"])
                        a_ap, a_key = uext[:, g, :], "uext"
                        dsh = 1
                        tgl = 0
                        for _ in range(g + 1):
                            o_ap, o_key = (pa, "pa") if tgl == 0 else (pb, "pb")
                            lo_ = 2 * dsh - 1
                            tt(o_ap[:, lo_:528], a_ap[:, lo_:528], a_ap[:, lo_ - dsh:528 - dsh], ALU.add,
                               [a_key], [o_key], eng="pool")
                            a_ap, a_key = o_ap, o_key
                            dsh *= 2
                            tgl ^= 1
                        w = WINS[g]
                        stt(mixb[:], a_ap[:, 16:528], 1.0 / w, uext[:, g, 16:528], ALU.mult, ALU.subtract,
                            [a_key, "uext"], ["mixb"])
                        if j == 0:
                            tt(t16[:], a_ap[:, 16:32], rc16[:, g, :], ALU.mult, [a_key, "cf"], ["t16"])
                            tt(mixb[:, 0:16], t16[:], uext[:, g, 16:32], ALU.subtract, ["t16", "uext", "mixb"], ["mixb"])
                        b2 = nb_bank()
                        mm(banks[b2][:], poolw[:, i, g, :], mixb[:], True, True, ["mixb", "poolw%d" % i], [bk[b2]])
                        stage_out(banks[b2][:], OT_d[(4 + g) * 128:(5 + g) * 128, cs], bk[b2], ("OT", j),
                                  scale=psc[:, i, g:g + 1])
                    S.add("pool", (lambda o_, i_: (lambda e: e.tensor_copy(out=o_, in_=i_)))(uext[:, :, 0:16], uext[:, :, 512:528]),
                          ["uext", "pa", "pb", "mixb"], ["uext"])

            S.barrier()
            apos[0] = 0
            allk = lambda nm, jj: [(nm, c) for c in range(jj + 1)]
            if not even:
                KTs = [carve(T, BF16) for _ in range(2)]
                Vz = [[carve(NKB * 128, BF16, [NKB, 128]) for _ in range(2)] for _ in range(2)]
                QTc = [carve(512, BF16) for _ in range(2)]
                Eb = [carve(1024, F32) for _ in range(2)]
                spb = [carve(1024, BF16) for _ in range(2)]
                Gb = [carve(1024, F32) for _ in range(2)]
                aTb = [carve(1024, BF16) for _ in range(2)]
                Rb = [carve(1024, BF16) for _ in range(2)]
                oTs = [carve(512, BF16) for _ in range(2)]
                for ks in range(2):
                    memset(Vz[ks][0][:, :, 64:128], 0.0, [("Vz", ks, 0)])
                    memset(Vz[ks][1][:, :, 0:64], 0.0, [("Vz", ks, 1)])
                Vv = V_d.rearrange("(kb s) c -> s kb c", s=128)
                AD = [psall[:, 0:1024], psall[:, 1024:2048]]
                ADk = [["ps0", "ps1"], ["ps2", "ps3"]]
                BD = psall[:, 2048:3072]
                BDk = ["ps4", "ps5"]

                def load_pair(hp):
                    ks = hp % 2
                    dma(KTs[ks][:, :], KT_d[hp * 128:(hp + 1) * 128, :], [], [("KTs", ks)], ("ktl", ks))
                    dma(Vz[ks][0][:, :, 0:64], Vv[:, :, hp * 128:hp * 128 + 64], [], [("Vz", ks, 0)], ("vz0", ks))
                    dma(Vz[ks][1][:, :, 64:128], Vv[:, :, hp * 128 + 64:hp * 128 + 128], [], [("Vz", ks, 1)], ("vz1", ks))

                steps = []
                gi = 0
                for hp in range(8):
                    for j in range(NCH):
                        nkb = 4 * j + 4
                        for n_, kb in enumerate(range(4 * j + 3, -1, -1)):
                            steps.append(dict(hp=hp, j=j, kb=kb, first=(n_ == 0), last=(kb == 0), g=gi, n=len(steps)))
                        gi += 1
                load_pair(0)
                NS = len(steps)

                def S1(t):
                    sp_ = steps[t]
                    hp, j, kb, x = sp_["hp"], sp_["j"], sp_["kb"], t % 2
                    ks, qs = hp % 2, sp_["g"] % 2
                    if j == 0 and kb == 1 and hp + 1 < 8:
                        load_pair(hp + 1)
                    if sp_["first"]:
                        dma(QTc[qs][:, :], QT_d[hp * 128:(hp + 1) * 128, j * 512:(j + 1) * 512], [], [("QTc", qs)], ("qtl", qs))
                    diag = kb >= 4 * j
                    ksl = slice(kb * 128, (kb + 1) * 128)
                    for e_ in range(2):
                        ps_ = slice(e_ * 64, (e_ + 1) * 64)
                        o_ = AD[x][:, e_ * 512:(e_ + 1) * 512]
                        mm(o_, KTs[ks][ps_, ksl], QTc[qs][ps_, :], True, not diag, [("KTs", ks), ("QTc", qs)], [ADk[x][e_]])
                        if diag:
                            mm(o_, ident, maskS[:, kb - 4 * j, :], False, True, ["cbf"], [ADk[x][e_]])
                    act(Eb[x][:], AD[x], AF.Exp, ADk[x], [("Eb", x)])
                    act(spb[x][:], Eb[x][:], AF.Ln, [("Eb", x), "cst"], [("spb", x)], bias=cst[:, 1:2], scale=1.0)

                def S4(t):
                    sp_ = steps[t]
                    x = t % 2
                    Rprev = None if sp_["first"] else (t - 1) % 2
                    for e_ in range(2):
                        hs = slice(e_ * 512, (e_ + 1) * 512)
                        mm(BD[:, hs], negtri, spb[x][:, hs], True, Rprev is None, ["cbf", ("spb", x)], [BDk[e_]])
                        if Rprev is not None:
                            mm(BD[:, hs], negones, Rb[Rprev][:, hs], False, True, ["cbf", ("Rb", Rprev)], [BDk[e_]])
                    act(Gb[x][:], BD, AF.Exp, BDk, [("Gb", x)])
                    tt(aTb[x][:], Eb[x][:], Gb[x][:], ALU.mult, [("Eb", x), ("Gb", x)], [("aTb", x)])
                    if not sp_["last"]:
                        if Rprev is None:
                            S.add("pool", (lambda o, i_: (lambda e: e.tensor_copy(out=o, in_=i_)))(Rb[x][:], spb[x][:]),
                                  [("spb", x)], [("Rb", x)])
                        else:
                            tt(Rb[x][:], Rb[Rprev][:], spb[x][:], ALU.add, [("Rb", Rprev), ("spb", x)], [("Rb", x)], eng="pool")

                def S7(t):
                    sp_ = steps[t]
                    hp, j, kb, x = sp_["hp"], sp_["j"], sp_["kb"], t % 2
                    ks, qs = hp % 2, sp_["g"] % 2
                    bo = 6 + qs
                    for e_ in range(2):
                        hs = slice(e_ * 512, (e_ + 1) * 512)
                        mm(banks[bo][:], Vz[ks][e_][:, kb, :], aTb[x][:, hs], sp_["first"] and e_ == 0, sp_["last"] and e_ == 1,
                           [("Vz", ks, e_), ("aTb", x)], [bk[bo]])
                    if sp_["last"]:
                        act(oTs[qs][:], banks[bo][:], AF.Copy, [bk[bo]], [("oTs", qs)])
                        dma(OT_d[hp * 128:(hp + 1) * 128, j * 512:(j + 1) * 512], oTs[qs][:], [("oTs", qs)], [("OT", j)], ("oto", qs))

                for t in range(NS + 2):
                    if t < NS:
                        S1(t)
                    if 0 <= t - 1 < NS:
                        S4(t - 1)
                    if 0 <= t - 2 < NS:
                        S7(t - 2)
            else:
                KTs = carve(T, BF16)
                KI2 = carve(T, BF16)
                VAs = carve(NKB * 130, BF16, [NKB, 130])
                sc = carve(T, F32)
                junk = carve(T, BF16)
                mb = [carve(T, BF16) for _ in range(4)]
                QTc = carve(4 * 512, BF16, [4, 512])
                QIc = carve(2 * 512, BF16, [2, 512])
                wic = carve(16, F32, [4, 4])
                rtmp = [carve(512, F32) for _ in range(2)]
                PT = [carve(512, BF16) for _ in range(2)]
                otok = carve(4 * 512, BF16, [4, 512])
                oTc = carve(4 * 512, BF16, [4, 512])
                sm = carve(8 + NIT, F32)
                rd = carve(4, F32)
                dma(KTs[:, :], KT_d[0:128, :], allk("KT", NCH - 1), ["KTs"], "ktl")
                dma(KI2[0:64, :], KI_d[:, :], allk("KI", NCH - 1), ["KI2"], "kil")
                dma(KI2[64:128, :], KI_d[:, :], allk("KI", NCH - 1), ["KI2"], "kil")
                dma(VAs[:, :, :], VA_d.rearrange("(kb s) c -> s kb c", s=128), allk("VA", NCH - 1), ["VAs"], "val")
                ti = 0
                for j in range(NCH):
                    cs = slice(j * 512, (j + 1) * 512)
                    L = (j + 1) * 512
                    dma(QTc[0:64, :, :], QT_d[0:256, cs].rearrange("(h d) t -> d h t", d=64), [("QT", j)], ["QTc"], "qtl")
                    dma(QTc[64:128, :, :], QT_d[256:512, cs].rearrange("(h d) t -> d h t", d=64), [("QT", j)], ["QTc"], "qtl")
                    dma(QIc[:, :, :], QI_d[:, cs].rearrange("(b p) t -> p b t", p=128), [("QI", j)], ["QIc"], "qil")
                    dma(wic[:, :, :], WI_d[cs, :].rearrange("(tb s) c -> s tb c", s=128), [("WI", j)], ["wic"], "wil")
                    for qb in range(4):
                        for sg in range(j + 1):
                            ssl = slice(sg * 512, (sg + 1) * 512)
                            for hi in range(4):
                                x = ti % 2
                                ti += 1
                                hp_ = slice((hi % 2) * 64, (hi % 2) * 64 + 64)
                                mm(banks[x][:], QIc[hp_, hi // 2, qb * 128:(qb + 1) * 128], KI2[hp_, ssl], True, True,
                                   ["QIc", "KI2"], [bk[x]])
                                act(rtmp[x][:], banks[x][:], AF.Relu, [bk[x]], [("rtmp", x)])
                                if hi == 0:
                                    ts(sc[:, ssl], rtmp[x][:], wic[:, qb, 0:1], None, ALU.mult, ALU.bypass,
                                       [("rtmp", x), "wic"], ["sc"])
                                else:
                                    stt(sc[:, ssl], rtmp[x][:], wic[:, qb, hi:hi + 1], sc[:, ssl], ALU.mult, ALU.add,
                                        [("rtmp", x), "wic", "sc"], ["sc"])
                        S.add("dve", (lambda o_, i_: (lambda e: e.tensor_reduce(out=o_, in_=i_, axis=AX.X, op=ALU.max)))(sm[:, 0:1], sc[:, 0:L]),
                              ["sc"], ["sm"])
                        S.add("dve", (lambda o_, i_: (lambda e: e.tensor_reduce(out=o_, in_=i_, axis=AX.X, op=ALU.min)))(sm[:, 1:2], sc[:, 0:L]),
                              ["sc"], ["sm"])
                        tt(sc[:, j * 512:(j + 1) * 512], sc[:, j * 512:(j + 1) * 512], maskQ[:, qb, :], ALU.add,
                           ["sc", "cf"], ["sc"])
                        if j == 0 and qb < 2:
                            thr = sm[:, 1:2]
                        else:
                            tt(sm[:, 2:3], sm[:, 0:1], sm[:, 1:2], ALU.subtract, ["sm"], ["sm"])
                            ts(sm[:, 8:8 + NIT], pow2, sm[:, 2:3], None, ALU.mult, ALU.bypass, ["sm", "cf"], ["sm"])
                            S.add("dve", (lambda o_, i_: (lambda e: e.tensor_copy(out=o_, in_=i_)))(sm[:, 3:4], sm[:, 1:2]), ["sm"], ["sm"])
                            for it in range(NIT):
                                tt(sm[:, 4:5], sm[:, 3:4], sm[:, 8 + it:9 + it], ALU.add, ["sm"], ["sm"])
                                ts(junk[:, 0:L], sc[:, 0:L], sm[:, 4:5], None, ALU.is_ge, ALU.add, ["sm", "sc"],
                                   ["junk", "sm"], accum=sm[:, 5:6])
                                ts(sm[:, 6:7], sm[:, 5:6], 256.0, sm[:, 8 + it:9 + it], ALU.is_ge, ALU.mult, ["sm"], ["sm"])
                                tt(sm[:, 3:4], sm[:, 3:4], sm[:, 6:7], ALU.add, ["sm"], ["sm"])
                            thr = sm[:, 3:4]
                        ts(mb[qb][:, 0:L], sc[:, 0:L], thr, NEG, ALU.is_lt, ALU.mult, ["sm", "sc"], [("mb", qb)])
                    tiles = [(h, kb) for h in range(8) for kb in range(4 * j + 4)]

                    def E1(n_):
                        h, kb = tiles[n_]
                        g = h // 4
                        gp = slice(g * 64, (g + 1) * 64)
                        x = n_ % 2
                        ksl = slice(kb * 128, (kb + 1) * 128)
                        mm(banks[2 + x][:], KTs[gp, ksl], QTc[gp, h % 4, :], True, False, ["KTs", "QTc"], [bk[2 + x]])
                        for qb in range(4):
                            mm(banks[2 + x][:, qb * 128:(qb + 1) * 128], mb[qb][:, ksl], ident, False, qb == 3,
                               [("mb", qb), "cbf"], [bk[2 + x]])
                        act(PT[x][:], banks[2 + x][:], AF.Exp, [bk[2 + x]], [("PT", x)])

                    def E3(n_):
                        h, kb = tiles[n_]
                        g = h // 4
                        x = n_ % 2
                        bo = 4 + (h % 2)
                        for qb in range(4):
                            if kb <= 4 * j + qb:
                                mm(banks[bo][:, qb * 65:(qb + 1) * 65], PT[x][:, qb * 128:(qb + 1) * 128],
                                   VAs[:, kb, g * 65:(g + 1) * 65], (kb == 0 and qb == 0), kb == 4 * j + qb,
                                   [("PT", x), "VAs"], [bk[bo]])
                        if kb == 4 * j + 3:
                            bov = banks[bo][:, 0:260].rearrange("p (a b) -> p a b", a=4)
                            S.add("dve", (lambda o_, bv: (lambda e: e.reciprocal(out=o_, in_=bv)))(rd[:, :], bov[:, :, 64]), [bk[bo]], ["rd"])
                            for qb in range(4):
                                ts(otok[:, qb, h * 64:(h + 1) * 64], banks[bo][:, qb * 65:qb * 65 + 64], rd[:, qb:qb + 1], None,
                                   ALU.mult, ALU.bypass, [bk[bo], "rd"], ["otok"])

                    for n_ in range(len(tiles) + 1):
                        if n_ < len(tiles):
                            E1(n_)
                        if n_ >= 1:
                            E3(n_ - 1)
                    for cb in range(4):
                        x = ti % 2
                        ti += 1
                        for qb in range(4):
                            mm(banks[x][:, qb * 128:(qb + 1) * 128], otok[:, qb, cb * 128:(cb + 1) * 128], ident, True, True,
                               ["otok", "cbf"], [bk[x]])
                        act(oTc[:, cb, :], banks[x][:], AF.Copy, [bk[x]], ["oTc"])
                    dma(OT_d[0:512, cs].rearrange("(c p) t -> p c t", p=128), oTc[:, :, :], ["oTc"], [("OT", j)], "oto")

            S.barrier()
            apos[0] = 0
            ring = [carve(11520, BF16) for _ in range(3)]
            oTl = carve(8 * 512, BF16, [8, 512])
            gT = carve(NFB * 512, BF16, [NFB, 512])
            rct = [0]
            w_out = wview(od_w_out[i] if not even else ev_w_out[i])
            w1v = wview(w1[l])
            w3v = wview(w3[l])
            w2v = w2[l].rearrange("(f p) n -> p f n", p=128)
            for j in range(NCH):
                cs = slice(j * 512, (j + 1) * 512)
                dma(xTt[:], src[:, :, cs], [(srck, j)], ["xT"], "xld")
                dma(oTl[:, :, :], OT_d[:, cs].rearrange("(c p) t -> p c t", p=128), [("OT", j)], ["oTl"], "otl")
                Wo, wok = ring_load(w_out[:, :, :], [128, 8, D], None)
                pbk = [0]

                def nbank():
                    b = pbk[0] % 6
                    pbk[0] += 1
                    return b

                for nb in range(8):
                    b = nbank()
                    for c in range(8):
                        mm(banks[b][:], Wo[:, c, nb * 128:(nb + 1) * 128], oTl[:, c, :], c == 0, c == 7, [wok, "oTl"], [bk[b]])
                    tt(xTt[:, nb, :], xTt[:, nb, :], banks[b][:], ALU.add, ["xT", bk[b]], ["xT"])
                norm(gf, l, "gf")
                for half in range(2):
                    fs = slice(half * 1408, (half + 1) * 1408)
                    W1p, k1 = ring_load(w1v[:, :, fs], [128, 8, 1408], None)
                    W3p, k3 = ring_load(w3v[:, :, fs], [128, 8, 1408], None)
                    for fb in range(11):
                        f = half * 11 + fb
                        ba = nbank()
                        bb = nbank()
                        for k in range(8):
                            mm(banks[ba][:], W1p[:, k, fb * 128:(fb + 1) * 128], hT[:, k, :], k == 0, k == 7, [k1, ("hT", k)], [bk[ba]])
                        for k in range(8):
                            mm(banks[bb][:], W3p[:, k, fb * 128:(fb + 1) * 128], hT[:, k, :], k == 0, k == 7, [k3, ("hT", k)], [bk[bb]])
                        tsel = (t32a, "t32a") if f % 2 == 0 else (t32c, "t32c")
                        act(tsel[0][:], banks[ba][:], AF.Silu, [bk[ba]], [tsel[1]])
                        tt(gT[:, f, :], tsel[0][:], banks[bb][:], ALU.mult, [tsel[1], bk[bb]], [("gT", f)])
                for nh in range(2):
                    W2p, k2 = ring_load(w2v[:, :, nh * 512:(nh + 1) * 512], [128, NFB, 512], None)
                    for nbl in range(4):
                        nb = nh * 4 + nbl
                        b = nbank()
                        for f in range(NFB):
                            mm(banks[b][:], W2p[:, f, nbl * 128:(nbl + 1) * 128], gT[:, f, :], f == 0, f == NFB - 1,
                               [k2, ("gT", f)], [bk[b]])
                        tt(xTt[:, nb, :], xTt[:, nb, :], banks[b][:], ALU.add, ["xT", bk[b]], ["xT"])
                dma(dst[:, :, cs], xTt[:], ["xT"], [(dstk, j)], "xst")

        S.emit(final_dma=["xst"])
    return nc


def host_consts():
    s = np.arange(128)[:, None]
    t = np.arange(512)[None, :]
    maskS = np.zeros((128, 4, 512), np.float32)
    maskQ = np.zeros((128, 4, 512), np.float32)
    for a in range(4):
        maskS[:, a, :] = np.where(a * 128 + s < t, 0.0, NEG)
        maskQ[:, a, :] = np.where(t <= a * 128 + s, 0.0, -3.0e38)
    jj = np.arange(128)[:, None]
    ss = np.arange(128)[None, :]
    negtri = np.where(jj >= ss, -1.0, 0.0).astype(np.float32)
    negones = -np.ones((128, 128), np.float32)
    ident = np.eye(128, dtype=np.float32)
    blockones = (jj // 64 == ss // 64).astype(np.float32)
    ones = np.ones((128, 128), np.float32)
    cbf = np.concatenate([maskS.reshape(128, 2048), negtri, negones, ident, blockones, ones], axis=1)
    rc16 = np.zeros((128, 4, 16), np.float32)
    for g, w in enumerate(WINS):
        rc16[:, g, :] = 1.0 / np.minimum(np.arange(16) + 1, w)
    pow2 = np.tile((0.5 ** (np.arange(NIT) + 1)).astype(np.float32)[None, :], (128, 1))
    cf = np.concatenate([maskQ.reshape(128, 2048), rc16.reshape(128, 64), pow2], axis=1)
    return np.ascontiguousarray(cbf, np.float32), np.ascontiguousarray(cf, np.float32)


def make_in_maps(inputs, T, nseq):
    cbf, cf = host_consts()
    f = lambda a: np.ascontiguousarray(np.asarray(a, dtype=np.float32))
    x = np.asarray(inputs["x"], dtype=np.float32)

    def gl(g):
        return np.ascontiguousarray(np.asarray(g, np.float32).reshape(4, 8, 128).transpose(2, 0, 1))

    common = {
        "gmix": gl(inputs["norm_mix_g"]), "gffn": gl(inputs["norm_ffn_g"]),
        "ev_w_in": f(inputs["ev_w_in"]), "ev_w_out": f(inputs["ev_w_out"]),
        "od_w_qkv": f(inputs["od_w_qkv"]), "od_w_out": f(inputs["od_w_out"]),
        "ffn_w1": f(inputs["ffn_w1"]), "ffn_w3": f(inputs["ffn_w3"]), "ffn_w2": f(inputs["ffn_w2"]),
        "qg": np.ascontiguousarray(np.tile(np.asarray(inputs["ev_q_norm_g"], np.float32), (1, 2)).T),
        "kg": np.ascontiguousarray(np.tile(np.asarray(inputs["ev_k_norm_g"], np.float32), (1, 2)).T),
        "pool_w": f(inputs["ev_pool_w"]),
        "pscale": np.ascontiguousarray(np.asarray(inputs["ev_pool_scale"], np.float32).reshape(2, 4, 128).transpose(2, 0, 1)),
        "cbf": cbf, "cf": cf,
    }
    maps = []
    for c in range(8):
        b = c % nseq
        xT = np.ascontiguousarray(x[b].T.reshape(8, 128, T).transpose(1, 0, 2))
        m = dict(common)
        m["xT"] = xT
        maps.append(m)
    return maps


_NC_CACHE = {}


def run(inputs, T, layers, nseq):
    key = (T, layers)
    if key not in _NC_CACHE:
        _NC_CACHE[key] = build(T, layers)
    nc = _NC_CACHE[key]
    maps = make_in_maps(inputs, T, nseq)
    res = run_bass_kernel_spmd(nc, maps, core_ids=list(range(8)))
    outs = []
    for b in range(nseq):
        yT = np.asarray(res.results[b]["yT"])
        outs.append(yT.transpose(1, 0, 2).reshape(D, T).T)
    return np.ascontiguousarray(np.stack(outs, 0)).astype(np.float32)


def kernel(**inputs):
    return run(inputs, 4096, 4, 4)
```

```python
import contextlib
import numpy as np
import concourse.bass as bass
import concourse.mybir as mybir
from concourse.bass_utils import run_bass_kernel_spmd

F32 = mybir.dt.float32
BF16 = mybir.dt.bfloat16
ALU = mybir.AluOpType
AF = mybir.ActivationFunctionType
AX = mybir.AxisListType

D = 1024
DFF = 2816
NFB = 22
EV_COLS = 1604
NIT = 15
NEG = -30000.0
IDX_SCALE = 256 ** -0.5
WINS = (2, 4, 8, 16)
EPS = 1e-6


class Sched:
    def __init__(self, nc):
        self.nc = nc
        self.ops = []
        self.lastw = {}
        self.readers = {}
        self.pending = {}
        self.last_on = {}
        self.last_dma = {}
        self.epoch = 0

    def add(self, eng, fn, reads=(), writes=(), dma=None):
        idx = len(self.ops)
        hard, soft = set(), set()
        for k in reads:
            w = self.lastw.get(k)
            if w is not None:
                hard.add(w)
        for k in writes:
            w = self.lastw.get(k)
            if w is not None:
                hard.add(w)
            for r in self.readers.get(k, ()):
                soft.add(r)
        pb = self.pending.pop(eng, None)
        if pb:
            hard |= pb
        for k in reads:
            self.readers.setdefault(k, []).append(idx)
        for k in writes:
            self.lastw[k] = idx
            self.readers[k] = []
        self.ops.append(dict(eng=eng, fn=fn, hard=hard, soft=soft, dma=dma, sig=False, cnt=0, ep=self.epoch))
        if dma is None:
            self.last_on[eng] = idx
        else:
            self.last_dma[dma] = idx
        return idx

    def barrier(self):
        b = set(self.last_on.values()) | set(v for k, v in self.last_dma.items() if not (isinstance(k, tuple) and k[0] == "wcast"))
        self.epoch += 1
        for e in ("pe", "act", "dve", "pool", "sp"):
            self.pending[e] = set(b) | self.pending.get(e, set())

    def emit(self, final_dma):
        nc = self.nc
        ops = self.ops
        need = [None] * len(ops)
        for i, op in enumerate(ops):
            deps = set()
            for d in op["hard"]:
                od = ops[d]
                if od["dma"] is None and op["dma"] is None and od["eng"] == op["eng"] == "pe":
                    continue
                deps.add(d)
            for d in op["soft"]:
                od = ops[d]
                if od["dma"] is None and op["dma"] is None and od["eng"] == op["eng"]:
                    continue
                deps.add(d)
            need[i] = deps
            for d in deps:
                ops[d]["sig"] = True
        ecount = {}
        dcount = {}
        for op in ops:
            if op["dma"] is not None:
                dcount[op["dma"]] = dcount.get(op["dma"], 0) + 16
                op["cnt"] = dcount[op["dma"]]
            elif op["sig"]:
                ek = (op["eng"], op["ep"])
                ecount[ek] = ecount.get(ek, 0) + 1
                op["cnt"] = ecount[ek]
        with contextlib.ExitStack() as st:
            esem = {ek: st.enter_context(nc.semaphore("es_%s_%d" % ek)) for ek in sorted(ecount)}
            dsem = {k: st.enter_context(nc.semaphore("ds_%d" % n)) for n, k in enumerate(sorted(dcount, key=str))}
            block = st.enter_context(nc.Block())
            streams = {e: [] for e in ("pe", "act", "dve", "pool", "sp")}
            for i, op in enumerate(ops):
                streams[op["eng"]].append(i)

            def run(e, ename):
                waited = {}
                for i in streams[ename]:
                    op = ops[i]
                    tgt = {}
                    for d in need[i]:
                        od = ops[d]
                        s = dsem[od["dma"]] if od["dma"] is not None else esem[(od["eng"], od["ep"])]
                        key = id(s)
                        if od["cnt"] > tgt.get(key, (None, 0))[1]:
                            tgt[key] = (s, od["cnt"])
                    for key, (s, v) in tgt.items():
                        if waited.get(key, 0) < v:
                            e.wait_ge(s, v)
                            waited[key] = v
                    ins = op["fn"](e)
                    if op["dma"] is not None:
                        ins.then_inc(dsem[op["dma"]], 16)
                    elif op["sig"]:
                        ins.then_inc(esem[(ename, op["ep"])], 1)
                if ename == "sp":
                    for k in final_dma:
                        e.wait_ge(dsem[k], dcount[k])

            block.tensor(lambda e: run(e, "pe"))
            block.scalar(lambda e: run(e, "act"))
            block.vector(lambda e: run(e, "dve"))
            block.gpsimd(lambda e: run(e, "pool"))
            block.sync(lambda e: run(e, "sp"))


def build(T, layers):
    NCH = T // 512
    NKB = T // 128
    nc = bass.Bass("TRN2", target_bir_lowering=False)

    def din(name, shape, dt=F32):
        return nc.dram_tensor(name, list(shape), dt, kind="ExternalInput").ap()

    xT_in = din("xT", [128, 8, T])
    gmix = din("gmix", [128, 4, 8])
    gffn = din("gffn", [128, 4, 8])
    ev_w_in = din("ev_w_in", [2, D, EV_COLS])
    ev_w_out = din("ev_w_out", [2, D, D])
    od_w_qkv = din("od_w_qkv", [2, D, 3 * D])
    od_w_out = din("od_w_out", [2, D, D])
    w1 = din("ffn_w1", [4, D, DFF])
    w3 = din("ffn_w3", [4, D, DFF])
    w2 = din("ffn_w2", [4, DFF, D])
    qg_in = din("qg", [128, 2])
    kg_in = din("kg", [128, 2])
    poolw_in = din("pool_w", [2, 4, 128, 128])
    pscale_in = din("pscale", [128, 2, 4])
    cbf_in = din("cbf", [128, 2048 + 5 * 128])
    cf_in = din("cf", [128, 2048 + 64 + NIT])
    yT = nc.dram_tensor("yT", [128, 8, T], F32, kind="ExternalOutput").ap()

    XT_d = nc.dram_tensor("XT_d", [128, 8, T], F32).ap()
    QT_d = nc.dram_tensor("QT_d", [D, T], BF16).ap()
    KT_d = nc.dram_tensor("KT_d", [D, T], BF16).ap()
    V_d = nc.dram_tensor("V_d", [T, D], BF16).ap()
    VA_d = nc.dram_tensor("VA_d", [T, 130], BF16).ap()
    QI_d = nc.dram_tensor("QI_d", [256, T], BF16).ap()
    KI_d = nc.dram_tensor("KI_d", [64, T], BF16).ap()
    WI_d = nc.dram_tensor("WI_d", [T, 4], F32).ap()
    OT_d = nc.dram_tensor("OT_d", [D, T], BF16).ap()

    def wscr(name, ap):
        return nc.dram_tensor(name, list(ap.shape), BF16).ap()

    ev_w_in_b = wscr("ev_w_in_b", ev_w_in)
    ev_w_out_b = wscr("ev_w_out_b", ev_w_out)
    od_w_qkv_b = wscr("od_w_qkv_b", od_w_qkv)
    od_w_out_b = wscr("od_w_out_b", od_w_out)
    w1_b = wscr("w1_b", w1)
    w3_b = wscr("w3_b", w3)
    w2_b = wscr("w2_b", w2)

    S = Sched(nc)
    with contextlib.ExitStack() as st:
        def sb(name, shape, dt):
            return st.enter_context(nc.sbuf_tensor(name, list(shape), dt))

        xTt = sb("xTt", [128, 8, 512], F32)
        hT = sb("hT", [128, 8, 512], BF16)
        sq = sb("sq", [128, 8, 512], BF16)
        cbf = sb("cbf_s", [128, 2048 + 5 * 128], BF16)
        cf = sb("cf_s", [128, 2048 + 64 + NIT], F32)
        gm = sb("gm", [128, 4, 8], F32)
        gf = sb("gf", [128, 4, 8], F32)
        qg = sb("qg_s", [128, 2], F32)
        kg = sb("kg_s", [128, 2], F32)
        psc = sb("psc", [128, 2, 4], F32)
        poolw = sb("poolw", [128, 2, 4, 128], BF16)
        cst = sb("cst", [128, 4], F32)
        t32a = sb("t32a", [128, 512], F32)
        t32b = sb("t32b", [128, 512], F32)
        t32c = sb("t32c", [128, 512], F32)
        ARENA = 56320
        arena = sb("arena", [128, ARENA], BF16)
        psall = st.enter_context(nc.psum_tensor("psall", [128, 4096], F32))
        banks = [psall[:, i * 512:(i + 1) * 512] for i in range(8)]

        maskS = cbf[:, 0:2048].rearrange("p (a b) -> p a b", a=4)
        negtri = cbf[:, 2048:2176]
        negones = cbf[:, 2176:2304]
        ident = cbf[:, 2304:2432]
        blockones = cbf[:, 2432:2560]
        ones = cbf[:, 2560:2688]
        maskQ = cf[:, 0:2048].rearrange("p (a b) -> p a b", a=4)
        rc16 = cf[:, 2048:2112].rearrange("p (a b) -> p a b", a=4)
        pow2 = cf[:, 2112:2112 + NIT]

        apos = [0]

        def carve(n_el, dt, shape=None):
            nb = n_el * (4 if dt == F32 else 2)
            nb = (nb + 63) // 64 * 64
            o = apos[0]
            assert o + nb <= ARENA * 2, (o, nb)
            apos[0] = o + nb
            v = arena[:, o // 2:(o + nb) // 2]
            if dt == F32:
                v = v.bitcast(F32)
            v = v[:, 0:n_el]
            if shape is not None:
                names = " ".join("a%d" % i for i in range(len(shape)))
                kw = {"a%d" % i: s for i, s in enumerate(shape[:-1])}
                v = v.rearrange("p (%s) -> p %s" % (names, names), **kw)
            return v

        def mm(out, lhsT, rhs, start, stop, reads, writes):
            S.add("pe", lambda e: e.matmul(out, lhsT, rhs, start=start, stop=stop), reads, writes)

        def act(out, in_, func, reads, writes, bias=None, scale=None):
            kw = {}
            if bias is not None:
                kw["bias"] = bias
            if scale is not None:
                kw["scale"] = scale
            S.add("act", lambda e: e.activation(out=out, in_=in_, func=func, **kw), reads, writes)

        def dma(out, in_, reads, writes, slot, eng="sp"):
            S.add(eng, lambda e: e.dma_start(out=out, in_=in_), reads, writes, dma=slot)

        def tt(out, in0, in1, op, reads, writes, eng="dve"):
            S.add(eng, lambda e: e.tensor_tensor(out=out, in0=in0, in1=in1, op=op), reads, writes)

        def ts(out, in0, s1, s2, op0, op1, reads, writes, accum=None, eng="dve"):
            if accum is None:
                S.add(eng, lambda e: e.tensor_scalar(out=out, in0=in0, scalar1=s1, scalar2=s2, op0=op0, op1=op1),
                      reads, writes)
            else:
                S.add(eng, lambda e: e.tensor_scalar(out=out, in0=in0, scalar1=s1, scalar2=s2, op0=op0, op1=op1,
                                                     accum_out=accum), reads, writes)

        def stt(out, in0, scalar, in1, op0, op1, reads, writes, eng="dve"):
            S.add(eng, lambda e: e.scalar_tensor_tensor(out=out, in0=in0, scalar=scalar, in1=in1, op0=op0, op1=op1),
                  reads, writes)

        def memset(ap, v, writes, eng="pool"):
            S.add(eng, lambda e: e.memset(ap, v), (), writes)

        def wview(w_ap):
            return w_ap.rearrange("(k p) n -> p k n", p=128)

        dma(cbf[:], cbf_in[:, :], (), ["cbf"], "c", eng="pool")
        dma(cf[:], cf_in[:, :], (), ["cf"], "c")
        dma(gm[:], gmix[:, :, :], (), ["gm"], "c")
        dma(gf[:], gffn[:, :, :], (), ["gf"], "c")
        dma(qg[:], qg_in[:, :], (), ["qg"], "c")
        dma(kg[:], kg_in[:, :], (), ["kg"], "c")
        dma(psc[:], pscale_in[:, :, :], (), ["psc"], "c")
        for i in range(2):
            dma(poolw[:, i, :, :], poolw_in[i].rearrange("g c d -> c g d"), (), ["poolw%d" % i], "c", eng="pool")
        memset(cst[:, 0:1], EPS, ["cst"])
        memset(cst[:, 1:2], 1.0, ["cst"])
        memset(cst[:, 2:3], 64 * EPS, ["cst"])
        memset(cst[:, 3:4], 0.0, ["cst"])

        bk = ["ps%d" % i for i in range(8)]

        def cast_w(src, dstb, l, grp):
            rows = src.shape[0]
            for r0 in range(0, rows, 128):
                dma(dstb[r0:r0 + 128, :], src[r0:r0 + 128, :], [], [("wb", l, grp)], ("wcast", l, grp), eng="pool")

        for l_ in range(layers):
            i_ = l_ // 2
            if l_ % 2 == 0:
                cast_w(ev_w_in[i_], ev_w_in_b[i_], l_, 0)
            else:
                cast_w(od_w_qkv[i_], od_w_qkv_b[i_], l_, 0)
        for l_ in range(layers):
            i_ = l_ // 2
            cast_w((ev_w_out if l_ % 2 == 0 else od_w_out)[i_], (ev_w_out_b if l_ % 2 == 0 else od_w_out_b)[i_], l_, 1)
            cast_w(w1[l_], w1_b[l_], l_, 1)
            cast_w(w3[l_], w3_b[l_], l_, 1)
            cast_w(w2[l_], w2_b[l_], l_, 1)

        def norm(gcol_t, l, gkey):
            act(sq[:], xTt[:], AF.Square, ["xT"], ["sq"])
            for k in range(8):
                mm(banks[7][:], ones, sq[:, k, :], k == 0, k == 7, ["sq", "cbf"], [bk[7]])
            act(t32a[:], banks[7][:], AF.Ln, [bk[7], "cst"], ["t32a"], bias=cst[:, 0:1], scale=1.0 / D)
            act(t32b[:], t32a[:], AF.Exp, ["t32a"], ["t32b"], scale=-0.5)
            for k in range(8):
                stt(hT[:, k, :], xTt[:, k, :], gcol_t[:, l, k:k + 1], t32b[:], ALU.mult, ALU.mult,
                    ["xT", "t32b", gkey], [("hT", k)])

        hkeys = [("hT", k) for k in range(8)]

        for l in range(layers):
            i = l // 2
            even = (l % 2 == 0)
            src = xT_in if l == 0 else XT_d
            dst = yT if l == layers - 1 else XT_d
            srck = "xin" if l == 0 else "XT"
            dstk = "yT" if l == layers - 1 else "XT"

            S.barrier()
            apos[0] = 0
            ring = [carve(11520, BF16) for _ in range(3)]
            stg = [carve(512, BF16) for _ in range(4)]
            rct = [0]
            sct = [0]

            def ring_load(src_ap, shape3, key):
                r = rct[0] % 3
                rct[0] += 1
                n = shape3[1] * shape3[2]
                v = ring[r][:, 0:n].rearrange("p (a b) -> p a b", a=shape3[1])
                dma(v, src_ap, [key], [("ring", r)], ("ring", r), eng="pool")
                return v, ("ring", r)

            def stage_out(ps_ap, dram_ap, psk, dkey, scale=None, nparts=128):
                s = sct[0] % 4
                sct[0] += 1
                o = stg[s][0:nparts, :]
                if scale is None:
                    act(o, ps_ap, AF.Copy, [psk], [("stg", s)])
                else:
                    act(o, ps_ap, AF.Copy, [psk, "psc"], [("stg", s)], scale=scale)
                dma(dram_ap, o, [("stg", s)], [dkey], ("stgo", s))

            if even:
                uext = carve(4 * 528, F32, [4, 528])
                pa = carve(528, F32)
                pb = carve(528, F32)
                mixb = carve(512, BF16)
                vstage = carve(4 * 130, BF16, [4, 130])
                wist = carve(16, F32, [4, 4])
                t16 = carve(16, F32)
                memset(uext[:, :, 0:16], 0.0, ["uext"])
                memset(vstage[:], 1.0, ["vstage"])

            for j in range(NCH):
                cs = slice(j * 512, (j + 1) * 512)
                dma(xTt[:], src[:, :, cs], [(srck, j)], ["xT"], "xld")
                norm(gm, l, "gm")
                bi = [0]

                def nb_bank():
                    b = bi[0] % 4
                    bi[0] += 1
                    return b

                if not even:
                    wq = wview(od_w_qkv_b[i])
                    for piece in range(3):
                        W, wk = ring_load(wq[:, :, piece * D:(piece + 1) * D], [128, 8, D], ("wb", l, 0))
                        if piece < 2:
                            tgt = QT_d if piece == 0 else KT_d
                            tk = "QT" if piece == 0 else "KT"
                            for nb in range(8):
                                b = nb_bank()
                                for k in range(8):
                                    mm(banks[b][:], W[:, k, nb * 128:(nb + 1) * 128], hT[:, k, :], k == 0, k == 7,
                                       [wk, ("hT", k)], [bk[b]])
                                stage_out(banks[b][:], tgt[nb * 128:(nb + 1) * 128, cs], bk[b], (tk, j),
                                          scale=(0.125 if piece == 0 else None))
                        else:
                            for tb in range(4):
                                for nh in range(2):
                                    b = nb_bank()
                                    for k in range(8):
                                        mm(banks[b][:], hT[:, k, tb * 128:(tb + 1) * 128], W[:, k, nh * 512:(nh + 1) * 512],
                                           k == 0, k == 7, [wk, ("hT", k)], [bk[b]])
                                    stage_out(banks[b][:], V_d[j * 512 + tb * 128:j * 512 + (tb + 1) * 128, nh * 512:(nh + 1) * 512],
                                              bk[b], ("V", j))
                else:
                    wv = wview(ev_w_in_b[i])
                    WA, wak = ring_load(wv[:, :, 0:1092], [128, 8, 1092], ("wb", l, 0))
                    WB, wbk = ring_load(wv[:, :, 1092:1604], [128, 8, 512], ("wb", l, 0))

                    def qkproc(c0, gcol, gkey, lnscale, lnbias, dram_ap, dkey):
                        b = nb_bank()
                        for k in range(8):
                            mm(banks[b][:], WA[:, k, c0:c0 + 128], hT[:, k, :], k == 0, k == 7, [wak, ("hT", k)], [bk[b]])
                        act(t32c[:], banks[b][:], AF.Copy, [bk[b]], ["t32c"])
                        act(sq[:, 0, :], banks[b][:], AF.Square, [bk[b]], ["sq"])
                        mm(banks[6][:], blockones, sq[:, 0, :], True, True, ["sq", "cbf"], [bk[6]])
                        act(t32a[:], banks[6][:], AF.Ln, [bk[6], "cst"], ["t32a"], bias=lnbias, scale=lnscale)
                        act(t32b[:], t32a[:], AF.Exp, ["t32a"], ["t32b"], scale=-0.5)
                        s = sct[0] % 4
                        sct[0] += 1
                        stt(stg[s][:], t32c[:], gcol, t32b[:], ALU.mult, ALU.mult, ["t32c", "t32b", gkey], [("stg", s)])
                        dma(dram_ap, stg[s][:], [("stg", s)], [dkey], ("stgo", s))

                    for nb in range(4):
                        qkproc(nb * 128, qg[:, i:i + 1], "qg", 1.0, cst[:, 2:3], QT_d[nb * 128:(nb + 1) * 128, cs], ("QT", j))
                    qkproc(512, kg[:, i:i + 1], "kg", 1.0 / 64, cst[:, 0:1], KT_d[0:128, cs], ("KT", j))
                    for tb in range(4):
                        b = nb_bank()
                        for k in range(8):
                            mm(banks[b][:, 0:128], hT[:, k, tb * 128:(tb + 1) * 128], WA[:, k, 640:768], k == 0, k == 7,
                               [wak, ("hT", k)], [bk[b]])
                        for k in range(8):
                            mm(banks[b][:, 128:132], hT[:, k, tb * 128:(tb + 1) * 128], WA[:, k, 1088:1092], k == 0, k == 7,
                               [wak, ("hT", k)], [bk[b]])
                        for g in range(2):
                            act(vstage[:, tb, g * 65:g * 65 + 64], banks[b][:, g * 64:(g + 1) * 64], AF.Copy, [bk[b]], ["vstage"])
                        act(wist[:, tb, :], banks[b][:, 128:132], AF.Copy, [bk[b]], ["wist"], scale=IDX_SCALE)
                    dma(VA_d[cs, :].rearrange("(tb s) c -> s tb c", s=128), vstage[:], ["vstage"], [("VA", j)], "vao")
                    dma(WI_d[cs, :].rearrange("(tb s) c -> s tb c", s=128), wist[:], ["wist"], [("WI", j)], "wio")
                    for nb in range(2):
                        b = nb_bank()
                        for k in range(8):
                            mm(banks[b][:], WA[:, k, 768 + nb * 128:768 + (nb + 1) * 128], hT[:, k, :], k == 0, k == 7,
                               [wak, ("hT", k)], [bk[b]])
                        stage_out(banks[b][:], QI_d[nb * 128:(nb + 1) * 128, cs], bk[b], ("QI", j))
                    b = nb_bank()
                    for k in range(8):
                        mm(banks[b][0:64, :], WA[:, k, 1024:1088], hT[:, k, :], k == 0, k == 7, [wak, ("hT", k)], [bk[b]])
                    stage_out(banks[b][0:64, :], KI_d[0:64, cs], bk[b], ("KI", j), nparts=64)
                    for g in range(4):
                        b = nb_bank()
                        for k in range(8):
                            mm(banks[b][:], WB[:, k, g * 128:(g + 1) * 128], hT[:, k, :], k == 0, k == 7, [wbk, ("hT", k)], [bk[b]])
                        act(uext[:, g, 16:528], banks[b][:], AF.Copy, [bk[b]], ["uext"])
                        a_ap, a_key = uext[:, g, :], "uext"
                        dsh = 1
                        tgl = 0
                        for _ in range(g + 1):
                            o_ap, o_key = (pa, "pa") if tgl == 0 else (pb, "pb")
                            lo_ = 2 * dsh - 1
                            tt(o_ap[:, lo_:528], a_ap[:, lo_:528], a_ap[:, lo_ - dsh:528 - dsh], ALU.add,
                               [a_key], [o_key], eng="pool")
                            a_ap, a_key = o_ap, o_key
                            dsh *= 2
                            tgl ^= 1
                        w = WINS[g]
                        stt(mixb[:], a_ap[:, 16:528], 1.0 / w, uext[:, g, 16:528], ALU.mult, ALU.subtract,
                            [a_key, "uext"], ["mixb"])
                        if j == 0:
                            tt(t16[:], a_ap[:, 16:32], rc16[:, g, :], ALU.mult, [a_key, "cf"], ["t16"])
                            tt(mixb[:, 0:16], t16[:], uext[:, g, 16:32], ALU.subtract, ["t16", "uext", "mixb"], ["mixb"])
                        b2 = nb_bank()
                        mm(banks[b2][:], poolw[:, i, g, :], mixb[:], True, True, ["mixb", "poolw%d" % i], [bk[b2]])
                        stage_out(banks[b2][:], OT_d[(4 + g) * 128:(5 + g) * 128, cs], bk[b2], ("OT", j),
                                  scale=psc[:, i, g:g + 1])
                    S.add("pool", (lambda o_, i_: (lambda e: e.tensor_copy(out=o_, in_=i_)))(uext[:, :, 0:16], uext[:, :, 512:528]),
                          ["uext", "pa", "pb", "mixb"], ["uext"])

            S.barrier()
            apos[0] = 0
            allk = lambda nm, jj: [(nm, c) for c in range(jj + 1)]
            if not even:
                KTs = [carve(T, BF16) for _ in range(2)]
                Vz = [[carve(NKB * 128, BF16, [NKB, 128]) for _ in range(2)] for _ in range(2)]
                QTc = [carve(512, BF16) for _ in range(2)]
                Eb = [carve(1024, F32) for _ in range(2)]
                spb = [carve(1024, BF16) for _ in range(2)]
                Gb = [carve(1024, F32) for _ in range(2)]
                aTb = [carve(1024, BF16) for _ in range(2)]
                Rb = [carve(1024, BF16) for _ in range(2)]
                oTs = [carve(512, BF16) for _ in range(2)]
                for ks in range(2):
                    memset(Vz[ks][0][:, :, 64:128], 0.0, [("Vz", ks, 0)])
                    memset(Vz[ks][1][:, :, 0:64], 0.0, [("Vz", ks, 1)])
                Vv = V_d.rearrange("(kb s) c -> s kb c", s=128)
                AD = [psall[:, 0:1024], psall[:, 1024:2048]]
                ADk = [["ps0", "ps1"], ["ps2", "ps3"]]
                BD = psall[:, 2048:3072]
                BDk = ["ps4", "ps5"]

                def load_pair(hp):
                    ks = hp % 2
                    dma(KTs[ks][:, :], KT_d[hp * 128:(hp + 1) * 128, :], [], [("KTs", ks)], ("ktl", ks))
                    dma(Vz[ks][0][:, :, 0:64], Vv[:, :, hp * 128:hp * 128 + 64], [], [("Vz", ks, 0)], ("vz0", ks))
                    dma(Vz[ks][1][:, :, 64:128], Vv[:, :, hp * 128 + 64:hp * 128 + 128], [], [("Vz", ks, 1)], ("vz1", ks))

                steps = []
                gi = 0
                for hp in range(8):
                    for j in range(NCH):
                        nkb = 4 * j + 4
                        for n_, kb in enumerate(range(4 * j + 3, -1, -1)):
                            steps.append(dict(hp=hp, j=j, kb=kb, first=(n_ == 0), last=(kb == 0), g=gi, n=len(steps)))
                        gi += 1
                load_pair(0)
                NS = len(steps)

                def S1(t):
                    sp_ = steps[t]
                    hp, j, kb, x = sp_["hp"], sp_["j"], sp_["kb"], t % 2
                    ks, qs = hp % 2, sp_["g"] % 2
                    if j == 0 and kb == 1 and hp + 1 < 8:
                        load_pair(hp + 1)
                    if sp_["first"]:
                        dma(QTc[qs][:, :], QT_d[hp * 128:(hp + 1) * 128, j * 512:(j + 1) * 512], [], [("QTc", qs)], ("qtl", qs))
                    diag = kb >= 4 * j
                    ksl = slice(kb * 128, (kb + 1) * 128)
                    for e_ in range(2):
                        ps_ = slice(e_ * 64, (e_ + 1) * 64)
                        o_ = AD[x][:, e_ * 512:(e_ + 1) * 512]
                        mm(o_, KTs[ks][ps_, ksl], QTc[qs][ps_, :], True, not diag, [("KTs", ks), ("QTc", qs)], [ADk[x][e_]])
                        if diag:
                            mm(o_, ident, maskS[:, kb - 4 * j, :], False, True, ["cbf"], [ADk[x][e_]])
                    act(Eb[x][:], AD[x], AF.Exp, ADk[x], [("Eb", x)])
                    act(spb[x][:], Eb[x][:], AF.Ln, [("Eb", x), "cst"], [("spb", x)], bias=cst[:, 1:2], scale=1.0)

                def S4(t):
                    sp_ = steps[t]
                    x = t % 2
                    Rprev = None if sp_["first"] else (t - 1) % 2
                    for e_ in range(2):
                        hs = slice(e_ * 512, (e_ + 1) * 512)
                        mm(BD[:, hs], negtri, spb[x][:, hs], True, Rprev is None, ["cbf", ("spb", x)], [BDk[e_]])
                        if Rprev is not None:
                            mm(BD[:, hs], negones, Rb[Rprev][:, hs], False, True, ["cbf", ("Rb", Rprev)], [BDk[e_]])
                    act(Gb[x][:], BD, AF.Exp, BDk, [("Gb", x)])
                    tt(aTb[x][:], Eb[x][:], Gb[x][:], ALU.mult, [("Eb", x), ("Gb", x)], [("aTb", x)])
                    if not sp_["last"]:
                        if Rprev is None:
                            S.add("dve", (lambda o, i_: (lambda e: e.tensor_copy(out=o, in_=i_)))(Rb[x][:], spb[x][:]),
                                  [("spb", x)], [("Rb", x)])
                        else:
                            tt(Rb[x][:], Rb[Rprev][:], spb[x][:], ALU.add, [("Rb", Rprev), ("spb", x)], [("Rb", x)])

                def S7(t):
                    sp_ = steps[t]
                    hp, j, kb, x = sp_["hp"], sp_["j"], sp_["kb"], t % 2
                    ks, qs = hp % 2, sp_["g"] % 2
                    bo = 6 + qs
                    for e_ in range(2):
                        hs = slice(e_ * 512, (e_ + 1) * 512)
                        mm(banks[bo][:], Vz[ks][e_][:, kb, :], aTb[x][:, hs], sp_["first"] and e_ == 0, sp_["last"] and e_ == 1,
                           [("Vz", ks, e_), ("aTb", x)], [bk[bo]])
                    if sp_["last"]:
                        act(oTs[qs][:], banks[bo][:], AF.Copy, [bk[bo]], [("oTs", qs)])
                        dma(OT_d[hp * 128:(hp + 1) * 128, j * 512:(j + 1) * 512], oTs[qs][:], [("oTs", qs)], [("OT", j)], ("oto", qs))

                for t in range(NS + 2):
                    if t < NS:
                        S1(t)
                    if 0 <= t - 1 < NS:
                        S4(t - 1)
                    if 0 <= t - 2 < NS:
                        S7(t - 2)
            else:
                KTs = carve(T, BF16)
                KI2 = carve(T, BF16)
                VAs = carve(NKB * 130, BF16, [NKB, 130])
                sc = carve(T, F32)
                junk = carve(T, BF16)
                mb = [carve(T, BF16) for _ in range(4)]
                QTc = carve(4 * 512, BF16, [4, 512])
                QIc = carve(2 * 512, BF16, [2, 512])
                wic = carve(16, F32, [4, 4])
                rtmp = [carve(512, F32) for _ in range(2)]
                PT = [carve(512, BF16) for _ in range(2)]
                otok = carve(4 * 512, BF16, [4, 512])
                oTc = carve(4 * 512, BF16, [4, 512])
                sm = carve(8 + NIT, F32)
                rd = carve(4, F32)
                dma(KTs[:, :], KT_d[0:128, :], allk("KT", NCH - 1), ["KTs"], "ktl")
                dma(KI2[0:64, :], KI_d[:, :], allk("KI", NCH - 1), ["KI2"], "kil")
                dma(KI2[64:128, :], KI_d[:, :], allk("KI", NCH - 1), ["KI2"], "kil")
                dma(VAs[:, :, :], VA_d.rearrange("(kb s) c -> s kb c", s=128), allk("VA", NCH - 1), ["VAs"], "val")
                ti = 0
                for j in range(NCH):
                    cs = slice(j * 512, (j + 1) * 512)
                    L = (j + 1) * 512
                    dma(QTc[0:64, :, :], QT_d[0:256, cs].rearrange("(h d) t -> d h t", d=64), [("QT", j)], ["QTc"], "qtl")
                    dma(QTc[64:128, :, :], QT_d[256:512, cs].rearrange("(h d) t -> d h t", d=64), [("QT", j)], ["QTc"], "qtl")
                    dma(QIc[:, :, :], QI_d[:, cs].rearrange("(b p) t -> p b t", p=128), [("QI", j)], ["QIc"], "qil")
                    dma(wic[:, :, :], WI_d[cs, :].rearrange("(tb s) c -> s tb c", s=128), [("WI", j)], ["wic"], "wil")
                    for qb in range(4):
                        for sg in range(j + 1):
                            ssl = slice(sg * 512, (sg + 1) * 512)
                            for hi in range(4):
                                x = ti % 2
                                ti += 1
                                hp_ = slice((hi % 2) * 64, (hi % 2) * 64 + 64)
                                mm(banks[x][:], QIc[hp_, hi // 2, qb * 128:(qb + 1) * 128], KI2[hp_, ssl], True, True,
                                   ["QIc", "KI2"], [bk[x]])
                                act(rtmp[x][:], banks[x][:], AF.Relu, [bk[x]], [("rtmp", x)])
                                if hi == 0:
                                    ts(sc[:, ssl], rtmp[x][:], wic[:, qb, 0:1], None, ALU.mult, ALU.bypass,
                                       [("rtmp", x), "wic"], ["sc"])
                                else:
                                    stt(sc[:, ssl], rtmp[x][:], wic[:, qb, hi:hi + 1], sc[:, ssl], ALU.mult, ALU.add,
                                        [("rtmp", x), "wic", "sc"], ["sc"])
                        S.add("dve", (lambda o_, i_: (lambda e: e.tensor_reduce(out=o_, in_=i_, axis=AX.X, op=ALU.max)))(sm[:, 0:1], sc[:, 0:L]),
                              ["sc"], ["sm"])
                        S.add("dve", (lambda o_, i_: (lambda e: e.tensor_reduce(out=o_, in_=i_, axis=AX.X, op=ALU.min)))(sm[:, 1:2], sc[:, 0:L]),
                              ["sc"], ["sm"])
                        tt(sc[:, j * 512:(j + 1) * 512], sc[:, j * 512:(j + 1) * 512], maskQ[:, qb, :], ALU.add,
                           ["sc", "cf"], ["sc"])
                        if j == 0 and qb < 2:
                            thr = sm[:, 1:2]
                        else:
                            tt(sm[:, 2:3], sm[:, 0:1], sm[:, 1:2], ALU.subtract, ["sm"], ["sm"])
                            ts(sm[:, 8:8 + NIT], pow2, sm[:, 2:3], None, ALU.mult, ALU.bypass, ["sm", "cf"], ["sm"])
                            S.add("dve", (lambda o_, i_: (lambda e: e.tensor_copy(out=o_, in_=i_)))(sm[:, 3:4], sm[:, 1:2]), ["sm"], ["sm"])
                            for it in range(NIT):
                                tt(sm[:, 4:5], sm[:, 3:4], sm[:, 8 + it:9 + it], ALU.add, ["sm"], ["sm"])
                                ts(junk[:, 0:L], sc[:, 0:L], sm[:, 4:5], None, ALU.is_ge, ALU.add, ["sm", "sc"],
                                   ["junk", "sm"], accum=sm[:, 5:6])
                                ts(sm[:, 6:7], sm[:, 5:6], 256.0, sm[:, 8 + it:9 + it], ALU.is_ge, ALU.mult, ["sm"], ["sm"])
                                tt(sm[:, 3:4], sm[:, 3:4], sm[:, 6:7], ALU.add, ["sm"], ["sm"])
                            thr = sm[:, 3:4]
                        ts(mb[qb][:, 0:L], sc[:, 0:L], thr, NEG, ALU.is_lt, ALU.mult, ["sm", "sc"], [("mb", qb)])
                    tiles = [(h, kb) for h in range(8) for kb in range(4 * j + 4)]

                    def E1(n_):
                        h, kb = tiles[n_]
                        g = h // 4
                        gp = slice(g * 64, (g + 1) * 64)
                        x = n_ % 2
                        ksl = slice(kb * 128, (kb + 1) * 128)
                        mm(banks[2 + x][:], KTs[gp, ksl], QTc[gp, h % 4, :], True, False, ["KTs", "QTc"], [bk[2 + x]])
                        for qb in range(4):
                            mm(banks[2 + x][:, qb * 128:(qb + 1) * 128], mb[qb][:, ksl], ident, False, qb == 3,
                               [("mb", qb), "cbf"], [bk[2 + x]])
                        act(PT[x][:], banks[2 + x][:], AF.Exp, [bk[2 + x]], [("PT", x)])

                    def E3(n_):
                        h, kb = tiles[n_]
                        g = h // 4
                        x = n_ % 2
                        bo = 4 + (h % 2)
                        for qb in range(4):
                            if kb <= 4 * j + qb:
                                mm(banks[bo][:, qb * 65:(qb + 1) * 65], PT[x][:, qb * 128:(qb + 1) * 128],
                                   VAs[:, kb, g * 65:(g + 1) * 65], (kb == 0 and qb == 0), kb == 4 * j + qb,
                                   [("PT", x), "VAs"], [bk[bo]])
                        if kb == 4 * j + 3:
                            bov = banks[bo][:, 0:260].rearrange("p (a b) -> p a b", a=4)
                            S.add("dve", (lambda o_, bv: (lambda e: e.reciprocal(out=o_, in_=bv)))(rd[:, :], bov[:, :, 64]), [bk[bo]], ["rd"])
                            for qb in range(4):
                                ts(otok[:, qb, h * 64:(h + 1) * 64], banks[bo][:, qb * 65:qb * 65 + 64], rd[:, qb:qb + 1], None,
                                   ALU.mult, ALU.bypass, [bk[bo], "rd"], ["otok"])

                    for n_ in range(len(tiles) + 1):
                        if n_ < len(tiles):
                            E1(n_)
                        if n_ >= 1:
                            E3(n_ - 1)
                    for cb in range(4):
                        x = ti % 2
                        ti += 1
                        for qb in range(4):
                            mm(banks[x][:, qb * 128:(qb + 1) * 128], otok[:, qb, cb * 128:(cb + 1) * 128], ident, True, True,
                               ["otok", "cbf"], [bk[x]])
                        act(oTc[:, cb, :], banks[x][:], AF.Copy, [bk[x]], ["oTc"])
                    dma(OT_d[0:512, cs].rearrange("(c p) t -> p c t", p=128), oTc[:, :, :], ["oTc"], [("OT", j)], "oto")

            S.barrier()
            apos[0] = 0
            ring = [carve(11520, BF16) for _ in range(3)]
            oTl = carve(8 * 512, BF16, [8, 512])
            gT = carve(NFB * 512, BF16, [NFB, 512])
            rct = [0]
            w_out = wview(od_w_out_b[i] if not even else ev_w_out_b[i])
            w1v = wview(w1_b[l])
            w3v = wview(w3_b[l])
            w2v = w2_b[l].rearrange("(f p) n -> p f n", p=128)
            for j in range(NCH):
                cs = slice(j * 512, (j + 1) * 512)
                dma(xTt[:], src[:, :, cs], [(srck, j)], ["xT"], "xld")
                dma(oTl[:, :, :], OT_d[:, cs].rearrange("(c p) t -> p c t", p=128), [("OT", j)], ["oTl"], "otl")
                Wo, wok = ring_load(w_out[:, :, :], [128, 8, D], ("wb", l, 1))
                pbk = [0]

                def nbank():
                    b = pbk[0] % 6
                    pbk[0] += 1
                    return b

                for nb in range(8):
                    b = nbank()
                    for c in range(8):
                        mm(banks[b][:], Wo[:, c, nb * 128:(nb + 1) * 128], oTl[:, c, :], c == 0, c == 7, [wok, "oTl"], [bk[b]])
                    tt(xTt[:, nb, :], xTt[:, nb, :], banks[b][:], ALU.add, ["xT", bk[b]], ["xT"])
                norm(gf, l, "gf")
                for half in range(2):
                    fs = slice(half * 1408, (half + 1) * 1408)
                    W1p, k1 = ring_load(w1v[:, :, fs], [128, 8, 1408], ("wb", l, 1))
                    W3p, k3 = ring_load(w3v[:, :, fs], [128, 8, 1408], ("wb", l, 1))
                    for fb in range(11):
                        f = half * 11 + fb
                        ba = nbank()
                        bb = nbank()
                        for k in range(8):
                            mm(banks[ba][:], W1p[:, k, fb * 128:(fb + 1) * 128], hT[:, k, :], k == 0, k == 7, [k1, ("hT", k)], [bk[ba]])
                        for k in range(8):
                            mm(banks[bb][:], W3p[:, k, fb * 128:(fb + 1) * 128], hT[:, k, :], k == 0, k == 7, [k3, ("hT", k)], [bk[bb]])
                        tsel = (t32a, "t32a") if f % 2 == 0 else (t32c, "t32c")
                        act(tsel[0][:], banks[ba][:], AF.Silu, [bk[ba]], [tsel[1]])
                        tt(gT[:, f, :], tsel[0][:], banks[bb][:], ALU.mult, [tsel[1], bk[bb]], [("gT", f)])
                for nh in range(2):
                    W2p, k2 = ring_load(w2v[:, :, nh * 512:(nh + 1) * 512], [128, NFB, 512], ("wb", l, 1))
                    for nbl in range(4):
                        nb = nh * 4 + nbl
                        b = nbank()
                        for f in range(NFB):
                            mm(banks[b][:], W2p[:, f, nbl * 128:(nbl + 1) * 128], gT[:, f, :], f == 0, f == NFB - 1,
                               [k2, ("gT", f)], [bk[b]])
                        tt(xTt[:, nb, :], xTt[:, nb, :], banks[b][:], ALU.add, ["xT", bk[b]], ["xT"])
                dma(dst[:, :, cs], xTt[:], ["xT"], [(dstk, j)], "xst")

        S.emit(final_dma=["xst"])
    return nc


def host_consts():
    s = np.arange(128)[:, None]
    t = np.arange(512)[None, :]
    maskS = np.zeros((128, 4, 512), np.float32)
    maskQ = np.zeros((128, 4, 512), np.float32)
    for a in range(4):
        maskS[:, a, :] = np.where(a * 128 + s < t, 0.0, NEG)
        maskQ[:, a, :] = np.where(t <= a * 128 + s, 0.0, -3.0e38)
    jj = np.arange(128)[:, None]
    ss = np.arange(128)[None, :]
    negtri = np.where(jj >= ss, -1.0, 0.0).astype(np.float32)
    negones = -np.ones((128, 128), np.float32)
    ident = np.eye(128, dtype=np.float32)
    blockones = (jj // 64 == ss // 64).astype(np.float32)
    ones = np.ones((128, 128), np.float32)
    cbf = np.concatenate([maskS.reshape(128, 2048), negtri, negones, ident, blockones, ones], axis=1)
    rc16 = np.zeros((128, 4, 16), np.float32)
    for g, w in enumerate(WINS):
        rc16[:, g, :] = 1.0 / np.minimum(np.arange(16) + 1, w)
    pow2 = np.tile((0.5 ** (np.arange(NIT) + 1)).astype(np.float32)[None, :], (128, 1))
    cf = np.concatenate([maskQ.reshape(128, 2048), rc16.reshape(128, 64), pow2], axis=1)
    return np.ascontiguousarray(cbf, np.float32), np.ascontiguousarray(cf, np.float32)


def make_in_maps(inputs, T, nseq):
    cbf, cf = host_consts()
    f = lambda a: np.ascontiguousarray(np.asarray(a, dtype=np.float32))
    x = np.asarray(inputs["x"], dtype=np.float32)

    def gl(g):
        return np.ascontiguousarray(np.asarray(g, np.float32).reshape(4, 8, 128).transpose(2, 0, 1))

    common = {
        "gmix": gl(inputs["norm_mix_g"]), "gffn": gl(inputs["norm_ffn_g"]),
        "ev_w_in": f(inputs["ev_w_in"]), "ev_w_out": f(inputs["ev_w_out"]),
        "od_w_qkv": f(inputs["od_w_qkv"]), "od_w_out": f(inputs["od_w_out"]),
        "ffn_w1": f(inputs["ffn_w1"]), "ffn_w3": f(inputs["ffn_w3"]), "ffn_w2": f(inputs["ffn_w2"]),
        "qg": np.ascontiguousarray(np.tile(np.asarray(inputs["ev_q_norm_g"], np.float32), (1, 2)).T),
        "kg": np.ascontiguousarray(np.tile(np.asarray(inputs["ev_k_norm_g"], np.float32), (1, 2)).T),
        "pool_w": f(inputs["ev_pool_w"]),
        "pscale": np.ascontiguousarray(np.asarray(inputs["ev_pool_scale"], np.float32).reshape(2, 4, 128).transpose(2, 0, 1)),
        "cbf": cbf, "cf": cf,
    }
    maps = []
    for c in range(8):
        b = c % nseq
        xT = np.ascontiguousarray(x[b].T.reshape(8, 128, T).transpose(1, 0, 2))
        m = dict(common)
        m["xT"] = xT
        maps.append(m)
    return maps


_NC_CACHE = {}


def run(inputs, T, layers, nseq):
    key = (T, layers)
    if key not in _NC_CACHE:
        _NC_CACHE[key] = build(T, layers)
    nc = _NC_CACHE[key]
    maps = make_in_maps(inputs, T, nseq)
    res = run_bass_kernel_spmd(nc, maps, core_ids=list(range(8)))
    outs = []
    for b in range(nseq):
        yT = np.asarray(res.results[b]["yT"])
        outs.append(yT.transpose(1, 0, 2).reshape(D, T).T)
    return np.ascontiguousarray(np.stack(outs, 0)).astype(np.float32)


def kernel(**inputs):
    return run(inputs, 4096, 4, 4)
```

```python
import contextlib
import numpy as np
import concourse.bass as bass
import concourse.mybir as mybir
from concourse.bass_utils import run_bass_kernel_spmd

F32 = mybir.dt.float32
BF16 = mybir.dt.bfloat16
ALU = mybir.AluOpType
AF = mybir.ActivationFunctionType
AX = mybir.AxisListType

D = 1024
DFF = 2816
NFB = 22
EV_COLS = 1604
NIT = 15
NEG = -30000.0
IDX_SCALE = 256 ** -0.5
WINS = (2, 4, 8, 16)
EPS = 1e-6


class Sched:
    def __init__(self, nc):
        self.nc = nc
        self.ops = []
        self.lastw = {}
        self.readers = {}
        self.pending = {}
        self.last_on = {}
        self.last_dma = {}
        self.epoch = 0

    def add(self, eng, fn, reads=(), writes=(), dma=None):
        idx = len(self.ops)
        hard, soft = set(), set()
        for k in reads:
            w = self.lastw.get(k)
            if w is not None:
                hard.add(w)
        for k in writes:
            w = self.lastw.get(k)
            if w is not None:
                hard.add(w)
            for r in self.readers.get(k, ()):
                soft.add(r)
        pb = self.pending.pop(eng, None)
        if pb:
            hard |= pb
        for k in reads:
            self.readers.setdefault(k, []).append(idx)
        for k in writes:
            self.lastw[k] = idx
            self.readers[k] = []
        self.ops.append(dict(eng=eng, fn=fn, hard=hard, soft=soft, dma=dma, sig=False, cnt=0, ep=self.epoch))
        if dma is None:
            self.last_on[eng] = idx
        else:
            self.last_dma[dma] = idx
        return idx

    def barrier(self):
        b = set(self.last_on.values()) | set(v for k, v in self.last_dma.items() if not (isinstance(k, tuple) and k[0] == "wcast"))
        self.epoch += 1
        for e in ("pe", "act", "dve", "pool", "sp"):
            self.pending[e] = set(b) | self.pending.get(e, set())

    def emit(self, final_dma):
        nc = self.nc
        ops = self.ops
        need = [None] * len(ops)
        for i, op in enumerate(ops):
            deps = set()
            for d in op["hard"]:
                od = ops[d]
                if od["dma"] is None and op["dma"] is None and od["eng"] == op["eng"] == "pe":
                    continue
                deps.add(d)
            for d in op["soft"]:
                od = ops[d]
                if od["dma"] is None and op["dma"] is None and od["eng"] == op["eng"]:
                    continue
                deps.add(d)
            need[i] = deps
            for d in deps:
                ops[d]["sig"] = True
        ecount = {}
        dcount = {}
        for op in ops:
            if op["dma"] is not None:
                dcount[op["dma"]] = dcount.get(op["dma"], 0) + 16
                op["cnt"] = dcount[op["dma"]]
            elif op["sig"]:
                ek = (op["eng"], op["ep"])
                ecount[ek] = ecount.get(ek, 0) + 1
                op["cnt"] = ecount[ek]
        with contextlib.ExitStack() as st:
            esem = {ek: st.enter_context(nc.semaphore("es_%s_%d" % ek)) for ek in sorted(ecount)}
            dsem = {k: st.enter_context(nc.semaphore("ds_%d" % n)) for n, k in enumerate(sorted(dcount, key=str))}
            block = st.enter_context(nc.Block())
            streams = {e: [] for e in ("pe", "act", "dve", "pool", "sp")}
            for i, op in enumerate(ops):
                streams[op["eng"]].append(i)

            def run(e, ename):
                waited = {}
                for i in streams[ename]:
                    op = ops[i]
                    tgt = {}
                    for d in need[i]:
                        od = ops[d]
                        s = dsem[od["dma"]] if od["dma"] is not None else esem[(od["eng"], od["ep"])]
                        key = id(s)
                        if od["cnt"] > tgt.get(key, (None, 0))[1]:
                            tgt[key] = (s, od["cnt"])
                    for key, (s, v) in tgt.items():
                        if waited.get(key, 0) < v:
                            e.wait_ge(s, v)
                            waited[key] = v
                    ins = op["fn"](e)
                    if op["dma"] is not None:
                        ins.then_inc(dsem[op["dma"]], 16)
                    elif op["sig"]:
                        ins.then_inc(esem[(ename, op["ep"])], 1)
                if ename == "sp":
                    for k in final_dma:
                        e.wait_ge(dsem[k], dcount[k])

            block.tensor(lambda e: run(e, "pe"))
            block.scalar(lambda e: run(e, "act"))
            block.vector(lambda e: run(e, "dve"))
            block.gpsimd(lambda e: run(e, "pool"))
            block.sync(lambda e: run(e, "sp"))


def build(T, layers):
    NCH = T // 512
    NKB = T // 128
    nc = bass.Bass("TRN2", target_bir_lowering=False)

    def din(name, shape, dt=F32):
        return nc.dram_tensor(name, list(shape), dt, kind="ExternalInput").ap()

    xT_in = din("xT", [128, 8, T])
    gmix = din("gmix", [128, 4, 8])
    gffn = din("gffn", [128, 4, 8])
    ev_w_in = din("ev_w_in", [2, D, EV_COLS])
    ev_w_out = din("ev_w_out", [2, D, D])
    od_w_qkv = din("od_w_qkv", [2, D, 3 * D])
    od_w_out = din("od_w_out", [2, D, D])
    w1 = din("ffn_w1", [4, D, DFF])
    w3 = din("ffn_w3", [4, D, DFF])
    w2 = din("ffn_w2", [4, DFF, D])
    qg_in = din("qg", [128, 2])
    kg_in = din("kg", [128, 2])
    poolw_in = din("pool_w", [2, 4, 128, 128])
    pscale_in = din("pscale", [128, 2, 4])
    cbf_in = din("cbf", [128, 2048 + 5 * 128])
    cf_in = din("cf", [128, 2048 + 64 + NIT])
    yT = nc.dram_tensor("yT", [128, 8, T], F32, kind="ExternalOutput").ap()

    XT_d = nc.dram_tensor("XT_d", [128, 8, T], F32).ap()
    QT_d = nc.dram_tensor("QT_d", [D, T], BF16).ap()
    KT_d = nc.dram_tensor("KT_d", [D, T], BF16).ap()
    V_d = nc.dram_tensor("V_d", [T, D], BF16).ap()
    VA_d = nc.dram_tensor("VA_d", [T, 130], BF16).ap()
    QI_d = nc.dram_tensor("QI_d", [256, T], BF16).ap()
    KI_d = nc.dram_tensor("KI_d", [64, T], BF16).ap()
    WI_d = nc.dram_tensor("WI_d", [T, 4], F32).ap()
    OT_d = nc.dram_tensor("OT_d", [D, T], BF16).ap()

    def wscr(name, ap):
        return nc.dram_tensor(name, list(ap.shape), BF16).ap()

    ev_w_in_b = wscr("ev_w_in_b", ev_w_in)
    ev_w_out_b = wscr("ev_w_out_b", ev_w_out)
    od_w_qkv_b = wscr("od_w_qkv_b", od_w_qkv)
    od_w_out_b = wscr("od_w_out_b", od_w_out)
    w1_b = wscr("w1_b", w1)
    w3_b = wscr("w3_b", w3)
    w2_b = wscr("w2_b", w2)

    S = Sched(nc)
    with contextlib.ExitStack() as st:
        def sb(name, shape, dt):
            return st.enter_context(nc.sbuf_tensor(name, list(shape), dt))

        xTt = sb("xTt", [128, 8, 512], F32)
        hT = sb("hT", [128, 8, 512], BF16)
        sq = sb("sq", [128, 8, 512], BF16)
        cbf = sb("cbf_s", [128, 2048 + 5 * 128], BF16)
        cf = sb("cf_s", [128, 2048 + 64 + NIT], F32)
        gm = sb("gm", [128, 4, 8], F32)
        gf = sb("gf", [128, 4, 8], F32)
        qg = sb("qg_s", [128, 2], F32)
        kg = sb("kg_s", [128, 2], F32)
        psc = sb("psc", [128, 2, 4], F32)
        poolw = sb("poolw", [128, 2, 4, 128], BF16)
        cst = sb("cst", [128, 4], F32)
        t32a = sb("t32a", [128, 512], F32)
        t32b = sb("t32b", [128, 512], F32)
        t32c = sb("t32c", [128, 512], F32)
        ARENA = 67584
        arena = sb("arena", [128, ARENA], BF16)
        psall = st.enter_context(nc.psum_tensor("psall", [128, 4096], F32))
        banks = [psall[:, i * 512:(i + 1) * 512] for i in range(8)]

        maskS = cbf[:, 0:2048].rearrange("p (a b) -> p a b", a=4)
        negtri = cbf[:, 2048:2176]
        negones = cbf[:, 2176:2304]
        ident = cbf[:, 2304:2432]
        blockones = cbf[:, 2432:2560]
        ones = cbf[:, 2560:2688]
        maskQ = cf[:, 0:2048].rearrange("p (a b) -> p a b", a=4)
        rc16 = cf[:, 2048:2112].rearrange("p (a b) -> p a b", a=4)
        pow2 = cf[:, 2112:2112 + NIT]

        apos = [0]

        def carve(n_el, dt, shape=None):
            nb = n_el * (4 if dt == F32 else 2)
            nb = (nb + 63) // 64 * 64
            o = apos[0]
            assert o + nb <= ARENA * 2, (o, nb)
            apos[0] = o + nb
            v = arena[:, o // 2:(o + nb) // 2]
            if dt == F32:
                v = v.bitcast(F32)
            v = v[:, 0:n_el]
            if shape is not None:
                names = " ".join("a%d" % i for i in range(len(shape)))
                kw = {"a%d" % i: s for i, s in enumerate(shape[:-1])}
                v = v.rearrange("p (%s) -> p %s" % (names, names), **kw)
            return v

        def mm(out, lhsT, rhs, start, stop, reads, writes):
            S.add("pe", lambda e: e.matmul(out, lhsT, rhs, start=start, stop=stop), reads, writes)

        def act(out, in_, func, reads, writes, bias=None, scale=None):
            kw = {}
            if bias is not None:
                kw["bias"] = bias
            if scale is not None:
                kw["scale"] = scale
            S.add("act", lambda e: e.activation(out=out, in_=in_, func=func, **kw), reads, writes)

        def dma(out, in_, reads, writes, slot, eng="sp"):
            S.add(eng, lambda e: e.dma_start(out=out, in_=in_), reads, writes, dma=slot)

        def tt(out, in0, in1, op, reads, writes, eng="dve"):
            S.add(eng, lambda e: e.tensor_tensor(out=out, in0=in0, in1=in1, op=op), reads, writes)

        def ts(out, in0, s1, s2, op0, op1, reads, writes, accum=None, eng="dve"):
            if accum is None:
                S.add(eng, lambda e: e.tensor_scalar(out=out, in0=in0, scalar1=s1, scalar2=s2, op0=op0, op1=op1),
                      reads, writes)
            else:
                S.add(eng, lambda e: e.tensor_scalar(out=out, in0=in0, scalar1=s1, scalar2=s2, op0=op0, op1=op1,
                                                     accum_out=accum), reads, writes)

        def stt(out, in0, scalar, in1, op0, op1, reads, writes, eng="dve"):
            S.add(eng, lambda e: e.scalar_tensor_tensor(out=out, in0=in0, scalar=scalar, in1=in1, op0=op0, op1=op1),
                  reads, writes)

        def memset(ap, v, writes, eng="pool"):
            S.add(eng, lambda e: e.memset(ap, v), (), writes)

        def wview(w_ap):
            return w_ap.rearrange("(k p) n -> p k n", p=128)

        dma(cbf[:], cbf_in[:, :], (), ["cbf"], "c", eng="pool")
        dma(cf[:], cf_in[:, :], (), ["cf"], "c")
        dma(gm[:], gmix[:, :, :], (), ["gm"], "c")
        dma(gf[:], gffn[:, :, :], (), ["gf"], "c")
        dma(qg[:], qg_in[:, :], (), ["qg"], "c")
        dma(kg[:], kg_in[:, :], (), ["kg"], "c")
        dma(psc[:], pscale_in[:, :, :], (), ["psc"], "c")
        for i in range(2):
            dma(poolw[:, i, :, :], poolw_in[i].rearrange("g c d -> c g d"), (), ["poolw%d" % i], "c", eng="pool")
        memset(cst[:, 0:1], EPS, ["cst"])
        memset(cst[:, 1:2], 1.0, ["cst"])
        memset(cst[:, 2:3], 64 * EPS, ["cst"])
        memset(cst[:, 3:4], 0.0, ["cst"])

        bk = ["ps%d" % i for i in range(8)]

        def cast_w(src, dstb, l, grp):
            rows = src.shape[0]
            for r0 in range(0, rows, 128):
                dma(dstb[r0:r0 + 128, :], src[r0:r0 + 128, :], [], [("wb", l, grp)], ("wcast", l, grp), eng="pool")

        def cast_group(l_, grp):
            i_ = l_ // 2
            if grp == 0:
                if l_ % 2 == 0:
                    cast_w(ev_w_in[i_], ev_w_in_b[i_], l_, 0)
                else:
                    cast_w(od_w_qkv[i_], od_w_qkv_b[i_], l_, 0)
            else:
                cast_w((ev_w_out if l_ % 2 == 0 else od_w_out)[i_], (ev_w_out_b if l_ % 2 == 0 else od_w_out_b)[i_], l_, 1)
                cast_w(w1[l_], w1_b[l_], l_, 1)
                cast_w(w3[l_], w3_b[l_], l_, 1)
                cast_w(w2[l_], w2_b[l_], l_, 1)

        cast_group(0, 0)

        def norm(gcol_t, l, gkey):
            act(sq[:], xTt[:], AF.Square, ["xT"], ["sq"])
            for k in range(8):
                mm(banks[7][:], ones, sq[:, k, :], k == 0, k == 7, ["sq", "cbf"], [bk[7]])
            act(t32a[:], banks[7][:], AF.Ln, [bk[7], "cst"], ["t32a"], bias=cst[:, 0:1], scale=1.0 / D)
            act(t32b[:], t32a[:], AF.Exp, ["t32a"], ["t32b"], scale=-0.5)
            for k in range(8):
                stt(hT[:, k, :], xTt[:, k, :], gcol_t[:, l, k:k + 1], t32b[:], ALU.mult, ALU.mult,
                    ["xT", "t32b", gkey], [("hT", k)])

        hkeys = [("hT", k) for k in range(8)]

        for l in range(layers):
            i = l // 2
            even = (l % 2 == 0)
            src = xT_in if l == 0 else XT_d
            dst = yT if l == layers - 1 else XT_d
            srck = "xin" if l == 0 else "XT"
            dstk = "yT" if l == layers - 1 else "XT"

            S.barrier()
            apos[0] = 0
            ring = [carve(11520, BF16) for _ in range(3)]
            stg = [carve(512, BF16) for _ in range(4)]
            rct = [0]
            sct = [0]

            def ring_load(src_ap, shape3, key):
                r = rct[0] % 3
                rct[0] += 1
                n = shape3[1] * shape3[2]
                v = ring[r][:, 0:n].rearrange("p (a b) -> p a b", a=shape3[1])
                dma(v, src_ap, [key], [("ring", r)], ("ring", r), eng="pool")
                return v, ("ring", r)

            def stage_out(ps_ap, dram_ap, psk, dkey, scale=None, nparts=128):
                s = sct[0] % 4
                sct[0] += 1
                o = stg[s][0:nparts, :]
                if scale is None:
                    act(o, ps_ap, AF.Copy, [psk], [("stg", s)])
                else:
                    act(o, ps_ap, AF.Copy, [psk, "psc"], [("stg", s)], scale=scale)
                dma(dram_ap, o, [("stg", s)], [dkey], ("stgo", s))

            if even:
                uext = carve(4 * 528, F32, [4, 528])
                pa = carve(528, F32)
                pb = carve(528, F32)
                mixb = carve(512, BF16)
                vstage = carve(4 * 130, BF16, [4, 130])
                wist = carve(16, F32, [4, 4])
                t16 = carve(16, F32)
                memset(uext[:, :, 0:16], 0.0, ["uext"])
                memset(vstage[:], 1.0, ["vstage"])

            for j in range(NCH):
                cs = slice(j * 512, (j + 1) * 512)
                dma(xTt[:], src[:, :, cs], [(srck, j)], ["xT"], "xld")
                norm(gm, l, "gm")
                bi = [0]

                def nb_bank():
                    b = bi[0] % 4
                    bi[0] += 1
                    return b

                if not even:
                    wq = wview(od_w_qkv_b[i])
                    for piece in range(3):
                        W, wk = ring_load(wq[:, :, piece * D:(piece + 1) * D], [128, 8, D], ("wb", l, 0))
                        if piece < 2:
                            tgt = QT_d if piece == 0 else KT_d
                            tk = "QT" if piece == 0 else "KT"
                            for nb in range(8):
                                b = nb_bank()
                                for k in range(8):
                                    mm(banks[b][:], W[:, k, nb * 128:(nb + 1) * 128], hT[:, k, :], k == 0, k == 7,
                                       [wk, ("hT", k)], [bk[b]])
                                stage_out(banks[b][:], tgt[nb * 128:(nb + 1) * 128, cs], bk[b], (tk, j),
                                          scale=(0.125 if piece == 0 else None))
                        else:
                            for tb in range(4):
                                for nh in range(2):
                                    b = nb_bank()
                                    for k in range(8):
                                        mm(banks[b][:], hT[:, k, tb * 128:(tb + 1) * 128], W[:, k, nh * 512:(nh + 1) * 512],
                                           k == 0, k == 7, [wk, ("hT", k)], [bk[b]])
                                    stage_out(banks[b][:], V_d[j * 512 + tb * 128:j * 512 + (tb + 1) * 128, nh * 512:(nh + 1) * 512],
                                              bk[b], ("V", j))
                else:
                    wv = wview(ev_w_in_b[i])
                    WA, wak = ring_load(wv[:, :, 0:1092], [128, 8, 1092], ("wb", l, 0))
                    WB, wbk = ring_load(wv[:, :, 1092:1604], [128, 8, 512], ("wb", l, 0))

                    def qkproc(c0, gcol, gkey, lnscale, lnbias, dram_ap, dkey):
                        b = nb_bank()
                        for k in range(8):
                            mm(banks[b][:], WA[:, k, c0:c0 + 128], hT[:, k, :], k == 0, k == 7, [wak, ("hT", k)], [bk[b]])
                        act(t32c[:], banks[b][:], AF.Copy, [bk[b]], ["t32c"])
                        act(sq[:, 0, :], banks[b][:], AF.Square, [bk[b]], ["sq"])
                        mm(banks[6][:], blockones, sq[:, 0, :], True, True, ["sq", "cbf"], [bk[6]])
                        act(t32a[:], banks[6][:], AF.Ln, [bk[6], "cst"], ["t32a"], bias=lnbias, scale=lnscale)
                        act(t32b[:], t32a[:], AF.Exp, ["t32a"], ["t32b"], scale=-0.5)
                        s = sct[0] % 4
                        sct[0] += 1
                        stt(stg[s][:], t32c[:], gcol, t32b[:], ALU.mult, ALU.mult, ["t32c", "t32b", gkey], [("stg", s)])
                        dma(dram_ap, stg[s][:], [("stg", s)], [dkey], ("stgo", s))

                    for nb in range(4):
                        qkproc(nb * 128, qg[:, i:i + 1], "qg", 1.0, cst[:, 2:3], QT_d[nb * 128:(nb + 1) * 128, cs], ("QT", j))
                    qkproc(512, kg[:, i:i + 1], "kg", 1.0 / 64, cst[:, 0:1], KT_d[0:128, cs], ("KT", j))
                    for tb in range(4):
                        b = nb_bank()
                        for k in range(8):
                            mm(banks[b][:, 0:128], hT[:, k, tb * 128:(tb + 1) * 128], WA[:, k, 640:768], k == 0, k == 7,
                               [wak, ("hT", k)], [bk[b]])
                        for k in range(8):
                            mm(banks[b][:, 128:132], hT[:, k, tb * 128:(tb + 1) * 128], WA[:, k, 1088:1092], k == 0, k == 7,
                               [wak, ("hT", k)], [bk[b]])
                        for g in range(2):
                            act(vstage[:, tb, g * 65:g * 65 + 64], banks[b][:, g * 64:(g + 1) * 64], AF.Copy, [bk[b]], ["vstage"])
                        act(wist[:, tb, :], banks[b][:, 128:132], AF.Copy, [bk[b]], ["wist"], scale=IDX_SCALE)
                    dma(VA_d[cs, :].rearrange("(tb s) c -> s tb c", s=128), vstage[:], ["vstage"], [("VA", j)], "vao")
                    dma(WI_d[cs, :].rearrange("(tb s) c -> s tb c", s=128), wist[:], ["wist"], [("WI", j)], "wio")
                    for nb in range(2):
                        b = nb_bank()
                        for k in range(8):
                            mm(banks[b][:], WA[:, k, 768 + nb * 128:768 + (nb + 1) * 128], hT[:, k, :], k == 0, k == 7,
                               [wak, ("hT", k)], [bk[b]])
                        stage_out(banks[b][:], QI_d[nb * 128:(nb + 1) * 128, cs], bk[b], ("QI", j))
                    b = nb_bank()
                    for k in range(8):
                        mm(banks[b][0:64, :], WA[:, k, 1024:1088], hT[:, k, :], k == 0, k == 7, [wak, ("hT", k)], [bk[b]])
                    stage_out(banks[b][0:64, :], KI_d[0:64, cs], bk[b], ("KI", j), nparts=64)
                    for g in range(4):
                        b = nb_bank()
                        for k in range(8):
                            mm(banks[b][:], WB[:, k, g * 128:(g + 1) * 128], hT[:, k, :], k == 0, k == 7, [wbk, ("hT", k)], [bk[b]])
                        act(uext[:, g, 16:528], banks[b][:], AF.Copy, [bk[b]], ["uext"])
                        a_ap, a_key = uext[:, g, :], "uext"
                        dsh = 1
                        tgl = 0
                        for _ in range(g + 1):
                            o_ap, o_key = (pa, "pa") if tgl == 0 else (pb, "pb")
                            lo_ = 2 * dsh - 1
                            tt(o_ap[:, lo_:528], a_ap[:, lo_:528], a_ap[:, lo_ - dsh:528 - dsh], ALU.add,
                               [a_key], [o_key], eng="pool")
                            a_ap, a_key = o_ap, o_key
                            dsh *= 2
                            tgl ^= 1
                        w = WINS[g]
                        stt(mixb[:], a_ap[:, 16:528], 1.0 / w, uext[:, g, 16:528], ALU.mult, ALU.subtract,
                            [a_key, "uext"], ["mixb"])
                        if j == 0:
                            tt(t16[:], a_ap[:, 16:32], rc16[:, g, :], ALU.mult, [a_key, "cf"], ["t16"])
                            tt(mixb[:, 0:16], t16[:], uext[:, g, 16:32], ALU.subtract, ["t16", "uext", "mixb"], ["mixb"])
                        b2 = nb_bank()
                        mm(banks[b2][:], poolw[:, i, g, :], mixb[:], True, True, ["mixb", "poolw%d" % i], [bk[b2]])
                        stage_out(banks[b2][:], OT_d[(4 + g) * 128:(5 + g) * 128, cs], bk[b2], ("OT", j),
                                  scale=psc[:, i, g:g + 1])
                    S.add("pool", (lambda o_, i_: (lambda e: e.tensor_copy(out=o_, in_=i_)))(uext[:, :, 0:16], uext[:, :, 512:528]),
                          ["uext", "pa", "pb", "mixb"], ["uext"])

            S.barrier()
            apos[0] = 0
            allk = lambda nm, jj: [(nm, c) for c in range(jj + 1)]
            if not even:
                KTs = [carve(T, BF16) for _ in range(2)]
                Vz = [[carve(NKB * 128, BF16, [NKB, 128]) for _ in range(2)] for _ in range(2)]
                QTc = [carve(512, BF16) for _ in range(2)]
                Eb = [carve(1024, F32) for _ in range(3)]
                spb = [carve(1024, BF16) for _ in range(3)]
                Gb = [carve(1024, F32) for _ in range(2)]
                aTb = [carve(1024, BF16) for _ in range(2)]
                Rb = [carve(1024, BF16) for _ in range(2)]
                oTs = [carve(512, BF16) for _ in range(2)]
                for ks in range(2):
                    memset(Vz[ks][0][:, :, 64:128], 0.0, [("Vz", ks, 0)])
                    memset(Vz[ks][1][:, :, 0:64], 0.0, [("Vz", ks, 1)])
                cast_group(l, 1)
                if l + 1 < layers:
                    cast_group(l + 1, 0)
                Vv = V_d.rearrange("(kb s) c -> s kb c", s=128)
                AD = [psall[:, 0:1024], psall[:, 1024:2048]]
                ADk = [["ps0", "ps1"], ["ps2", "ps3"]]
                BD = psall[:, 2048:3072]
                BDk = ["ps4", "ps5"]

                def load_pair(hp):
                    ks = hp % 2
                    dma(KTs[ks][:, :], KT_d[hp * 128:(hp + 1) * 128, :], [], [("KTs", ks)], ("ktl", ks))
                    dma(Vz[ks][0][:, :, 0:64], Vv[:, :, hp * 128:hp * 128 + 64], [], [("Vz", ks, 0)], ("vz0", ks))
                    dma(Vz[ks][1][:, :, 64:128], Vv[:, :, hp * 128 + 64:hp * 128 + 128], [], [("Vz", ks, 1)], ("vz1", ks))

                steps = []
                gi = 0
                for hp in range(8):
                    for j in range(NCH):
                        nkb = 4 * j + 4
                        for n_, kb in enumerate(range(4 * j + 3, -1, -1)):
                            steps.append(dict(hp=hp, j=j, kb=kb, first=(n_ == 0), last=(kb == 0), g=gi, n=len(steps)))
                        gi += 1
                load_pair(0)
                NS = len(steps)

                def S1(t):
                    sp_ = steps[t]
                    hp, j, kb, x = sp_["hp"], sp_["j"], sp_["kb"], t % 2
                    ks, qs = hp % 2, sp_["g"] % 2
                    if j == 0 and kb == 1 and hp + 1 < 8:
                        load_pair(hp + 1)
                    if sp_["first"]:
                        dma(QTc[qs][:, :], QT_d[hp * 128:(hp + 1) * 128, j * 512:(j + 1) * 512], [], [("QTc", qs)], ("qtl", qs))
                    diag = kb >= 4 * j
                    ksl = slice(kb * 128, (kb + 1) * 128)
                    for e_ in range(2):
                        ps_ = slice(e_ * 64, (e_ + 1) * 64)
                        o_ = AD[x][:, e_ * 512:(e_ + 1) * 512]
                        mm(o_, KTs[ks][ps_, ksl], QTc[qs][ps_, :], True, not diag, [("KTs", ks), ("QTc", qs)], [ADk[x][e_]])
                        if diag:
                            mm(o_, ident, maskS[:, kb - 4 * j, :], False, True, ["cbf"], [ADk[x][e_]])
                    xe = t % 3
                    act(Eb[xe][:], AD[x], AF.Exp, ADk[x], [("Eb", xe)])
                    act(spb[xe][:], Eb[xe][:], AF.Ln, [("Eb", xe), "cst"], [("spb", xe)], bias=cst[:, 1:2], scale=1.0)

                def S4(t):
                    sp_ = steps[t]
                    x = t % 2
                    xe = t % 3
                    Rprev = None if sp_["first"] else (t - 1) % 2
                    for e_ in range(2):
                        hs = slice(e_ * 512, (e_ + 1) * 512)
                        mm(BD[:, hs], negtri, spb[xe][:, hs], True, Rprev is None, ["cbf", ("spb", xe)], [BDk[e_]])
                        if Rprev is not None:
                            mm(BD[:, hs], negones, Rb[Rprev][:, hs], False, True, ["cbf", ("Rb", Rprev)], [BDk[e_]])
                    act(Gb[x][:], BD, AF.Exp, BDk, [("Gb", x)])
                    tt(aTb[x][:], Eb[xe][:], Gb[x][:], ALU.mult, [("Eb", xe), ("Gb", x)], [("aTb", x)])
                    if not sp_["last"]:
                        if Rprev is None:
                            S.add("dve", (lambda o, i_: (lambda e: e.tensor_copy(out=o, in_=i_)))(Rb[x][:], spb[xe][:]),
                                  [("spb", xe)], [("Rb", x)])
                        else:
                            tt(Rb[x][:], Rb[Rprev][:], spb[xe][:], ALU.add, [("Rb", Rprev), ("spb", xe)], [("Rb", x)])

                def S7(t):
                    sp_ = steps[t]
                    hp, j, kb, x = sp_["hp"], sp_["j"], sp_["kb"], t % 2
                    ks, qs = hp % 2, sp_["g"] % 2
                    bo = 6 + qs
                    for e_ in range(2):
                        hs = slice(e_ * 512, (e_ + 1) * 512)
                        mm(banks[bo][:], Vz[ks][e_][:, kb, :], aTb[x][:, hs], sp_["first"] and e_ == 0, sp_["last"] and e_ == 1,
                           [("Vz", ks, e_), ("aTb", x)], [bk[bo]])
                    if sp_["last"]:
                        act(oTs[qs][:], banks[bo][:], AF.Copy, [bk[bo]], [("oTs", qs)])
                        dma(OT_d[hp * 128:(hp + 1) * 128, j * 512:(j + 1) * 512], oTs[qs][:], [("oTs", qs)], [("OT", j)], ("oto", qs))

                for t in range(NS + 2):
                    if t < NS:
                        S1(t)
                    if 0 <= t - 1 < NS:
                        S4(t - 1)
                    if 0 <= t - 2 < NS:
                        S7(t - 2)
            else:
                KTs = carve(T, BF16)
                KI2 = carve(T, BF16)
                VAs = carve(NKB * 130, BF16, [NKB, 130])
                sc = carve(T, F32)
                mbb = [[carve(T, BF16) for _ in range(4)] for _ in range(2)]
                QTcs = [carve(4 * 512, BF16, [4, 512]) for _ in range(2)]
                QIcs = [carve(2 * 512, BF16, [2, 512]) for _ in range(2)]
                wics = [carve(16, F32, [4, 4]) for _ in range(2)]
                rtmp = [carve(512, F32) for _ in range(2)]
                PT = [carve(512, BF16) for _ in range(2)]
                otok = carve(4 * 512, BF16, [4, 512])
                oTc = carve(4 * 512, BF16, [4, 512])
                sm = carve(8 + NIT, F32)
                rd = carve(4, F32)
                dma(KTs[:, :], KT_d[0:128, :], [], ["KTs"], "ktl")
                dma(KI2[0:64, :], KI_d[:, :], [], ["KI2"], "kil")
                dma(KI2[64:128, :], KI_d[:, :], [], ["KI2"], "kil")
                dma(VAs[:, :, :], VA_d.rearrange("(kb s) c -> s kb c", s=128), [], ["VAs"], "val")
                tic = [0]
                cast_group(l, 1)
                if l + 1 < layers:
                    cast_group(l + 1, 0)

                def loads(j):
                    cs = slice(j * 512, (j + 1) * 512)
                    p = j % 2
                    dma(QTcs[p][0:64, :, :], QT_d[0:256, cs].rearrange("(h d) t -> d h t", d=64), [], [("QTc", p)], ("qtl", p))
                    dma(QTcs[p][64:128, :, :], QT_d[256:512, cs].rearrange("(h d) t -> d h t", d=64), [], [("QTc", p)], ("qtl", p))
                    dma(QIcs[p][:, :, :], QI_d[:, cs].rearrange("(b p) t -> p b t", p=128), [], [("QIc", p)], ("qil", p))
                    dma(wics[p][:, :, :], WI_d[cs, :].rearrange("(tb s) c -> s tb c", s=128), [], [("wic", p)], ("wil", p))

                def search(j, qb):
                    p = j % 2
                    L = (j + 1) * 512
                    QIc, wic, mbq = QIcs[p], wics[p], mbb[p][qb]
                    for sg in range(j + 1):
                        ssl = slice(sg * 512, (sg + 1) * 512)
                        for hi in range(4):
                            x = tic[0] % 2
                            tic[0] += 1
                            hp_ = slice((hi % 2) * 64, (hi % 2) * 64 + 64)
                            mm(banks[x][:], QIc[hp_, hi // 2, qb * 128:(qb + 1) * 128], KI2[hp_, ssl], True, True,
                               [("QIc", p), "KI2"], [bk[x]])
                            act(rtmp[x][:], banks[x][:], AF.Relu, [bk[x]], [("rtmp", x)])
                            if hi == 0:
                                ts(sc[:, ssl], rtmp[x][:], wic[:, qb, 0:1], None, ALU.mult, ALU.bypass,
                                   [("rtmp", x), ("wic", p)], ["sc"])
                            else:
                                stt(sc[:, ssl], rtmp[x][:], wic[:, qb, hi:hi + 1], sc[:, ssl], ALU.mult, ALU.add,
                                    [("rtmp", x), ("wic", p), "sc"], ["sc"])
                    S.add("dve", (lambda o_, i_: (lambda e: e.tensor_reduce(out=o_, in_=i_, axis=AX.X, op=ALU.max)))(sm[:, 0:1], sc[:, 0:L]),
                          ["sc"], ["sm"])
                    S.add("dve", (lambda o_, i_: (lambda e: e.tensor_reduce(out=o_, in_=i_, axis=AX.X, op=ALU.min)))(sm[:, 1:2], sc[:, 0:L]),
                          ["sc"], ["sm"])
                    tt(sc[:, j * 512:(j + 1) * 512], sc[:, j * 512:(j + 1) * 512], maskQ[:, qb, :], ALU.add,
                       ["sc", "cf"], ["sc"])
                    if j == 0 and qb < 2:
                        thr = sm[:, 1:2]
                    else:
                        tt(sm[:, 2:3], sm[:, 0:1], sm[:, 1:2], ALU.subtract, ["sm"], ["sm"])
                        ts(sm[:, 8:8 + NIT], pow2, sm[:, 2:3], None, ALU.mult, ALU.bypass, ["sm", "cf"], ["sm"])
                        S.add("dve", (lambda o_, i_: (lambda e: e.tensor_copy(out=o_, in_=i_)))(sm[:, 3:4], sm[:, 1:2]), ["sm"], ["sm"])
                        for it in range(NIT):
                            tt(sm[:, 4:5], sm[:, 3:4], sm[:, 8 + it:9 + it], ALU.add, ["sm"], ["sm"])
                            ts(mbq[:, 0:L], sc[:, 0:L], sm[:, 4:5], None, ALU.is_ge, ALU.add, ["sm", "sc"],
                               [("mb", p, qb), "sm"], accum=sm[:, 5:6])
                            ts(sm[:, 6:7], sm[:, 5:6], 256.0, sm[:, 8 + it:9 + it], ALU.is_ge, ALU.mult, ["sm"], ["sm"])
                            tt(sm[:, 3:4], sm[:, 3:4], sm[:, 6:7], ALU.add, ["sm"], ["sm"])
                        thr = sm[:, 3:4]
                    ts(mbq[:, 0:L], sc[:, 0:L], thr, NEG, ALU.is_lt, ALU.mult, ["sm", "sc"], [("mb", p, qb)])

                def att_part(j, heads):
                    p = j % 2
                    QTc, mbp = QTcs[p], mbb[p]
                    tiles = [(h, kb) for h in heads for kb in range(4 * j + 4)]

                    def E1(n_):
                        h, kb = tiles[n_]
                        g = h // 4
                        gp = slice(g * 64, (g + 1) * 64)
                        x = n_ % 2
                        ksl = slice(kb * 128, (kb + 1) * 128)
                        mm(banks[2 + x][:], KTs[gp, ksl], QTc[gp, h % 4, :], True, False, ["KTs", ("QTc", p)], [bk[2 + x]])
                        for qb in range(4):
                            mm(banks[2 + x][:, qb * 128:(qb + 1) * 128], mbp[qb][:, ksl], ident, False, qb == 3,
                               [("mb", p, qb), "cbf"], [bk[2 + x]])
                        act(PT[x][:], banks[2 + x][:], AF.Exp, [bk[2 + x]], [("PT", x)])

                    def E3(n_):
                        h, kb = tiles[n_]
                        g = h // 4
                        x = n_ % 2
                        bo = 4 + (h % 2)
                        for qb in range(4):
                            if kb <= 4 * j + qb:
                                mm(banks[bo][:, qb * 65:(qb + 1) * 65], PT[x][:, qb * 128:(qb + 1) * 128],
                                   VAs[:, kb, g * 65:(g + 1) * 65], (kb == 0 and qb == 0), kb == 4 * j + qb,
                                   [("PT", x), "VAs"], [bk[bo]])
                        if kb == 4 * j + 3:
                            bov = banks[bo][:, 0:260].rearrange("p (a b) -> p a b", a=4)
                            S.add("dve", (lambda o_, bv: (lambda e: e.reciprocal(out=o_, in_=bv)))(rd[:, :], bov[:, :, 64]), [bk[bo]], ["rd"])
                            for qb in range(4):
                                ts(otok[:, qb, h * 64:(h + 1) * 64], banks[bo][:, qb * 65:qb * 65 + 64], rd[:, qb:qb + 1], None,
                                   ALU.mult, ALU.bypass, [bk[bo], "rd"], ["otok"])

                    for n_ in range(len(tiles) + 1):
                        if n_ < len(tiles):
                            E1(n_)
                        if n_ >= 1:
                            E3(n_ - 1)

                loads(0)
                for qb in range(4):
                    search(0, qb)
                for j in range(NCH):
                    cs = slice(j * 512, (j + 1) * 512)
                    if j + 1 < NCH:
                        loads(j + 1)
                    for qb in range(4):
                        if j + 1 < NCH:
                            search(j + 1, qb)
                        att_part(j, (2 * qb, 2 * qb + 1))
                    for cb in range(4):
                        x = tic[0] % 2
                        tic[0] += 1
                        for qb in range(4):
                            mm(banks[x][:, qb * 128:(qb + 1) * 128], otok[:, qb, cb * 128:(cb + 1) * 128], ident, True, True,
                               ["otok", "cbf"], [bk[x]])
                        act(oTc[:, cb, :], banks[x][:], AF.Copy, [bk[x]], ["oTc"])
                    dma(OT_d[0:512, cs].rearrange("(c p) t -> p c t", p=128), oTc[:, :, :], ["oTc"], [("OT", j)], "oto")

            S.barrier()
            apos[0] = 0
            ring = [carve(11520, BF16) for _ in range(3)]
            oTl = carve(8 * 512, BF16, [8, 512])
            gT = carve(NFB * 512, BF16, [NFB, 512])
            rct = [0]
            w_out = wview(od_w_out_b[i] if not even else ev_w_out_b[i])
            w1v = wview(w1_b[l])
            w3v = wview(w3_b[l])
            w2v = w2_b[l].rearrange("(f p) n -> p f n", p=128)
            for j in range(NCH):
                cs = slice(j * 512, (j + 1) * 512)
                dma(xTt[:], src[:, :, cs], [(srck, j)], ["xT"], "xld")
                dma(oTl[:, :, :], OT_d[:, cs].rearrange("(c p) t -> p c t", p=128), [("OT", j)], ["oTl"], "otl")
                Wo, wok = ring_load(w_out[:, :, :], [128, 8, D], ("wb", l, 1))
                pbk = [0]

                def nbank():
                    b = pbk[0] % 6
                    pbk[0] += 1
                    return b

                for nb in range(8):
                    b = nbank()
                    for c in range(8):
                        mm(banks[b][:], Wo[:, c, nb * 128:(nb + 1) * 128], oTl[:, c, :], c == 0, c == 7, [wok, "oTl"], [bk[b]])
                    tt(xTt[:, nb, :], xTt[:, nb, :], banks[b][:], ALU.add, ["xT", bk[b]], ["xT"])
                norm(gf, l, "gf")
                for half in range(2):
                    fs = slice(half * 1408, (half + 1) * 1408)
                    W1p, k1 = ring_load(w1v[:, :, fs], [128, 8, 1408], ("wb", l, 1))
                    W3p, k3 = ring_load(w3v[:, :, fs], [128, 8, 1408], ("wb", l, 1))
                    for fb in range(11):
                        f = half * 11 + fb
                        ba = nbank()
                        bb = nbank()
                        for k in range(8):
                            mm(banks[ba][:], W1p[:, k, fb * 128:(fb + 1) * 128], hT[:, k, :], k == 0, k == 7, [k1, ("hT", k)], [bk[ba]])
                        for k in range(8):
                            mm(banks[bb][:], W3p[:, k, fb * 128:(fb + 1) * 128], hT[:, k, :], k == 0, k == 7, [k3, ("hT", k)], [bk[bb]])
                        tsel = (t32a, "t32a") if f % 2 == 0 else (t32c, "t32c")
                        act(tsel[0][:], banks[ba][:], AF.Silu, [bk[ba]], [tsel[1]])
                        tt(gT[:, f, :], tsel[0][:], banks[bb][:], ALU.mult, [tsel[1], bk[bb]], [("gT", f)])
                for nh in range(2):
                    W2p, k2 = ring_load(w2v[:, :, nh * 512:(nh + 1) * 512], [128, NFB, 512], ("wb", l, 1))
                    for nbl in range(4):
                        nb = nh * 4 + nbl
                        b = nbank()
                        for f in range(NFB):
                            mm(banks[b][:], W2p[:, f, nbl * 128:(nbl + 1) * 128], gT[:, f, :], f == 0, f == NFB - 1,
                               [k2, ("gT", f)], [bk[b]])
                        tt(xTt[:, nb, :], xTt[:, nb, :], banks[b][:], ALU.add, ["xT", bk[b]], ["xT"])
                dma(dst[:, :, cs], xTt[:], ["xT"], [(dstk, j)], "xst")

        S.emit(final_dma=["xst"])
    return nc


def host_consts():
    s = np.arange(128)[:, None]
    t = np.arange(512)[None, :]
    maskS = np.zeros((128, 4, 512), np.float32)
    maskQ = np.zeros((128, 4, 512), np.float32)
    for a in range(4):
        maskS[:, a, :] = np.where(a * 128 + s < t, 0.0, NEG)
        maskQ[:, a, :] = np.where(t <= a * 128 + s, 0.0, -3.0e38)
    jj = np.arange(128)[:, None]
    ss = np.arange(128)[None, :]
    negtri = np.where(jj >= ss, -1.0, 0.0).astype(np.float32)
    negones = -np.ones((128, 128), np.float32)
    ident = np.eye(128, dtype=np.float32)
    blockones = (jj // 64 == ss // 64).astype(np.float32)
    ones = np.ones((128, 128), np.float32)
    cbf = np.concatenate([maskS.reshape(128, 2048), negtri, negones, ident, blockones, ones], axis=1)
    rc16 = np.zeros((128, 4, 16), np.float32)
    for g, w in enumerate(WINS):
        rc16[:, g, :] = 1.0 / np.minimum(np.arange(16) + 1, w)
    pow2 = np.tile((0.5 ** (np.arange(NIT) + 1)).astype(np.float32)[None, :], (128, 1))
    cf = np.concatenate([maskQ.reshape(128, 2048), rc16.reshape(128, 64), pow2], axis=1)
    return np.ascontiguousarray(cbf, np.float32), np.ascontiguousarray(cf, np.float32)


def make_in_maps(inputs, T, nseq):
    cbf, cf = host_consts()
    f = lambda a: np.ascontiguousarray(np.asarray(a, dtype=np.float32))
    x = np.asarray(inputs["x"], dtype=np.float32)

    def gl(g):
        return np.ascontiguousarray(np.asarray(g, np.float32).reshape(4, 8, 128).transpose(2, 0, 1))

    common = {
        "gmix": gl(inputs["norm_mix_g"]), "gffn": gl(inputs["norm_ffn_g"]),
        "ev_w_in": f(inputs["ev_w_in"]), "ev_w_out": f(inputs["ev_w_out"]),
        "od_w_qkv": f(inputs["od_w_qkv"]), "od_w_out": f(inputs["od_w_out"]),
        "ffn_w1": f(inputs["ffn_w1"]), "ffn_w3": f(inputs["ffn_w3"]), "ffn_w2": f(inputs["ffn_w2"]),
        "qg": np.ascontiguousarray(np.tile(np.asarray(inputs["ev_q_norm_g"], np.float32), (1, 2)).T),
        "kg": np.ascontiguousarray(np.tile(np.asarray(inputs["ev_k_norm_g"], np.float32), (1, 2)).T),
        "pool_w": f(inputs["ev_pool_w"]),
        "pscale": np.ascontiguousarray(np.asarray(inputs["ev_pool_scale"], np.float32).reshape(2, 4, 128).transpose(2, 0, 1)),
        "cbf": cbf, "cf": cf,
    }
    maps = []
    for c in range(8):
        b = c % nseq
        xT = np.ascontiguousarray(x[b].T.reshape(8, 128, T).transpose(1, 0, 2))
        m = dict(common)
        m["xT"] = xT
        maps.append(m)
    return maps


_NC_CACHE = {}


def run(inputs, T, layers, nseq):
    key = (T, layers)
    if key not in _NC_CACHE:
        _NC_CACHE[key] = build(T, layers)
    nc = _NC_CACHE[key]
    maps = make_in_maps(inputs, T, nseq)
    res = run_bass_kernel_spmd(nc, maps, core_ids=list(range(8)))
    outs = []
    for b in range(nseq):
        yT = np.asarray(res.results[b]["yT"])
        outs.append(yT.transpose(1, 0, 2).reshape(D, T).T)
    return np.ascontiguousarray(np.stack(outs, 0)).astype(np.float32)


def kernel(**inputs):
    return run(inputs, 4096, 4, 4)
```

```python
import contextlib
import numpy as np
import concourse.bass as bass
import concourse.mybir as mybir
from concourse.bass_utils import run_bass_kernel_spmd

F32 = mybir.dt.float32
BF16 = mybir.dt.bfloat16
ALU = mybir.AluOpType
AF = mybir.ActivationFunctionType
AX = mybir.AxisListType

D = 1024
DFF = 2816
NFB = 22
EV_COLS = 1604
NIT = 15
NEG = -30000.0
IDX_SCALE = 256 ** -0.5
WINS = (2, 4, 8, 16)
EPS = 1e-6


class Sched:
    def __init__(self, nc):
        self.nc = nc
        self.ops = []
        self.lastw = {}
        self.readers = {}
        self.pending = {}
        self.last_on = {}
        self.last_dma = {}
        self.epoch = 0

    def add(self, eng, fn, reads=(), writes=(), dma=None):
        idx = len(self.ops)
        hard, soft = set(), set()
        for k in reads:
            w = self.lastw.get(k)
            if w is not None:
                hard.add(w)
        for k in writes:
            w = self.lastw.get(k)
            if w is not None:
                hard.add(w)
            for r in self.readers.get(k, ()):
                soft.add(r)
        pb = self.pending.pop(eng, None)
        if pb:
            hard |= pb
        for k in reads:
            self.readers.setdefault(k, []).append(idx)
        for k in writes:
            self.lastw[k] = idx
            self.readers[k] = []
        self.ops.append(dict(eng=eng, fn=fn, hard=hard, soft=soft, dma=dma, sig=False, cnt=0, ep=self.epoch))
        if dma is None:
            self.last_on[eng] = idx
        else:
            self.last_dma[dma] = idx
        return idx

    def barrier(self):
        b = set(self.last_on.values()) | set(v for k, v in self.last_dma.items() if not (isinstance(k, tuple) and k[0] == "wcast"))
        self.epoch += 1
        for e in ("pe", "act", "dve", "pool", "sp"):
            self.pending[e] = set(b) | self.pending.get(e, set())

    def emit(self, final_dma):
        nc = self.nc
        ops = self.ops
        need = [None] * len(ops)
        for i, op in enumerate(ops):
            deps = set()
            for d in op["hard"]:
                od = ops[d]
                if od["dma"] is None and op["dma"] is None and od["eng"] == op["eng"] == "pe":
                    continue
                deps.add(d)
            for d in op["soft"]:
                od = ops[d]
                if od["dma"] is None and op["dma"] is None and od["eng"] == op["eng"]:
                    continue
                deps.add(d)
            need[i] = deps
            for d in deps:
                ops[d]["sig"] = True
        ecount = {}
        dcount = {}
        for op in ops:
            if op["dma"] is not None:
                dcount[op["dma"]] = dcount.get(op["dma"], 0) + 16
                op["cnt"] = dcount[op["dma"]]
            elif op["sig"]:
                ek = (op["eng"], op["ep"])
                ecount[ek] = ecount.get(ek, 0) + 1
                op["cnt"] = ecount[ek]
        with contextlib.ExitStack() as st:
            esem = {ek: st.enter_context(nc.semaphore("es_%s_%d" % ek)) for ek in sorted(ecount)}
            dsem = {k: st.enter_context(nc.semaphore("ds_%d" % n)) for n, k in enumerate(sorted(dcount, key=str))}
            block = st.enter_context(nc.Block())
            streams = {e: [] for e in ("pe", "act", "dve", "pool", "sp")}
            for i, op in enumerate(ops):
                streams[op["eng"]].append(i)

            def run(e, ename):
                waited = {}
                for i in streams[ename]:
                    op = ops[i]
                    tgt = {}
                    for d in need[i]:
                        od = ops[d]
                        s = dsem[od["dma"]] if od["dma"] is not None else esem[(od["eng"], od["ep"])]
                        key = id(s)
                        if od["cnt"] > tgt.get(key, (None, 0))[1]:
                            tgt[key] = (s, od["cnt"])
                    for key, (s, v) in tgt.items():
                        if waited.get(key, 0) < v:
                            e.wait_ge(s, v)
                            waited[key] = v
                    ins = op["fn"](e)
                    if op["dma"] is not None:
                        ins.then_inc(dsem[op["dma"]], 16)
                    elif op["sig"]:
                        ins.then_inc(esem[(ename, op["ep"])], 1)
                if ename == "sp":
                    for k in final_dma:
                        e.wait_ge(dsem[k], dcount[k])

            block.tensor(lambda e: run(e, "pe"))
            block.scalar(lambda e: run(e, "act"))
            block.vector(lambda e: run(e, "dve"))
            block.gpsimd(lambda e: run(e, "pool"))
            block.sync(lambda e: run(e, "sp"))


def build(T, layers):
    NCH = T // 512
    NKB = T // 128
    nc = bass.Bass("TRN2", target_bir_lowering=False)

    def din(name, shape, dt=F32):
        return nc.dram_tensor(name, list(shape), dt, kind="ExternalInput").ap()

    xT_in = din("xT", [128, 8, T])
    gmix = din("gmix", [128, 4, 8])
    gffn = din("gffn", [128, 4, 8])
    ev_w_in = din("ev_w_in", [2, D, EV_COLS])
    ev_w_out = din("ev_w_out", [2, D, D])
    od_w_qkv = din("od_w_qkv", [2, D, 3 * D])
    od_w_out = din("od_w_out", [2, D, D])
    w1 = din("ffn_w1", [4, D, DFF])
    w3 = din("ffn_w3", [4, D, DFF])
    w2 = din("ffn_w2", [4, DFF, D])
    qg_in = din("qg", [128, 2])
    kg_in = din("kg", [128, 2])
    poolw_in = din("pool_w", [2, 4, 128, 128])
    pscale_in = din("pscale", [128, 2, 4])
    cbf_in = din("cbf", [128, 2048 + 5 * 128])
    cf_in = din("cf", [128, 2048 + 64 + NIT])
    yT = nc.dram_tensor("yT", [128, 8, T], F32, kind="ExternalOutput").ap()

    XT_d = nc.dram_tensor("XT_d", [128, 8, T], F32).ap()
    QT_d = nc.dram_tensor("QT_d", [D, T], BF16).ap()
    KT_d = nc.dram_tensor("KT_d", [D, T], BF16).ap()
    V_d = nc.dram_tensor("V_d", [T, D], BF16).ap()
    VA_d = nc.dram_tensor("VA_d", [T, 130], BF16).ap()
    QI_d = nc.dram_tensor("QI_d", [256, T], BF16).ap()
    KI_d = nc.dram_tensor("KI_d", [64, T], BF16).ap()
    WI_d = nc.dram_tensor("WI_d", [T, 4], F32).ap()
    OT_d = nc.dram_tensor("OT_d", [D, T], BF16).ap()

    def wscr(name, ap):
        return nc.dram_tensor(name, list(ap.shape), BF16).ap()

    ev_w_in_b = wscr("ev_w_in_b", ev_w_in)
    ev_w_out_b = wscr("ev_w_out_b", ev_w_out)
    od_w_qkv_b = wscr("od_w_qkv_b", od_w_qkv)
    od_w_out_b = wscr("od_w_out_b", od_w_out)
    w1_b = wscr("w1_b", w1)
    w3_b = wscr("w3_b", w3)
    w2_b = wscr("w2_b", w2)

    S = Sched(nc)
    with contextlib.ExitStack() as st:
        def sb(name, shape, dt):
            return st.enter_context(nc.sbuf_tensor(name, list(shape), dt))

        xTt = sb("xTt", [128, 8, 512], F32)
        hT = sb("hT", [128, 8, 512], BF16)
        sq = sb("sq", [128, 8, 512], BF16)
        cbf = sb("cbf_s", [128, 2048 + 5 * 128], BF16)
        cf = sb("cf_s", [128, 2048 + 64 + NIT], F32)
        gm = sb("gm", [128, 4, 8], F32)
        gf = sb("gf", [128, 4, 8], F32)
        qg = sb("qg_s", [128, 2], F32)
        kg = sb("kg_s", [128, 2], F32)
        psc = sb("psc", [128, 2, 4], F32)
        poolw = sb("poolw", [128, 2, 4, 128], BF16)
        cst = sb("cst", [128, 4], F32)
        t32a = sb("t32a", [128, 512], F32)
        t32b = sb("t32b", [128, 512], F32)
        t32c = sb("t32c", [128, 512], F32)
        ARENA = 67584
        arena = sb("arena", [128, ARENA], BF16)
        psall = st.enter_context(nc.psum_tensor("psall", [128, 4096], F32))
        banks = [psall[:, i * 512:(i + 1) * 512] for i in range(8)]

        maskS = cbf[:, 0:2048].rearrange("p (a b) -> p a b", a=4)
        negtri = cbf[:, 2048:2176]
        negones = cbf[:, 2176:2304]
        ident = cbf[:, 2304:2432]
        blockones = cbf[:, 2432:2560]
        ones = cbf[:, 2560:2688]
        maskQ = cf[:, 0:2048].rearrange("p (a b) -> p a b", a=4)
        rc16 = cf[:, 2048:2112].rearrange("p (a b) -> p a b", a=4)
        pow2 = cf[:, 2112:2112 + NIT]

        apos = [0]

        def carve(n_el, dt, shape=None):
            nb = n_el * (4 if dt == F32 else 2)
            nb = (nb + 63) // 64 * 64
            o = apos[0]
            assert o + nb <= ARENA * 2, (o, nb)
            apos[0] = o + nb
            v = arena[:, o // 2:(o + nb) // 2]
            if dt == F32:
                v = v.bitcast(F32)
            v = v[:, 0:n_el]
            if shape is not None:
                names = " ".join("a%d" % i for i in range(len(shape)))
                kw = {"a%d" % i: s for i, s in enumerate(shape[:-1])}
                v = v.rearrange("p (%s) -> p %s" % (names, names), **kw)
            return v

        def mm(out, lhsT, rhs, start, stop, reads, writes):
            S.add("pe", lambda e: e.matmul(out, lhsT, rhs, start=start, stop=stop), reads, writes)

        def act(out, in_, func, reads, writes, bias=None, scale=None):
            kw = {}
            if bias is not None:
                kw["bias"] = bias
            if scale is not None:
                kw["scale"] = scale
            S.add("act", lambda e: e.activation(out=out, in_=in_, func=func, **kw), reads, writes)

        def dma(out, in_, reads, writes, slot, eng="sp"):
            S.add(eng, lambda e: e.dma_start(out=out, in_=in_), reads, writes, dma=slot)

        def tt(out, in0, in1, op, reads, writes, eng="dve"):
            S.add(eng, lambda e: e.tensor_tensor(out=out, in0=in0, in1=in1, op=op), reads, writes)

        def ts(out, in0, s1, s2, op0, op1, reads, writes, accum=None, eng="dve"):
            if accum is None:
                S.add(eng, lambda e: e.tensor_scalar(out=out, in0=in0, scalar1=s1, scalar2=s2, op0=op0, op1=op1),
                      reads, writes)
            else:
                S.add(eng, lambda e: e.tensor_scalar(out=out, in0=in0, scalar1=s1, scalar2=s2, op0=op0, op1=op1,
                                                     accum_out=accum), reads, writes)

        def stt(out, in0, scalar, in1, op0, op1, reads, writes, eng="dve"):
            S.add(eng, lambda e: e.scalar_tensor_tensor(out=out, in0=in0, scalar=scalar, in1=in1, op0=op0, op1=op1),
                  reads, writes)

        def memset(ap, v, writes, eng="pool"):
            S.add(eng, lambda e: e.memset(ap, v), (), writes)

        def wview(w_ap):
            return w_ap.rearrange("(k p) n -> p k n", p=128)

        dma(cbf[:], cbf_in[:, :], (), ["cbf"], "c", eng="pool")
        dma(cf[:], cf_in[:, :], (), ["cf"], "c")
        dma(gm[:], gmix[:, :, :], (), ["gm"], "c")
        dma(gf[:], gffn[:, :, :], (), ["gf"], "c")
        dma(qg[:], qg_in[:, :], (), ["qg"], "c")
        dma(kg[:], kg_in[:, :], (), ["kg"], "c")
        dma(psc[:], pscale_in[:, :, :], (), ["psc"], "c")
        for i in range(2):
            dma(poolw[:, i, :, :], poolw_in[i].rearrange("g c d -> c g d"), (), ["poolw%d" % i], "c", eng="pool")
        memset(cst[:, 0:1], EPS, ["cst"])
        memset(cst[:, 1:2], 1.0, ["cst"])
        memset(cst[:, 2:3], 64 * EPS, ["cst"])
        memset(cst[:, 3:4], 0.0, ["cst"])

        bk = ["ps%d" % i for i in range(8)]

        def cast_w(src, dstb, l, grp):
            rows = src.shape[0]
            for r0 in range(0, rows, 128):
                dma(dstb[r0:r0 + 128, :], src[r0:r0 + 128, :], [], [("wb", l, grp)], ("wcast", l, grp), eng="pool")

        def cast_group(l_, grp):
            i_ = l_ // 2
            if grp == 0:
                if l_ % 2 == 0:
                    cast_w(ev_w_in[i_], ev_w_in_b[i_], l_, 0)
                else:
                    cast_w(od_w_qkv[i_], od_w_qkv_b[i_], l_, 0)
            else:
                cast_w((ev_w_out if l_ % 2 == 0 else od_w_out)[i_], (ev_w_out_b if l_ % 2 == 0 else od_w_out_b)[i_], l_, 1)
                cast_w(w1[l_], w1_b[l_], l_, 1)
                cast_w(w3[l_], w3_b[l_], l_, 1)
                cast_w(w2[l_], w2_b[l_], l_, 1)

        cast_group(0, 0)

        def norm(gcol_t, l, gkey):
            act(sq[:], xTt[:], AF.Square, ["xT"], ["sq"])
            for k in range(8):
                mm(banks[7][:], ones, sq[:, k, :], k == 0, k == 7, ["sq", "cbf"], [bk[7]])
            act(t32a[:], banks[7][:], AF.Ln, [bk[7], "cst"], ["t32a"], bias=cst[:, 0:1], scale=1.0 / D)
            act(t32b[:], t32a[:], AF.Exp, ["t32a"], ["t32b"], scale=-0.5)
            for k in range(8):
                stt(hT[:, k, :], xTt[:, k, :], gcol_t[:, l, k:k + 1], t32b[:], ALU.mult, ALU.mult,
                    ["xT", "t32b", gkey], [("hT", k)])

        hkeys = [("hT", k) for k in range(8)]

        for l in range(layers):
            i = l // 2
            even = (l % 2 == 0)
            src = xT_in if l == 0 else XT_d
            dst = yT if l == layers - 1 else XT_d
            srck = "xin" if l == 0 else "XT"
            dstk = "yT" if l == layers - 1 else "XT"

            S.barrier()
            apos[0] = 0
            ring = [carve(11520, BF16) for _ in range(3)]
            stg = [carve(512, BF16) for _ in range(4)]
            rct = [0]
            sct = [0]

            def ring_load(src_ap, shape3, key):
                r = rct[0] % 3
                rct[0] += 1
                n = shape3[1] * shape3[2]
                v = ring[r][:, 0:n].rearrange("p (a b) -> p a b", a=shape3[1])
                dma(v, src_ap, [key], [("ring", r)], ("ring", r), eng="pool")
                return v, ("ring", r)

            def stage_out(ps_ap, dram_ap, psk, dkey, scale=None, nparts=128):
                s = sct[0] % 4
                sct[0] += 1
                o = stg[s][0:nparts, :]
                if scale is None:
                    act(o, ps_ap, AF.Copy, [psk], [("stg", s)])
                else:
                    act(o, ps_ap, AF.Copy, [psk, "psc"], [("stg", s)], scale=scale)
                dma(dram_ap, o, [("stg", s)], [dkey], ("stgo", s))

            if even:
                uext = carve(4 * 528, F32, [4, 528])
                pa = carve(528, F32)
                pb = carve(528, F32)
                mixb = carve(512, BF16)
                vstage = carve(4 * 130, BF16, [4, 130])
                wist = carve(16, F32, [4, 4])
                t16 = carve(16, F32)
                memset(uext[:, :, 0:16], 0.0, ["uext"])
                memset(vstage[:], 1.0, ["vstage"])

            for j in range(NCH):
                cs = slice(j * 512, (j + 1) * 512)
                dma(xTt[:], src[:, :, cs], [(srck, j)], ["xT"], "xld")
                norm(gm, l, "gm")
                bi = [0]

                def nb_bank():
                    b = bi[0] % 4
                    bi[0] += 1
                    return b

                if not even:
                    wq = wview(od_w_qkv_b[i])
                    for piece in range(3):
                        W, wk = ring_load(wq[:, :, piece * D:(piece + 1) * D], [128, 8, D], ("wb", l, 0))
                        if piece < 2:
                            tgt = QT_d if piece == 0 else KT_d
                            tk = "QT" if piece == 0 else "KT"
                            for nb in range(8):
                                b = nb_bank()
                                for k in range(8):
                                    mm(banks[b][:], W[:, k, nb * 128:(nb + 1) * 128], hT[:, k, :], k == 0, k == 7,
                                       [wk, ("hT", k)], [bk[b]])
                                stage_out(banks[b][:], tgt[nb * 128:(nb + 1) * 128, cs], bk[b], (tk, j),
                                          scale=(0.125 if piece == 0 else None))
                        else:
                            for tb in range(4):
                                for nh in range(2):
                                    b = nb_bank()
                                    for k in range(8):
                                        mm(banks[b][:], hT[:, k, tb * 128:(tb + 1) * 128], W[:, k, nh * 512:(nh + 1) * 512],
                                           k == 0, k == 7, [wk, ("hT", k)], [bk[b]])
                                    stage_out(banks[b][:], V_d[j * 512 + tb * 128:j * 512 + (tb + 1) * 128, nh * 512:(nh + 1) * 512],
                                              bk[b], ("V", j))
                else:
                    wv = wview(ev_w_in_b[i])
                    WA, wak = ring_load(wv[:, :, 0:1092], [128, 8, 1092], ("wb", l, 0))
                    WB, wbk = ring_load(wv[:, :, 1092:1604], [128, 8, 512], ("wb", l, 0))

                    def qkproc(c0, gcol, gkey, lnscale, lnbias, dram_ap, dkey):
                        b = nb_bank()
                        for k in range(8):
                            mm(banks[b][:], WA[:, k, c0:c0 + 128], hT[:, k, :], k == 0, k == 7, [wak, ("hT", k)], [bk[b]])
                        act(t32c[:], banks[b][:], AF.Copy, [bk[b]], ["t32c"])
                        act(sq[:, 0, :], banks[b][:], AF.Square, [bk[b]], ["sq"])
                        mm(banks[6][:], blockones, sq[:, 0, :], True, True, ["sq", "cbf"], [bk[6]])
                        act(t32a[:], banks[6][:], AF.Ln, [bk[6], "cst"], ["t32a"], bias=lnbias, scale=lnscale)
                        act(t32b[:], t32a[:], AF.Exp, ["t32a"], ["t32b"], scale=-0.5)
                        s = sct[0] % 4
                        sct[0] += 1
                        stt(stg[s][:], t32c[:], gcol, t32b[:], ALU.mult, ALU.mult, ["t32c", "t32b", gkey], [("stg", s)])
                        dma(dram_ap, stg[s][:], [("stg", s)], [dkey], ("stgo", s))

                    for nb in range(4):
                        qkproc(nb * 128, qg[:, i:i + 1], "qg", 1.0, cst[:, 2:3], QT_d[nb * 128:(nb + 1) * 128, cs], ("QT", j))
                    qkproc(512, kg[:, i:i + 1], "kg", 1.0 / 64, cst[:, 0:1], KT_d[0:128, cs], ("KT", j))
                    for tb in range(4):
                        b = nb_bank()
                        for k in range(8):
                            mm(banks[b][:, 0:128], hT[:, k, tb * 128:(tb + 1) * 128], WA[:, k, 640:768], k == 0, k == 7,
                               [wak, ("hT", k)], [bk[b]])
                        for k in range(8):
                            mm(banks[b][:, 128:132], hT[:, k, tb * 128:(tb + 1) * 128], WA[:, k, 1088:1092], k == 0, k == 7,
                               [wak, ("hT", k)], [bk[b]])
                        for g in range(2):
                            act(vstage[:, tb, g * 65:g * 65 + 64], banks[b][:, g * 64:(g + 1) * 64], AF.Copy, [bk[b]], ["vstage"])
                        act(wist[:, tb, :], banks[b][:, 128:132], AF.Copy, [bk[b]], ["wist"], scale=IDX_SCALE)
                    dma(VA_d[cs, :].rearrange("(tb s) c -> s tb c", s=128), vstage[:], ["vstage"], [("VA", j)], "vao")
                    dma(WI_d[cs, :].rearrange("(tb s) c -> s tb c", s=128), wist[:], ["wist"], [("WI", j)], "wio")
                    for nb in range(2):
                        b = nb_bank()
                        for k in range(8):
                            mm(banks[b][:], WA[:, k, 768 + nb * 128:768 + (nb + 1) * 128], hT[:, k, :], k == 0, k == 7,
                               [wak, ("hT", k)], [bk[b]])
                        stage_out(banks[b][:], QI_d[nb * 128:(nb + 1) * 128, cs], bk[b], ("QI", j))
                    b = nb_bank()
                    for k in range(8):
                        mm(banks[b][0:64, :], WA[:, k, 1024:1088], hT[:, k, :], k == 0, k == 7, [wak, ("hT", k)], [bk[b]])
                    stage_out(banks[b][0:64, :], KI_d[0:64, cs], bk[b], ("KI", j), nparts=64)
                    for g in range(4):
                        b = nb_bank()
                        for k in range(8):
                            mm(banks[b][:], WB[:, k, g * 128:(g + 1) * 128], hT[:, k, :], k == 0, k == 7, [wbk, ("hT", k)], [bk[b]])
                        act(uext[:, g, 16:528], banks[b][:], AF.Copy, [bk[b]], ["uext"])
                        a_ap, a_key = uext[:, g, :], "uext"
                        dsh = 1
                        tgl = 0
                        for _ in range(g + 1):
                            o_ap, o_key = (pa, "pa") if tgl == 0 else (pb, "pb")
                            lo_ = 2 * dsh - 1
                            tt(o_ap[:, lo_:528], a_ap[:, lo_:528], a_ap[:, lo_ - dsh:528 - dsh], ALU.add,
                               [a_key], [o_key], eng="pool")
                            a_ap, a_key = o_ap, o_key
                            dsh *= 2
                            tgl ^= 1
                        w = WINS[g]
                        stt(mixb[:], a_ap[:, 16:528], 1.0 / w, uext[:, g, 16:528], ALU.mult, ALU.subtract,
                            [a_key, "uext"], ["mixb"])
                        if j == 0:
                            tt(t16[:], a_ap[:, 16:32], rc16[:, g, :], ALU.mult, [a_key, "cf"], ["t16"])
                            tt(mixb[:, 0:16], t16[:], uext[:, g, 16:32], ALU.subtract, ["t16", "uext", "mixb"], ["mixb"])
                        b2 = nb_bank()
                        mm(banks[b2][:], poolw[:, i, g, :], mixb[:], True, True, ["mixb", "poolw%d" % i], [bk[b2]])
                        stage_out(banks[b2][:], OT_d[(4 + g) * 128:(5 + g) * 128, cs], bk[b2], ("OT", j),
                                  scale=psc[:, i, g:g + 1])
                    S.add("pool", (lambda o_, i_: (lambda e: e.tensor_copy(out=o_, in_=i_)))(uext[:, :, 0:16], uext[:, :, 512:528]),
                          ["uext", "pa", "pb", "mixb"], ["uext"])

            S.barrier()
            apos[0] = 0
            allk = lambda nm, jj: [(nm, c) for c in range(jj + 1)]
            if not even:
                KTs = [carve(T, BF16) for _ in range(2)]
                Vz = [[carve(NKB * 128, BF16, [NKB, 128]) for _ in range(2)] for _ in range(2)]
                QTc = [carve(512, BF16) for _ in range(2)]
                Eb = [carve(1024, F32) for _ in range(3)]
                spb = [carve(1024, BF16) for _ in range(3)]
                Gb = [carve(1024, F32) for _ in range(2)]
                aTb = [carve(1024, BF16) for _ in range(2)]
                Rb = [carve(1024, BF16) for _ in range(3)]
                oTs = [carve(512, BF16) for _ in range(2)]
                for ks in range(2):
                    memset(Vz[ks][0][:, :, 64:128], 0.0, [("Vz", ks, 0)])
                    memset(Vz[ks][1][:, :, 0:64], 0.0, [("Vz", ks, 1)])
                cast_group(l, 1)
                if l + 1 < layers:
                    cast_group(l + 1, 0)
                Vv = V_d.rearrange("(kb s) c -> s kb c", s=128)
                AD = [psall[:, 0:1024], psall[:, 1024:2048]]
                ADk = [["ps0", "ps1"], ["ps2", "ps3"]]
                BD = psall[:, 2048:3072]
                BDk = ["ps4", "ps5"]

                def load_pair(hp):
                    ks = hp % 2
                    dma(KTs[ks][:, :], KT_d[hp * 128:(hp + 1) * 128, :], [], [("KTs", ks)], ("ktl", ks))
                    dma(Vz[ks][0][:, :, 0:64], Vv[:, :, hp * 128:hp * 128 + 64], [], [("Vz", ks, 0)], ("vz0", ks))
                    dma(Vz[ks][1][:, :, 64:128], Vv[:, :, hp * 128 + 64:hp * 128 + 128], [], [("Vz", ks, 1)], ("vz1", ks))

                steps = []
                gi = 0
                for hp in range(8):
                    for j in range(NCH):
                        nkb = 4 * j + 4
                        for n_, kb in enumerate(range(4 * j + 3, -1, -1)):
                            steps.append(dict(hp=hp, j=j, kb=kb, first=(n_ == 0), last=(kb == 0), g=gi, n=len(steps)))
                        gi += 1
                load_pair(0)
                NS = len(steps)

                def S1(t):
                    sp_ = steps[t]
                    hp, j, kb, x = sp_["hp"], sp_["j"], sp_["kb"], t % 2
                    ks, qs = hp % 2, sp_["g"] % 2
                    if j == 0 and kb == 1 and hp + 1 < 8:
                        load_pair(hp + 1)
                    if sp_["first"]:
                        dma(QTc[qs][:, :], QT_d[hp * 128:(hp + 1) * 128, j * 512:(j + 1) * 512], [], [("QTc", qs)], ("qtl", qs))
                    diag = kb >= 4 * j
                    ksl = slice(kb * 128, (kb + 1) * 128)
                    for e_ in range(2):
                        ps_ = slice(e_ * 64, (e_ + 1) * 64)
                        o_ = AD[x][:, e_ * 512:(e_ + 1) * 512]
                        mm(o_, KTs[ks][ps_, ksl], QTc[qs][ps_, :], True, not diag, [("KTs", ks), ("QTc", qs)], [ADk[x][e_]])
                        if diag:
                            mm(o_, ident, maskS[:, kb - 4 * j, :], False, True, ["cbf"], [ADk[x][e_]])
                    xe = t % 3
                    act(Eb[xe][:], AD[x], AF.Exp, ADk[x], [("Eb", xe)])
                    act(spb[xe][:], Eb[xe][:], AF.Ln, [("Eb", xe), "cst"], [("spb", xe)], bias=cst[:, 1:2], scale=1.0)
                    if not sp_["last"]:
                        if sp_["first"]:
                            S.add("dve", (lambda o, i_: (lambda e: e.tensor_copy(out=o, in_=i_)))(Rb[xe][:], spb[xe][:]),
                                  [("spb", xe)], [("Rb", xe)])
                        else:
                            rp = (t - 1) % 3
                            tt(Rb[xe][:], Rb[rp][:], spb[xe][:], ALU.add, [("Rb", rp), ("spb", xe)], [("Rb", xe)])

                def S4(t):
                    sp_ = steps[t]
                    x = t % 2
                    xe = t % 3
                    Rprev = None if sp_["first"] else (t - 1) % 3
                    for e_ in range(2):
                        hs = slice(e_ * 512, (e_ + 1) * 512)
                        mm(BD[:, hs], negtri, spb[xe][:, hs], True, Rprev is None, ["cbf", ("spb", xe)], [BDk[e_]])
                        if Rprev is not None:
                            mm(BD[:, hs], negones, Rb[Rprev][:, hs], False, True, ["cbf", ("Rb", Rprev)], [BDk[e_]])
                    act(Gb[x][:], BD, AF.Exp, BDk, [("Gb", x)])
                    tt(aTb[x][:], Eb[xe][:], Gb[x][:], ALU.mult, [("Eb", xe), ("Gb", x)], [("aTb", x)])

                def S7(t):
                    sp_ = steps[t]
                    hp, j, kb, x = sp_["hp"], sp_["j"], sp_["kb"], t % 2
                    ks, qs = hp % 2, sp_["g"] % 2
                    bo = 6 + qs
                    for e_ in range(2):
                        hs = slice(e_ * 512, (e_ + 1) * 512)
                        mm(banks[bo][:], Vz[ks][e_][:, kb, :], aTb[x][:, hs], sp_["first"] and e_ == 0, sp_["last"] and e_ == 1,
                           [("Vz", ks, e_), ("aTb", x)], [bk[bo]])
                    if sp_["last"]:
                        act(oTs[qs][:], banks[bo][:], AF.Copy, [bk[bo]], [("oTs", qs)])
                        dma(OT_d[hp * 128:(hp + 1) * 128, j * 512:(j + 1) * 512], oTs[qs][:], [("oTs", qs)], [("OT", j)], ("oto", qs))

                for t in range(NS + 2):
                    if t < NS:
                        S1(t)
                    if 0 <= t - 1 < NS:
                        S4(t - 1)
                    if 0 <= t - 2 < NS:
                        S7(t - 2)
            else:
                KTs = carve(T, BF16)
                KI2 = carve(T, BF16)
                VAs = carve(NKB * 130, BF16, [NKB, 130])
                sc = carve(T, F32)
                mbb = [[carve(T, BF16) for _ in range(4)] for _ in range(2)]
                QTcs = [carve(4 * 512, BF16, [4, 512]) for _ in range(2)]
                QIcs = [carve(2 * 512, BF16, [2, 512]) for _ in range(2)]
                wics = [carve(16, F32, [4, 4]) for _ in range(2)]
                rtmp = [carve(512, F32) for _ in range(2)]
                PT = [carve(512, BF16) for _ in range(2)]
                otok = carve(4 * 512, BF16, [4, 512])
                oTc = carve(4 * 512, BF16, [4, 512])
                sm = carve(8 + NIT, F32)
                rd = carve(4, F32)
                dma(KTs[:, :], KT_d[0:128, :], [], ["KTs"], "ktl")
                dma(KI2[0:64, :], KI_d[:, :], [], ["KI2"], "kil")
                dma(KI2[64:128, :], KI_d[:, :], [], ["KI2"], "kil")
                dma(VAs[:, :, :], VA_d.rearrange("(kb s) c -> s kb c", s=128), [], ["VAs"], "val")
                tic = [0]
                cast_group(l, 1)
                if l + 1 < layers:
                    cast_group(l + 1, 0)

                def loads(j):
                    cs = slice(j * 512, (j + 1) * 512)
                    p = j % 2
                    dma(QTcs[p][0:64, :, :], QT_d[0:256, cs].rearrange("(h d) t -> d h t", d=64), [], [("QTc", p)], ("qtl", p))
                    dma(QTcs[p][64:128, :, :], QT_d[256:512, cs].rearrange("(h d) t -> d h t", d=64), [], [("QTc", p)], ("qtl", p))
                    dma(QIcs[p][:, :, :], QI_d[:, cs].rearrange("(b p) t -> p b t", p=128), [], [("QIc", p)], ("qil", p))
                    dma(wics[p][:, :, :], WI_d[cs, :].rearrange("(tb s) c -> s tb c", s=128), [], [("wic", p)], ("wil", p))

                def search(j, qb):
                    p = j % 2
                    L = (j + 1) * 512
                    QIc, wic, mbq = QIcs[p], wics[p], mbb[p][qb]
                    for sg in range(j + 1):
                        ssl = slice(sg * 512, (sg + 1) * 512)
                        for hi in range(4):
                            x = tic[0] % 2
                            tic[0] += 1
                            hp_ = slice((hi % 2) * 64, (hi % 2) * 64 + 64)
                            mm(banks[x][:], QIc[hp_, hi // 2, qb * 128:(qb + 1) * 128], KI2[hp_, ssl], True, True,
                               [("QIc", p), "KI2"], [bk[x]])
                            act(rtmp[x][:], banks[x][:], AF.Relu, [bk[x]], [("rtmp", x)])
                            if hi == 0:
                                ts(sc[:, ssl], rtmp[x][:], wic[:, qb, 0:1], None, ALU.mult, ALU.bypass,
                                   [("rtmp", x), ("wic", p)], ["sc"])
                            else:
                                stt(sc[:, ssl], rtmp[x][:], wic[:, qb, hi:hi + 1], sc[:, ssl], ALU.mult, ALU.add,
                                    [("rtmp", x), ("wic", p), "sc"], ["sc"])
                    S.add("dve", (lambda o_, i_: (lambda e: e.tensor_reduce(out=o_, in_=i_, axis=AX.X, op=ALU.max)))(sm[:, 0:1], sc[:, 0:L]),
                          ["sc"], ["sm"])
                    S.add("dve", (lambda o_, i_: (lambda e: e.tensor_reduce(out=o_, in_=i_, axis=AX.X, op=ALU.min)))(sm[:, 1:2], sc[:, 0:L]),
                          ["sc"], ["sm"])
                    tt(sc[:, j * 512:(j + 1) * 512], sc[:, j * 512:(j + 1) * 512], maskQ[:, qb, :], ALU.add,
                       ["sc", "cf"], ["sc"])
                    if j == 0 and qb < 2:
                        thr = sm[:, 1:2]
                    else:
                        tt(sm[:, 2:3], sm[:, 0:1], sm[:, 1:2], ALU.subtract, ["sm"], ["sm"])
                        ts(sm[:, 8:8 + NIT], pow2, sm[:, 2:3], None, ALU.mult, ALU.bypass, ["sm", "cf"], ["sm"])
                        S.add("dve", (lambda o_, i_: (lambda e: e.tensor_copy(out=o_, in_=i_)))(sm[:, 3:4], sm[:, 1:2]), ["sm"], ["sm"])
                        for it in range(NIT):
                            tt(sm[:, 4:5], sm[:, 3:4], sm[:, 8 + it:9 + it], ALU.add, ["sm"], ["sm"])
                            ts(mbq[:, 0:L], sc[:, 0:L], sm[:, 4:5], None, ALU.is_ge, ALU.add, ["sm", "sc"],
                               [("mb", p, qb), "sm"], accum=sm[:, 5:6])
                            ts(sm[:, 6:7], sm[:, 5:6], 256.0, sm[:, 8 + it:9 + it], ALU.is_ge, ALU.mult, ["sm"], ["sm"])
                            tt(sm[:, 3:4], sm[:, 3:4], sm[:, 6:7], ALU.add, ["sm"], ["sm"])
                        thr = sm[:, 3:4]
                    ts(mbq[:, 0:L], sc[:, 0:L], thr, NEG, ALU.is_lt, ALU.mult, ["sm", "sc"], [("mb", p, qb)])

                def att_part(j, heads):
                    p = j % 2
                    QTc, mbp = QTcs[p], mbb[p]
                    tiles = [(h, kb) for h in heads for kb in range(4 * j + 4)]

                    def E1(n_):
                        h, kb = tiles[n_]
                        g = h // 4
                        gp = slice(g * 64, (g + 1) * 64)
                        x = n_ % 2
                        ksl = slice(kb * 128, (kb + 1) * 128)
                        mm(banks[2 + x][:], KTs[gp, ksl], QTc[gp, h % 4, :], True, False, ["KTs", ("QTc", p)], [bk[2 + x]])
                        for qb in range(4):
                            mm(banks[2 + x][:, qb * 128:(qb + 1) * 128], mbp[qb][:, ksl], ident, False, qb == 3,
                               [("mb", p, qb), "cbf"], [bk[2 + x]])
                        act(PT[x][:], banks[2 + x][:], AF.Exp, [bk[2 + x]], [("PT", x)])

                    def E3(n_):
                        h, kb = tiles[n_]
                        g = h // 4
                        x = n_ % 2
                        bo = 4 + (h % 2)
                        for qb in range(4):
                            if kb <= 4 * j + qb:
                                mm(banks[bo][:, qb * 65:(qb + 1) * 65], PT[x][:, qb * 128:(qb + 1) * 128],
                                   VAs[:, kb, g * 65:(g + 1) * 65], (kb == 0 and qb == 0), kb == 4 * j + qb,
                                   [("PT", x), "VAs"], [bk[bo]])
                        if kb == 4 * j + 3:
                            bov = banks[bo][:, 0:260].rearrange("p (a b) -> p a b", a=4)
                            S.add("dve", (lambda o_, bv: (lambda e: e.reciprocal(out=o_, in_=bv)))(rd[:, :], bov[:, :, 64]), [bk[bo]], ["rd"])
                            for qb in range(4):
                                ts(otok[:, qb, h * 64:(h + 1) * 64], banks[bo][:, qb * 65:qb * 65 + 64], rd[:, qb:qb + 1], None,
                                   ALU.mult, ALU.bypass, [bk[bo], "rd"], ["otok"])

                    for n_ in range(len(tiles) + 1):
                        if n_ < len(tiles):
                            E1(n_)
                        if n_ >= 1:
                            E3(n_ - 1)

                loads(0)
                for qb in range(4):
                    search(0, qb)
                for j in range(NCH):
                    cs = slice(j * 512, (j + 1) * 512)
                    if j + 1 < NCH:
                        loads(j + 1)
                    for qb in range(4):
                        if j + 1 < NCH:
                            search(j + 1, qb)
                        att_part(j, (2 * qb, 2 * qb + 1))
                    for cb in range(4):
                        x = tic[0] % 2
                        tic[0] += 1
                        for qb in range(4):
                            mm(banks[x][:, qb * 128:(qb + 1) * 128], otok[:, qb, cb * 128:(cb + 1) * 128], ident, True, True,
                               ["otok", "cbf"], [bk[x]])
                        act(oTc[:, cb, :], banks[x][:], AF.Copy, [bk[x]], ["oTc"])
                    dma(OT_d[0:512, cs].rearrange("(c p) t -> p c t", p=128), oTc[:, :, :], ["oTc"], [("OT", j)], "oto")

            S.barrier()
            apos[0] = 0
            ring = [carve(11520, BF16) for _ in range(3)]
            oTl = carve(8 * 512, BF16, [8, 512])
            gT = carve(NFB * 512, BF16, [NFB, 512])
            rct = [0]
            w_out = wview(od_w_out_b[i] if not even else ev_w_out_b[i])
            w1v = wview(w1_b[l])
            w3v = wview(w3_b[l])
            w2v = w2_b[l].rearrange("(f p) n -> p f n", p=128)
            for j in range(NCH):
                cs = slice(j * 512, (j + 1) * 512)
                dma(xTt[:], src[:, :, cs], [(srck, j)], ["xT"], "xld")
                dma(oTl[:, :, :], OT_d[:, cs].rearrange("(c p) t -> p c t", p=128), [("OT", j)], ["oTl"], "otl")
                Wo, wok = ring_load(w_out[:, :, :], [128, 8, D], ("wb", l, 1))
                pbk = [0]

                def nbank():
                    b = pbk[0] % 6
                    pbk[0] += 1
                    return b

                for nb in range(8):
                    b = nbank()
                    for c in range(8):
                        mm(banks[b][:], Wo[:, c, nb * 128:(nb + 1) * 128], oTl[:, c, :], c == 0, c == 7, [wok, "oTl"], [bk[b]])
                    tt(xTt[:, nb, :], xTt[:, nb, :], banks[b][:], ALU.add, ["xT", bk[b]], ["xT"])
                norm(gf, l, "gf")
                for half in range(2):
                    fs = slice(half * 1408, (half + 1) * 1408)
                    W1p, k1 = ring_load(w1v[:, :, fs], [128, 8, 1408], ("wb", l, 1))
                    W3p, k3 = ring_load(w3v[:, :, fs], [128, 8, 1408], ("wb", l, 1))
                    for fb in range(11):
                        f = half * 11 + fb
                        ba = nbank()
                        bb = nbank()
                        for k in range(8):
                            mm(banks[ba][:], W1p[:, k, fb * 128:(fb + 1) * 128], hT[:, k, :], k == 0, k == 7, [k1, ("hT", k)], [bk[ba]])
                        for k in range(8):
                            mm(banks[bb][:], W3p[:, k, fb * 128:(fb + 1) * 128], hT[:, k, :], k == 0, k == 7, [k3, ("hT", k)], [bk[bb]])
                        tsel = (t32a, "t32a") if f % 2 == 0 else (t32c, "t32c")
                        act(tsel[0][:], banks[ba][:], AF.Silu, [bk[ba]], [tsel[1]])
                        tt(gT[:, f, :], tsel[0][:], banks[bb][:], ALU.mult, [tsel[1], bk[bb]], [("gT", f)])
                for nh in range(2):
                    W2p, k2 = ring_load(w2v[:, :, nh * 512:(nh + 1) * 512], [128, NFB, 512], ("wb", l, 1))
                    for nbl in range(4):
                        nb = nh * 4 + nbl
                        b = nbank()
                        for f in range(NFB):
                            mm(banks[b][:], W2p[:, f, nbl * 128:(nbl + 1) * 128], gT[:, f, :], f == 0, f == NFB - 1,
                               [k2, ("gT", f)], [bk[b]])
                        tt(xTt[:, nb, :], xTt[:, nb, :], banks[b][:], ALU.add, ["xT", bk[b]], ["xT"])
                dma(dst[:, :, cs], xTt[:], ["xT"], [(dstk, j)], "xst")

        S.emit(final_dma=["xst"])
    return nc


def host_consts():
    s = np.arange(128)[:, None]
    t = np.arange(512)[None, :]
    maskS = np.zeros((128, 4, 512), np.float32)
    maskQ = np.zeros((128, 4, 512), np.float32)
    for a in range(4):
        maskS[:, a, :] = np.where(a * 128 + s < t, 0.0, NEG)
        maskQ[:, a, :] = np.where(t <= a * 128 + s, 0.0, -3.0e38)
    jj = np.arange(128)[:, None]
    ss = np.arange(128)[None, :]
    negtri = np.where(jj >= ss, -1.0, 0.0).astype(np.float32)
    negones = -np.ones((128, 128), np.float32)
    ident = np.eye(128, dtype=np.float32)
    blockones = (jj // 64 == ss // 64).astype(np.float32)
    ones = np.ones((128, 128), np.float32)
    cbf = np.concatenate([maskS.reshape(128, 2048), negtri, negones, ident, blockones, ones], axis=1)
    rc16 = np.zeros((128, 4, 16), np.float32)
    for g, w in enumerate(WINS):
        rc16[:, g, :] = 1.0 / np.minimum(np.arange(16) + 1, w)
    pow2 = np.tile((0.5 ** (np.arange(NIT) + 1)).astype(np.float32)[None, :], (128, 1))
    cf = np.concatenate([maskQ.reshape(128, 2048), rc16.reshape(128, 64), pow2], axis=1)
    return np.ascontiguousarray(cbf, np.float32), np.ascontiguousarray(cf, np.float32)


def make_in_maps(inputs, T, nseq):
    cbf, cf = host_consts()
    f = lambda a: np.ascontiguousarray(np.asarray(a, dtype=np.float32))
    x = np.asarray(inputs["x"], dtype=np.float32)

    def gl(g):
        return np.ascontiguousarray(np.asarray(g, np.float32).reshape(4, 8, 128).transpose(2, 0, 1))

    common = {
        "gmix": gl(inputs["norm_mix_g"]), "gffn": gl(inputs["norm_ffn_g"]),
        "ev_w_in": f(inputs["ev_w_in"]), "ev_w_out": f(inputs["ev_w_out"]),
        "od_w_qkv": f(inputs["od_w_qkv"]), "od_w_out": f(inputs["od_w_out"]),
        "ffn_w1": f(inputs["ffn_w1"]), "ffn_w3": f(inputs["ffn_w3"]), "ffn_w2": f(inputs["ffn_w2"]),
        "qg": np.ascontiguousarray(np.tile(np.asarray(inputs["ev_q_norm_g"], np.float32), (1, 2)).T),
        "kg": np.ascontiguousarray(np.tile(np.asarray(inputs["ev_k_norm_g"], np.float32), (1, 2)).T),
        "pool_w": f(inputs["ev_pool_w"]),
        "pscale": np.ascontiguousarray(np.asarray(inputs["ev_pool_scale"], np.float32).reshape(2, 4, 128).transpose(2, 0, 1)),
        "cbf": cbf, "cf": cf,
    }
    maps = []
    for c in range(8):
        b = c % nseq
        xT = np.ascontiguousarray(x[b].T.reshape(8, 128, T).transpose(1, 0, 2))
        m = dict(common)
        m["xT"] = xT
        maps.append(m)
    return maps


_NC_CACHE = {}


def run(inputs, T, layers, nseq):
    key = (T, layers)
    if key not in _NC_CACHE:
        _NC_CACHE[key] = build(T, layers)
    nc = _NC_CACHE[key]
    maps = make_in_maps(inputs, T, nseq)
    res = run_bass_kernel_spmd(nc, maps, core_ids=list(range(8)))
    outs = []
    for b in range(nseq):
        yT = np.asarray(res.results[b]["yT"])
        outs.append(yT.transpose(1, 0, 2).reshape(D, T).T)
    return np.ascontiguousarray(np.stack(outs, 0)).astype(np.float32)


def kernel(**inputs):
    return run(inputs, 4096, 4, 4)
```

```python
import contextlib
import numpy as np
import concourse.bass as bass
import concourse.mybir as mybir
from concourse.bass_utils import run_bass_kernel_spmd

F32 = mybir.dt.float32
BF16 = mybir.dt.bfloat16
ALU = mybir.AluOpType
AF = mybir.ActivationFunctionType
AX = mybir.AxisListType

D = 1024
DFF = 2816
NFB = 22
EV_COLS = 1604
NIT = 15
NEG = -30000.0
IDX_SCALE = 256 ** -0.5
WINS = (2, 4, 8, 16)
EPS = 1e-6


class Sched:
    def __init__(self, nc):
        self.nc = nc
        self.ops = []
        self.lastw = {}
        self.readers = {}
        self.pending = {}
        self.last_on = {}
        self.last_dma = {}
        self.epoch = 0

    def add(self, eng, fn, reads=(), writes=(), dma=None):
        idx = len(self.ops)
        hard, soft = set(), set()
        for k in reads:
            w = self.lastw.get(k)
            if w is not None:
                hard.add(w)
        for k in writes:
            w = self.lastw.get(k)
            if w is not None:
                hard.add(w)
            for r in self.readers.get(k, ()):
                soft.add(r)
        pb = self.pending.pop(eng, None)
        if pb:
            hard |= pb
        for k in reads:
            self.readers.setdefault(k, []).append(idx)
        for k in writes:
            self.lastw[k] = idx
            self.readers[k] = []
        self.ops.append(dict(eng=eng, fn=fn, hard=hard, soft=soft, dma=dma, sig=False, cnt=0, ep=self.epoch))
        if dma is None:
            self.last_on[eng] = idx
        else:
            self.last_dma[dma] = idx
        return idx

    def barrier(self):
        b = set(self.last_on.values()) | set(v for k, v in self.last_dma.items() if not (isinstance(k, tuple) and k[0] == "wcast"))
        self.epoch += 1
        for e in ("pe", "act", "dve", "pool", "sp"):
            self.pending[e] = set(b) | self.pending.get(e, set())

    def emit(self, final_dma):
        nc = self.nc
        ops = self.ops
        need = [None] * len(ops)
        for i, op in enumerate(ops):
            deps = set()
            for d in op["hard"]:
                od = ops[d]
                if od["dma"] is None and op["dma"] is None and od["eng"] == op["eng"] == "pe":
                    continue
                deps.add(d)
            for d in op["soft"]:
                od = ops[d]
                if od["dma"] is None and op["dma"] is None and od["eng"] == op["eng"]:
                    continue
                deps.add(d)
            need[i] = deps
            for d in deps:
                ops[d]["sig"] = True
        ecount = {}
        dcount = {}
        for op in ops:
            if op["dma"] is not None:
                dcount[op["dma"]] = dcount.get(op["dma"], 0) + 16
                op["cnt"] = dcount[op["dma"]]
            elif op["sig"]:
                ek = (op["eng"], op["ep"])
                ecount[ek] = ecount.get(ek, 0) + 1
                op["cnt"] = ecount[ek]
        with contextlib.ExitStack() as st:
            esem = {ek: st.enter_context(nc.semaphore("es_%s_%d" % ek)) for ek in sorted(ecount)}
            dsem = {k: st.enter_context(nc.semaphore("ds_%d" % n)) for n, k in enumerate(sorted(dcount, key=str))}
            block = st.enter_context(nc.Block())
            streams = {e: [] for e in ("pe", "act", "dve", "pool", "sp")}
            for i, op in enumerate(ops):
                streams[op["eng"]].append(i)

            def run(e, ename):
                waited = {}
                for i in streams[ename]:
                    op = ops[i]
                    tgt = {}
                    for d in need[i]:
                        od = ops[d]
                        s = dsem[od["dma"]] if od["dma"] is not None else esem[(od["eng"], od["ep"])]
                        key = id(s)
                        if od["cnt"] > tgt.get(key, (None, 0))[1]:
                            tgt[key] = (s, od["cnt"])
                    for key, (s, v) in tgt.items():
                        if waited.get(key, 0) < v:
                            e.wait_ge(s, v)
                            waited[key] = v
                    ins = op["fn"](e)
                    if op["dma"] is not None:
                        ins.then_inc(dsem[op["dma"]], 16)
                    elif op["sig"]:
                        ins.then_inc(esem[(ename, op["ep"])], 1)
                if ename == "sp":
                    for k in final_dma:
                        e.wait_ge(dsem[k], dcount[k])

            block.tensor(lambda e: run(e, "pe"))
            block.scalar(lambda e: run(e, "act"))
            block.vector(lambda e: run(e, "dve"))
            block.gpsimd(lambda e: run(e, "pool"))
            block.sync(lambda e: run(e, "sp"))


def build(T, layers):
    NCH = T // 512
    NKB = T // 128
    nc = bass.Bass("TRN2", target_bir_lowering=False)

    def din(name, shape, dt=F32):
        return nc.dram_tensor(name, list(shape), dt, kind="ExternalInput").ap()

    xT_in = din("xT", [128, 8, T])
    gmix = din("gmix", [128, 4, 8])
    gffn = din("gffn", [128, 4, 8])
    ev_w_in = din("ev_w_in", [2, D, EV_COLS])
    ev_w_out = din("ev_w_out", [2, D, D])
    od_w_qkv = din("od_w_qkv", [2, D, 3 * D])
    od_w_out = din("od_w_out", [2, D, D])
    w1 = din("ffn_w1", [4, D, DFF])
    w3 = din("ffn_w3", [4, D, DFF])
    w2 = din("ffn_w2", [4, DFF, D])
    qg_in = din("qg", [128, 2])
    kg_in = din("kg", [128, 2])
    poolw_in = din("pool_w", [2, 4, 128, 128])
    pscale_in = din("pscale", [128, 2, 4])
    cbf_in = din("cbf", [128, 2048 + 5 * 128])
    cf_in = din("cf", [128, 2048 + 64 + NIT])
    yT = nc.dram_tensor("yT", [128, 8, T], F32, kind="ExternalOutput").ap()

    XT_d = nc.dram_tensor("XT_d", [128, 8, T], F32).ap()
    QT_d = nc.dram_tensor("QT_d", [D, T], BF16).ap()
    KT_d = nc.dram_tensor("KT_d", [D, T], BF16).ap()
    V_d = nc.dram_tensor("V_d", [T, D], BF16).ap()
    VA_d = nc.dram_tensor("VA_d", [T, 130], BF16).ap()
    QI_d = nc.dram_tensor("QI_d", [256, T], BF16).ap()
    KI_d = nc.dram_tensor("KI_d", [64, T], BF16).ap()
    WI_d = nc.dram_tensor("WI_d", [T, 4], F32).ap()
    OT_d = nc.dram_tensor("OT_d", [D, T], BF16).ap()

    def wscr(name, ap):
        return nc.dram_tensor(name, list(ap.shape), BF16).ap()

    ev_w_in_b = wscr("ev_w_in_b", ev_w_in)
    ev_w_out_b = wscr("ev_w_out_b", ev_w_out)
    od_w_qkv_b = wscr("od_w_qkv_b", od_w_qkv)
    od_w_out_b = wscr("od_w_out_b", od_w_out)
    w1_b = wscr("w1_b", w1)
    w3_b = wscr("w3_b", w3)
    w2_b = wscr("w2_b", w2)

    S = Sched(nc)
    with contextlib.ExitStack() as st:
        def sb(name, shape, dt):
            return st.enter_context(nc.sbuf_tensor(name, list(shape), dt))

        xTt = sb("xTt", [128, 8, 512], F32)
        hT = sb("hT", [128, 8, 512], BF16)
        sq = sb("sq", [128, 8, 512], BF16)
        cbf = sb("cbf_s", [128, 2048 + 5 * 128], BF16)
        cf = sb("cf_s", [128, 2048 + 64 + NIT], F32)
        gm = sb("gm", [128, 4, 8], F32)
        gf = sb("gf", [128, 4, 8], F32)
        qg = sb("qg_s", [128, 2], F32)
        kg = sb("kg_s", [128, 2], F32)
        psc = sb("psc", [128, 2, 4], F32)
        poolw = sb("poolw", [128, 2, 4, 128], BF16)
        cst = sb("cst", [128, 4], F32)
        t32a = sb("t32a", [128, 512], F32)
        t32b = sb("t32b", [128, 512], F32)
        t32c = sb("t32c", [128, 512], F32)
        ARENA = 67584
        arena = sb("arena", [128, ARENA], BF16)
        psall = st.enter_context(nc.psum_tensor("psall", [128, 4096], F32))
        banks = [psall[:, i * 512:(i + 1) * 512] for i in range(8)]

        maskS = cbf[:, 0:2048].rearrange("p (a b) -> p a b", a=4)
        negtri = cbf[:, 2048:2176]
        negones = cbf[:, 2176:2304]
        ident = cbf[:, 2304:2432]
        blockones = cbf[:, 2432:2560]
        ones = cbf[:, 2560:2688]
        maskQ = cf[:, 0:2048].rearrange("p (a b) -> p a b", a=4)
        rc16 = cf[:, 2048:2112].rearrange("p (a b) -> p a b", a=4)
        pow2 = cf[:, 2112:2112 + NIT]

        apos = [0]

        def carve(n_el, dt, shape=None):
            nb = n_el * (4 if dt == F32 else 2)
            nb = (nb + 63) // 64 * 64
            o = apos[0]
            assert o + nb <= ARENA * 2, (o, nb)
            apos[0] = o + nb
            v = arena[:, o // 2:(o + nb) // 2]
            if dt == F32:
                v = v.bitcast(F32)
            v = v[:, 0:n_el]
            if shape is not None:
                names = " ".join("a%d" % i for i in range(len(shape)))
                kw = {"a%d" % i: s for i, s in enumerate(shape[:-1])}
                v = v.rearrange("p (%s) -> p %s" % (names, names), **kw)
            return v

        def mm(out, lhsT, rhs, start, stop, reads, writes):
            S.add("pe", lambda e: e.matmul(out, lhsT, rhs, start=start, stop=stop), reads, writes)

        def act(out, in_, func, reads, writes, bias=None, scale=None):
            kw = {}
            if bias is not None:
                kw["bias"] = bias
            if scale is not None:
                kw["scale"] = scale
            S.add("act", lambda e: e.activation(out=out, in_=in_, func=func, **kw), reads, writes)

        def dma(out, in_, reads, writes, slot, eng="sp"):
            S.add(eng, lambda e: e.dma_start(out=out, in_=in_), reads, writes, dma=slot)

        def tt(out, in0, in1, op, reads, writes, eng="dve"):
            S.add(eng, lambda e: e.tensor_tensor(out=out, in0=in0, in1=in1, op=op), reads, writes)

        def ts(out, in0, s1, s2, op0, op1, reads, writes, accum=None, eng="dve"):
            if accum is None:
                S.add(eng, lambda e: e.tensor_scalar(out=out, in0=in0, scalar1=s1, scalar2=s2, op0=op0, op1=op1),
                      reads, writes)
            else:
                S.add(eng, lambda e: e.tensor_scalar(out=out, in0=in0, scalar1=s1, scalar2=s2, op0=op0, op1=op1,
                                                     accum_out=accum), reads, writes)

        def stt(out, in0, scalar, in1, op0, op1, reads, writes, eng="dve"):
            S.add(eng, lambda e: e.scalar_tensor_tensor(out=out, in0=in0, scalar=scalar, in1=in1, op0=op0, op1=op1),
                  reads, writes)

        def memset(ap, v, writes, eng="pool"):
            S.add(eng, lambda e: e.memset(ap, v), (), writes)

        def wview(w_ap):
            return w_ap.rearrange("(k p) n -> p k n", p=128)

        dma(cbf[:], cbf_in[:, :], (), ["cbf"], "c", eng="pool")
        dma(cf[:], cf_in[:, :], (), ["cf"], "c")
        dma(gm[:], gmix[:, :, :], (), ["gm"], "c")
        dma(gf[:], gffn[:, :, :], (), ["gf"], "c")
        dma(qg[:], qg_in[:, :], (), ["qg"], "c")
        dma(kg[:], kg_in[:, :], (), ["kg"], "c")
        dma(psc[:], pscale_in[:, :, :], (), ["psc"], "c")
        for i in range(2):
            dma(poolw[:, i, :, :], poolw_in[i].rearrange("g c d -> c g d"), (), ["poolw%d" % i], "c", eng="pool")
        memset(cst[:, 0:1], EPS, ["cst"])
        memset(cst[:, 1:2], 1.0, ["cst"])
        memset(cst[:, 2:3], 64 * EPS, ["cst"])
        memset(cst[:, 3:4], 0.0, ["cst"])

        bk = ["ps%d" % i for i in range(8)]

        def cast_w(src, dstb, l, grp):
            rows = src.shape[0]
            for r0 in range(0, rows, 128):
                dma(dstb[r0:r0 + 128, :], src[r0:r0 + 128, :], [], [("wb", l, grp)], ("wcast", l, grp), eng="pool")

        def cast_group(l_, grp):
            i_ = l_ // 2
            if grp == 0:
                if l_ % 2 == 0:
                    cast_w(ev_w_in[i_], ev_w_in_b[i_], l_, 0)
                else:
                    cast_w(od_w_qkv[i_], od_w_qkv_b[i_], l_, 0)
            else:
                cast_w((ev_w_out if l_ % 2 == 0 else od_w_out)[i_], (ev_w_out_b if l_ % 2 == 0 else od_w_out_b)[i_], l_, 1)
                cast_w(w1[l_], w1_b[l_], l_, 1)
                cast_w(w3[l_], w3_b[l_], l_, 1)
                cast_w(w2[l_], w2_b[l_], l_, 1)

        cast_group(0, 0)

        def norm(gcol_t, l, gkey, xTt=xTt, xkey="xT"):
            act(sq[:], xTt[:], AF.Square, [xkey], ["sq"])
            for k in range(8):
                mm(banks[7][:], ones, sq[:, k, :], k == 0, k == 7, ["sq", "cbf"], [bk[7]])
            act(t32a[:], banks[7][:], AF.Ln, [bk[7], "cst"], ["t32a"], bias=cst[:, 0:1], scale=1.0 / D)
            act(t32b[:], t32a[:], AF.Exp, ["t32a"], ["t32b"], scale=-0.5)
            for k in range(8):
                stt(hT[:, k, :], xTt[:, k, :], gcol_t[:, l, k:k + 1], t32b[:], ALU.mult, ALU.mult,
                    [xkey, "t32b", gkey], [("hT", k)])

        hkeys = [("hT", k) for k in range(8)]

        for l in range(layers):
            i = l // 2
            even = (l % 2 == 0)
            src = xT_in if l == 0 else XT_d
            dst = yT if l == layers - 1 else XT_d
            srck = "xin" if l == 0 else "XT"
            dstk = "yT" if l == layers - 1 else "XT"

            S.barrier()
            apos[0] = 0
            ring = [carve(11520, BF16) for _ in range(3)]
            stg = [carve(512, BF16) for _ in range(4)]
            rct = [0]
            sct = [0]

            def ring_load(src_ap, shape3, key):
                r = rct[0] % 3
                rct[0] += 1
                n = shape3[1] * shape3[2]
                v = ring[r][:, 0:n].rearrange("p (a b) -> p a b", a=shape3[1])
                dma(v, src_ap, [key], [("ring", r)], ("ring", r), eng="pool")
                return v, ("ring", r)

            def stage_out(ps_ap, dram_ap, psk, dkey, scale=None, nparts=128):
                s = sct[0] % 4
                sct[0] += 1
                o = stg[s][0:nparts, :]
                if scale is None:
                    act(o, ps_ap, AF.Copy, [psk], [("stg", s)])
                else:
                    act(o, ps_ap, AF.Copy, [psk, "psc"], [("stg", s)], scale=scale)
                dma(dram_ap, o, [("stg", s)], [dkey], ("stgo", s))

            if even:
                uext = carve(4 * 528, F32, [4, 528])
                pa = carve(528, F32)
                pb = carve(528, F32)
                mixb = carve(512, BF16)
                vstage = carve(4 * 130, BF16, [4, 130])
                wist = carve(16, F32, [4, 4])
                t16 = carve(16, F32)
                memset(uext[:, :, 0:16], 0.0, ["uext"])
                memset(vstage[:], 1.0, ["vstage"])

            for j in range(NCH):
                cs = slice(j * 512, (j + 1) * 512)
                dma(xTt[:], src[:, :, cs], [(srck, j)], ["xT"], "xld")
                norm(gm, l, "gm")
                bi = [0]

                def nb_bank():
                    b = bi[0] % 4
                    bi[0] += 1
                    return b

                if not even:
                    wq = wview(od_w_qkv_b[i])
                    for piece in range(3):
                        W, wk = ring_load(wq[:, :, piece * D:(piece + 1) * D], [128, 8, D], ("wb", l, 0))
                        if piece < 2:
                            tgt = QT_d if piece == 0 else KT_d
                            tk = "QT" if piece == 0 else "KT"
                            for nb in range(8):
                                b = nb_bank()
                                for k in range(8):
                                    mm(banks[b][:], W[:, k, nb * 128:(nb + 1) * 128], hT[:, k, :], k == 0, k == 7,
                                       [wk, ("hT", k)], [bk[b]])
                                stage_out(banks[b][:], tgt[nb * 128:(nb + 1) * 128, cs], bk[b], (tk, j),
                                          scale=(0.125 if piece == 0 else None))
                        else:
                            for tb in range(4):
                                for nh in range(2):
                                    b = nb_bank()
                                    for k in range(8):
                                        mm(banks[b][:], hT[:, k, tb * 128:(tb + 1) * 128], W[:, k, nh * 512:(nh + 1) * 512],
                                           k == 0, k == 7, [wk, ("hT", k)], [bk[b]])
                                    stage_out(banks[b][:], V_d[j * 512 + tb * 128:j * 512 + (tb + 1) * 128, nh * 512:(nh + 1) * 512],
                                              bk[b], ("V", j))
                else:
                    wv = wview(ev_w_in_b[i])
                    WA, wak = ring_load(wv[:, :, 0:1092], [128, 8, 1092], ("wb", l, 0))
                    WB, wbk = ring_load(wv[:, :, 1092:1604], [128, 8, 512], ("wb", l, 0))

                    def qkproc(c0, gcol, gkey, lnscale, lnbias, dram_ap, dkey):
                        b = nb_bank()
                        for k in range(8):
                            mm(banks[b][:], WA[:, k, c0:c0 + 128], hT[:, k, :], k == 0, k == 7, [wak, ("hT", k)], [bk[b]])
                        act(t32c[:], banks[b][:], AF.Copy, [bk[b]], ["t32c"])
                        act(sq[:, 0, :], banks[b][:], AF.Square, [bk[b]], ["sq"])
                        mm(banks[6][:], blockones, sq[:, 0, :], True, True, ["sq", "cbf"], [bk[6]])
                        act(t32a[:], banks[6][:], AF.Ln, [bk[6], "cst"], ["t32a"], bias=lnbias, scale=lnscale)
                        act(t32b[:], t32a[:], AF.Exp, ["t32a"], ["t32b"], scale=-0.5)
                        s = sct[0] % 4
                        sct[0] += 1
                        stt(stg[s][:], t32c[:], gcol, t32b[:], ALU.mult, ALU.mult, ["t32c", "t32b", gkey], [("stg", s)])
                        dma(dram_ap, stg[s][:], [("stg", s)], [dkey], ("stgo", s))

                    for nb in range(4):
                        qkproc(nb * 128, qg[:, i:i + 1], "qg", 1.0, cst[:, 2:3], QT_d[nb * 128:(nb + 1) * 128, cs], ("QT", j))
                    qkproc(512, kg[:, i:i + 1], "kg", 1.0 / 64, cst[:, 0:1], KT_d[0:128, cs], ("KT", j))
                    for tb in range(4):
                        b = nb_bank()
                        for k in range(8):
                            mm(banks[b][:, 0:128], hT[:, k, tb * 128:(tb + 1) * 128], WA[:, k, 640:768], k == 0, k == 7,
                               [wak, ("hT", k)], [bk[b]])
                        for k in range(8):
                            mm(banks[b][:, 128:132], hT[:, k, tb * 128:(tb + 1) * 128], WA[:, k, 1088:1092], k == 0, k == 7,
                               [wak, ("hT", k)], [bk[b]])
                        for g in range(2):
                            act(vstage[:, tb, g * 65:g * 65 + 64], banks[b][:, g * 64:(g + 1) * 64], AF.Copy, [bk[b]], ["vstage"])
                        act(wist[:, tb, :], banks[b][:, 128:132], AF.Copy, [bk[b]], ["wist"], scale=IDX_SCALE)
                    dma(VA_d[cs, :].rearrange("(tb s) c -> s tb c", s=128), vstage[:], ["vstage"], [("VA", j)], "vao")
                    dma(WI_d[cs, :].rearrange("(tb s) c -> s tb c", s=128), wist[:], ["wist"], [("WI", j)], "wio")
                    for nb in range(2):
                        b = nb_bank()
                        for k in range(8):
                            mm(banks[b][:], WA[:, k, 768 + nb * 128:768 + (nb + 1) * 128], hT[:, k, :], k == 0, k == 7,
                               [wak, ("hT", k)], [bk[b]])
                        stage_out(banks[b][:], QI_d[nb * 128:(nb + 1) * 128, cs], bk[b], ("QI", j))
                    b = nb_bank()
                    for k in range(8):
                        mm(banks[b][0:64, :], WA[:, k, 1024:1088], hT[:, k, :], k == 0, k == 7, [wak, ("hT", k)], [bk[b]])
                    stage_out(banks[b][0:64, :], KI_d[0:64, cs], bk[b], ("KI", j), nparts=64)
                    for g in range(4):
                        b = nb_bank()
                        for k in range(8):
                            mm(banks[b][:], WB[:, k, g * 128:(g + 1) * 128], hT[:, k, :], k == 0, k == 7, [wbk, ("hT", k)], [bk[b]])
                        act(uext[:, g, 16:528], banks[b][:], AF.Copy, [bk[b]], ["uext"])
                        a_ap, a_key = uext[:, g, :], "uext"
                        dsh = 1
                        tgl = 0
                        for _ in range(g + 1):
                            o_ap, o_key = (pa, "pa") if tgl == 0 else (pb, "pb")
                            lo_ = 2 * dsh - 1
                            tt(o_ap[:, lo_:528], a_ap[:, lo_:528], a_ap[:, lo_ - dsh:528 - dsh], ALU.add,
                               [a_key], [o_key], eng="pool")
                            a_ap, a_key = o_ap, o_key
                            dsh *= 2
                            tgl ^= 1
                        w = WINS[g]
                        stt(mixb[:], a_ap[:, 16:528], 1.0 / w, uext[:, g, 16:528], ALU.mult, ALU.subtract,
                            [a_key, "uext"], ["mixb"])
                        if j == 0:
                            tt(t16[:], a_ap[:, 16:32], rc16[:, g, :], ALU.mult, [a_key, "cf"], ["t16"])
                            tt(mixb[:, 0:16], t16[:], uext[:, g, 16:32], ALU.subtract, ["t16", "uext", "mixb"], ["mixb"])
                        b2 = nb_bank()
                        mm(banks[b2][:], poolw[:, i, g, :], mixb[:], True, True, ["mixb", "poolw%d" % i], [bk[b2]])
                        stage_out(banks[b2][:], OT_d[(4 + g) * 128:(5 + g) * 128, cs], bk[b2], ("OT", j),
                                  scale=psc[:, i, g:g + 1])
                    S.add("pool", (lambda o_, i_: (lambda e: e.tensor_copy(out=o_, in_=i_)))(uext[:, :, 0:16], uext[:, :, 512:528]),
                          ["uext", "pa", "pb", "mixb"], ["uext"])

            S.barrier()
            apos[0] = 0
            allk = lambda nm, jj: [(nm, c) for c in range(jj + 1)]
            if not even:
                KTs = [carve(T, BF16) for _ in range(2)]
                Vz = [[carve(NKB * 128, BF16, [NKB, 128]) for _ in range(2)] for _ in range(2)]
                QTc = [carve(512, BF16) for _ in range(2)]
                Eb = [carve(1024, F32) for _ in range(3)]
                spb = [carve(1024, BF16) for _ in range(3)]
                Gb = [carve(1024, F32) for _ in range(2)]
                aTb = [carve(1024, BF16) for _ in range(2)]
                Rb = [carve(1024, BF16) for _ in range(3)]
                oTs = [carve(512, BF16) for _ in range(2)]
                for ks in range(2):
                    memset(Vz[ks][0][:, :, 64:128], 0.0, [("Vz", ks, 0)])
                    memset(Vz[ks][1][:, :, 0:64], 0.0, [("Vz", ks, 1)])
                cast_group(l, 1)
                if l + 1 < layers:
                    cast_group(l + 1, 0)
                Vv = V_d.rearrange("(kb s) c -> s kb c", s=128)
                AD = [psall[:, 0:1024], psall[:, 1024:2048]]
                ADk = [["ps0", "ps1"], ["ps2", "ps3"]]
                BD = psall[:, 2048:3072]
                BDk = ["ps4", "ps5"]

                def load_pair(hp):
                    ks = hp % 2
                    dma(KTs[ks][:, :], KT_d[hp * 128:(hp + 1) * 128, :], [], [("KTs", ks)], ("ktl", ks))
                    dma(Vz[ks][0][:, :, 0:64], Vv[:, :, hp * 128:hp * 128 + 64], [], [("Vz", ks, 0)], ("vz0", ks))
                    dma(Vz[ks][1][:, :, 64:128], Vv[:, :, hp * 128 + 64:hp * 128 + 128], [], [("Vz", ks, 1)], ("vz1", ks))

                steps = []
                gi = 0
                for hp in range(8):
                    for j in range(NCH):
                        nkb = 4 * j + 4
                        for n_, kb in enumerate(range(4 * j + 3, -1, -1)):
                            steps.append(dict(hp=hp, j=j, kb=kb, first=(n_ == 0), last=(kb == 0), g=gi, n=len(steps)))
                        gi += 1
                load_pair(0)
                NS = len(steps)

                def S1(t):
                    sp_ = steps[t]
                    hp, j, kb, x = sp_["hp"], sp_["j"], sp_["kb"], t % 2
                    ks, qs = hp % 2, sp_["g"] % 2
                    if j == 0 and kb == 1 and hp + 1 < 8:
                        load_pair(hp + 1)
                    if sp_["first"]:
                        dma(QTc[qs][:, :], QT_d[hp * 128:(hp + 1) * 128, j * 512:(j + 1) * 512], [], [("QTc", qs)], ("qtl", qs))
                    diag = kb >= 4 * j
                    ksl = slice(kb * 128, (kb + 1) * 128)
                    for e_ in range(2):
                        ps_ = slice(e_ * 64, (e_ + 1) * 64)
                        o_ = AD[x][:, e_ * 512:(e_ + 1) * 512]
                        mm(o_, KTs[ks][ps_, ksl], QTc[qs][ps_, :], True, not diag, [("KTs", ks), ("QTc", qs)], [ADk[x][e_]])
                        if diag:
                            mm(o_, ident, maskS[:, kb - 4 * j, :], False, True, ["cbf"], [ADk[x][e_]])
                    xe = t % 3
                    act(Eb[xe][:], AD[x], AF.Exp, ADk[x], [("Eb", xe)])
                    act(spb[xe][:], Eb[xe][:], AF.Ln, [("Eb", xe), "cst"], [("spb", xe)], bias=cst[:, 1:2], scale=1.0)
                    if not sp_["last"]:
                        if sp_["first"]:
                            S.add("dve", (lambda o, i_: (lambda e: e.tensor_copy(out=o, in_=i_)))(Rb[xe][:], spb[xe][:]),
                                  [("spb", xe)], [("Rb", xe)])
                        else:
                            rp = (t - 1) % 3
                            tt(Rb[xe][:], Rb[rp][:], spb[xe][:], ALU.add, [("Rb", rp), ("spb", xe)], [("Rb", xe)])

                def S4(t):
                    sp_ = steps[t]
                    x = t % 2
                    xe = t % 3
                    Rprev = None if sp_["first"] else (t - 1) % 3
                    for e_ in range(2):
                        hs = slice(e_ * 512, (e_ + 1) * 512)
                        mm(BD[:, hs], negtri, spb[xe][:, hs], True, Rprev is None, ["cbf", ("spb", xe)], [BDk[e_]])
                        if Rprev is not None:
                            mm(BD[:, hs], negones, Rb[Rprev][:, hs], False, True, ["cbf", ("Rb", Rprev)], [BDk[e_]])
                    act(Gb[x][:], BD, AF.Exp, BDk, [("Gb", x)])
                    tt(aTb[x][:], Eb[xe][:], Gb[x][:], ALU.mult, [("Eb", xe), ("Gb", x)], [("aTb", x)])

                def S7(t):
                    sp_ = steps[t]
                    hp, j, kb, x = sp_["hp"], sp_["j"], sp_["kb"], t % 2
                    ks, qs = hp % 2, sp_["g"] % 2
                    bo = 6 + qs
                    for e_ in range(2):
                        hs = slice(e_ * 512, (e_ + 1) * 512)
                        mm(banks[bo][:], Vz[ks][e_][:, kb, :], aTb[x][:, hs], sp_["first"] and e_ == 0, sp_["last"] and e_ == 1,
                           [("Vz", ks, e_), ("aTb", x)], [bk[bo]])
                    if sp_["last"]:
                        act(oTs[qs][:], banks[bo][:], AF.Copy, [bk[bo]], [("oTs", qs)])
                        dma(OT_d[hp * 128:(hp + 1) * 128, j * 512:(j + 1) * 512], oTs[qs][:], [("oTs", qs)], [("OT", j)], ("oto", qs))

                for t in range(NS + 2):
                    if t < NS:
                        S1(t)
                    if 0 <= t - 1 < NS:
                        S4(t - 1)
                    if 0 <= t - 2 < NS:
                        S7(t - 2)
            else:
                KTs = carve(T, BF16)
                KI2 = carve(T, BF16)
                VAs = carve(NKB * 130, BF16, [NKB, 130])
                sc = carve(T, F32)
                mbb = [[carve(T, BF16) for _ in range(4)] for _ in range(2)]
                QTcs = [carve(4 * 512, BF16, [4, 512]) for _ in range(2)]
                QIcs = [carve(2 * 512, BF16, [2, 512]) for _ in range(2)]
                wics = [carve(16, F32, [4, 4]) for _ in range(2)]
                rtmp = [carve(512, F32) for _ in range(2)]
                PT = [carve(512, BF16) for _ in range(2)]
                otok = carve(4 * 512, BF16, [4, 512])
                oTc = carve(4 * 512, BF16, [4, 512])
                sm = carve(8 + NIT, F32)
                rd = carve(4, F32)
                dma(KTs[:, :], KT_d[0:128, :], [], ["KTs"], "ktl")
                dma(KI2[0:64, :], KI_d[:, :], [], ["KI2"], "kil")
                dma(KI2[64:128, :], KI_d[:, :], [], ["KI2"], "kil")
                dma(VAs[:, :, :], VA_d.rearrange("(kb s) c -> s kb c", s=128), [], ["VAs"], "val")
                tic = [0]
                cast_group(l, 1)
                if l + 1 < layers:
                    cast_group(l + 1, 0)

                def loads(j):
                    cs = slice(j * 512, (j + 1) * 512)
                    p = j % 2
                    dma(QTcs[p][0:64, :, :], QT_d[0:256, cs].rearrange("(h d) t -> d h t", d=64), [], [("QTc", p)], ("qtl", p))
                    dma(QTcs[p][64:128, :, :], QT_d[256:512, cs].rearrange("(h d) t -> d h t", d=64), [], [("QTc", p)], ("qtl", p))
                    dma(QIcs[p][:, :, :], QI_d[:, cs].rearrange("(b p) t -> p b t", p=128), [], [("QIc", p)], ("qil", p))
                    dma(wics[p][:, :, :], WI_d[cs, :].rearrange("(tb s) c -> s tb c", s=128), [], [("wic", p)], ("wil", p))

                def search(j, qb):
                    p = j % 2
                    L = (j + 1) * 512
                    QIc, wic, mbq = QIcs[p], wics[p], mbb[p][qb]
                    for sg in range(j + 1):
                        ssl = slice(sg * 512, (sg + 1) * 512)
                        for hi in range(4):
                            x = tic[0] % 2
                            tic[0] += 1
                            hp_ = slice((hi % 2) * 64, (hi % 2) * 64 + 64)
                            mm(banks[x][:], QIc[hp_, hi // 2, qb * 128:(qb + 1) * 128], KI2[hp_, ssl], True, True,
                               [("QIc", p), "KI2"], [bk[x]])
                            act(rtmp[x][:], banks[x][:], AF.Relu, [bk[x]], [("rtmp", x)])
                            if hi == 0:
                                ts(sc[:, ssl], rtmp[x][:], wic[:, qb, 0:1], None, ALU.mult, ALU.bypass,
                                   [("rtmp", x), ("wic", p)], ["sc"])
                            else:
                                stt(sc[:, ssl], rtmp[x][:], wic[:, qb, hi:hi + 1], sc[:, ssl], ALU.mult, ALU.add,
                                    [("rtmp", x), ("wic", p), "sc"], ["sc"])
                    S.add("dve", (lambda o_, i_: (lambda e: e.tensor_reduce(out=o_, in_=i_, axis=AX.X, op=ALU.max)))(sm[:, 0:1], sc[:, 0:L]),
                          ["sc"], ["sm"])
                    S.add("dve", (lambda o_, i_: (lambda e: e.tensor_reduce(out=o_, in_=i_, axis=AX.X, op=ALU.min)))(sm[:, 1:2], sc[:, 0:L]),
                          ["sc"], ["sm"])
                    tt(sc[:, j * 512:(j + 1) * 512], sc[:, j * 512:(j + 1) * 512], maskQ[:, qb, :], ALU.add,
                       ["sc", "cf"], ["sc"])
                    if j == 0 and qb < 2:
                        thr = sm[:, 1:2]
                    else:
                        tt(sm[:, 2:3], sm[:, 0:1], sm[:, 1:2], ALU.subtract, ["sm"], ["sm"])
                        ts(sm[:, 8:8 + NIT], pow2, sm[:, 2:3], None, ALU.mult, ALU.bypass, ["sm", "cf"], ["sm"])
                        tt(sm[:, 4:5], sm[:, 1:2], sm[:, 8:9], ALU.add, ["sm"], ["sm"])
                        for it in range(NIT):
                            ts(mbq[:, 0:L], sc[:, 0:L], sm[:, 4:5], None, ALU.is_ge, ALU.add, ["sm", "sc"],
                               [("mb", p, qb), "sm"], accum=sm[:, 5:6])
                            ts(sm[:, 6:7], sm[:, 5:6], 256.0, 0.5, ALU.is_ge, ALU.subtract, ["sm"], ["sm"])
                            stt(sm[:, 4:5], sm[:, 6:7], sm[:, 8 + it:9 + it], sm[:, 4:5], ALU.mult, ALU.add, ["sm"], ["sm"])
                        stt(sm[:, 3:4], sm[:, 8 + NIT - 1:8 + NIT], -0.5, sm[:, 4:5], ALU.mult, ALU.add, ["sm"], ["sm"])
                        thr = sm[:, 3:4]
                    ts(mbq[:, 0:L], sc[:, 0:L], thr, NEG, ALU.is_lt, ALU.mult, ["sm", "sc"], [("mb", p, qb)])

                def att_part(j, heads):
                    p = j % 2
                    QTc, mbp = QTcs[p], mbb[p]
                    tiles = [(h, kb) for h in heads for kb in range(4 * j + 4)]

                    def E1(n_):
                        h, kb = tiles[n_]
                        g = h // 4
                        gp = slice(g * 64, (g + 1) * 64)
                        x = n_ % 2
                        ksl = slice(kb * 128, (kb + 1) * 128)
                        mm(banks[2 + x][:], KTs[gp, ksl], QTc[gp, h % 4, :], True, False, ["KTs", ("QTc", p)], [bk[2 + x]])
                        for qb in range(4):
                            mm(banks[2 + x][:, qb * 128:(qb + 1) * 128], mbp[qb][:, ksl], ident, False, qb == 3,
                               [("mb", p, qb), "cbf"], [bk[2 + x]])
                        act(PT[x][:], banks[2 + x][:], AF.Exp, [bk[2 + x]], [("PT", x)])

                    def E3(n_):
                        h, kb = tiles[n_]
                        g = h // 4
                        x = n_ % 2
                        bo = 4 + (h % 2)
                        for qb in range(4):
                            if kb <= 4 * j + qb:
                                mm(banks[bo][:, qb * 65:(qb + 1) * 65], PT[x][:, qb * 128:(qb + 1) * 128],
                                   VAs[:, kb, g * 65:(g + 1) * 65], (kb == 0 and qb == 0), kb == 4 * j + qb,
                                   [("PT", x), "VAs"], [bk[bo]])
                        if kb == 4 * j + 3:
                            bov = banks[bo][:, 0:260].rearrange("p (a b) -> p a b", a=4)
                            S.add("dve", (lambda o_, bv: (lambda e: e.reciprocal(out=o_, in_=bv)))(rd[:, :], bov[:, :, 64]), [bk[bo]], ["rd"])
                            for qb in range(4):
                                ts(otok[:, qb, h * 64:(h + 1) * 64], banks[bo][:, qb * 65:qb * 65 + 64], rd[:, qb:qb + 1], None,
                                   ALU.mult, ALU.bypass, [bk[bo], "rd"], ["otok"])

                    for n_ in range(len(tiles) + 1):
                        if n_ < len(tiles):
                            E1(n_)
                        if n_ >= 1:
                            E3(n_ - 1)

                loads(0)
                for qb in range(4):
                    search(0, qb)
                for j in range(NCH):
                    cs = slice(j * 512, (j + 1) * 512)
                    if j + 1 < NCH:
                        loads(j + 1)
                    for qb in range(4):
                        if j + 1 < NCH:
                            search(j + 1, qb)
                        att_part(j, (2 * qb, 2 * qb + 1))
                    for cb in range(4):
                        x = tic[0] % 2
                        tic[0] += 1
                        for qb in range(4):
                            mm(banks[x][:, qb * 128:(qb + 1) * 128], otok[:, qb, cb * 128:(cb + 1) * 128], ident, True, True,
                               ["otok", "cbf"], [bk[x]])
                        act(oTc[:, cb, :], banks[x][:], AF.Copy, [bk[x]], ["oTc"])
                    dma(OT_d[0:512, cs].rearrange("(c p) t -> p c t", p=128), oTc[:, :, :], ["oTc"], [("OT", j)], "oto")

            S.barrier()
            apos[0] = 0
            ring = [carve(11520, BF16) for _ in range(3)]
            oTl = carve(8 * 512, BF16, [8, 512])
            gT = carve(NFB * 512, BF16, [NFB, 512])
            rct = [0]
            w_out = wview(od_w_out_b[i] if not even else ev_w_out_b[i])
            w1v = wview(w1_b[l])
            w3v = wview(w3_b[l])
            w2v = w2_b[l].rearrange("(f p) n -> p f n", p=128)
            xT2 = carve(8 * 512, F32, [8, 512])
            oTl2 = carve(8 * 512, BF16, [8, 512])
            xb = [xTt, xT2]
            xk = ["xT", "xT2"]
            oTls = [oTl, oTl2]
            pbk = [0]

            def nbank():
                b = pbk[0] % 6
                pbk[0] += 1
                return b

            def p3_head(j):
                p = j % 2
                cs = slice(j * 512, (j + 1) * 512)
                dma(xb[p][:], src[:, :, cs], [(srck, j)], [xk[p]], ("xld", p))
                dma(oTls[p][:, :, :], OT_d[:, cs].rearrange("(c p) t -> p c t", p=128), [("OT", j)], [("oTl", p)], ("otl", p))
                Wo, wok = ring_load(w_out[:, :, :], [128, 8, D], ("wb", l, 1))
                for nb in range(8):
                    b = nbank()
                    for c in range(8):
                        mm(banks[b][:], Wo[:, c, nb * 128:(nb + 1) * 128], oTls[p][:, c, :], c == 0, c == 7, [wok, ("oTl", p)], [bk[b]])
                    tt(xb[p][:, nb, :], xb[p][:, nb, :], banks[b][:], ALU.add, [xk[p], bk[b]], [xk[p]])
                norm(gf, l, "gf", xTt=xb[p], xkey=xk[p])

            def p3_ffn13(j):
                for half in range(2):
                    fs = slice(half * 1408, (half + 1) * 1408)
                    W1p, k1 = ring_load(w1v[:, :, fs], [128, 8, 1408], ("wb", l, 1))
                    W3p, k3 = ring_load(w3v[:, :, fs], [128, 8, 1408], ("wb", l, 1))
                    for fb in range(11):
                        f = half * 11 + fb
                        ba = nbank()
                        bb = nbank()
                        for k in range(8):
                            mm(banks[ba][:], W1p[:, k, fb * 128:(fb + 1) * 128], hT[:, k, :], k == 0, k == 7, [k1, ("hT", k)], [bk[ba]])
                        for k in range(8):
                            mm(banks[bb][:], W3p[:, k, fb * 128:(fb + 1) * 128], hT[:, k, :], k == 0, k == 7, [k3, ("hT", k)], [bk[bb]])
                        tsel = (t32a, "t32a") if f % 2 == 0 else (t32c, "t32c")
                        act(tsel[0][:], banks[ba][:], AF.Silu, [bk[ba]], [tsel[1]])
                        tt(gT[:, f, :], tsel[0][:], banks[bb][:], ALU.mult, [tsel[1], bk[bb]], [("gT", f)])

            def p3_ffn2(j):
                p = j % 2
                cs = slice(j * 512, (j + 1) * 512)
                for nh in range(2):
                    W2p, k2 = ring_load(w2v[:, :, nh * 512:(nh + 1) * 512], [128, NFB, 512], ("wb", l, 1))
                    for nbl in range(4):
                        nb = nh * 4 + nbl
                        b = nbank()
                        for f in range(NFB):
                            mm(banks[b][:], W2p[:, f, nbl * 128:(nbl + 1) * 128], gT[:, f, :], f == 0, f == NFB - 1,
                               [k2, ("gT", f)], [bk[b]])
                        tt(xb[p][:, nb, :], xb[p][:, nb, :], banks[b][:], ALU.add, [xk[p], bk[b]], [xk[p]])
                dma(dst[:, :, cs], xb[p][:], [xk[p]], [(dstk, j)], ("xst", p))

            p3_head(0)
            for j in range(NCH):
                p3_ffn13(j)
                if j + 1 < NCH:
                    p3_head(j + 1)
                p3_ffn2(j)

        S.emit(final_dma=[("xst", 0), ("xst", 1)])
    return nc


def host_consts():
    s = np.arange(128)[:, None]
    t = np.arange(512)[None, :]
    maskS = np.zeros((128, 4, 512), np.float32)
    maskQ = np.zeros((128, 4, 512), np.float32)
    for a in range(4):
        maskS[:, a, :] = np.where(a * 128 + s < t, 0.0, NEG)
        maskQ[:, a, :] = np.where(t <= a * 128 + s, 0.0, -3.0e38)
    jj = np.arange(128)[:, None]
    ss = np.arange(128)[None, :]
    negtri = np.where(jj >= ss, -1.0, 0.0).astype(np.float32)
    negones = -np.ones((128, 128), np.float32)
    ident = np.eye(128, dtype=np.float32)
    blockones = (jj // 64 == ss // 64).astype(np.float32)
    ones = np.ones((128, 128), np.float32)
    cbf = np.concatenate([maskS.reshape(128, 2048), negtri, negones, ident, blockones, ones], axis=1)
    rc16 = np.zeros((128, 4, 16), np.float32)
    for g, w in enumerate(WINS):
        rc16[:, g, :] = 1.0 / np.minimum(np.arange(16) + 1, w)
    pow2 = np.tile((0.5 ** (np.arange(NIT) + 1)).astype(np.float32)[None, :], (128, 1))
    cf = np.concatenate([maskQ.reshape(128, 2048), rc16.reshape(128, 64), pow2], axis=1)
    return np.ascontiguousarray(cbf, np.float32), np.ascontiguousarray(cf, np.float32)


def make_in_maps(inputs, T, nseq):
    cbf, cf = host_consts()
    f = lambda a: np.ascontiguousarray(np.asarray(a, dtype=np.float32))
    x = np.asarray(inputs["x"], dtype=np.float32)

    def gl(g):
        return np.ascontiguousarray(np.asarray(g, np.float32).reshape(4, 8, 128).transpose(2, 0, 1))

    common = {
        "gmix": gl(inputs["norm_mix_g"]), "gffn": gl(inputs["norm_ffn_g"]),
        "ev_w_in": f(inputs["ev_w_in"]), "ev_w_out": f(inputs["ev_w_out"]),
        "od_w_qkv": f(inputs["od_w_qkv"]), "od_w_out": f(inputs["od_w_out"]),
        "ffn_w1": f(inputs["ffn_w1"]), "ffn_w3": f(inputs["ffn_w3"]), "ffn_w2": f(inputs["ffn_w2"]),
        "qg": np.ascontiguousarray(np.tile(np.asarray(inputs["ev_q_norm_g"], np.float32), (1, 2)).T),
        "kg": np.ascontiguousarray(np.tile(np.asarray(inputs["ev_k_norm_g"], np.float32), (1, 2)).T),
        "pool_w": f(inputs["ev_pool_w"]),
        "pscale": np.ascontiguousarray(np.asarray(inputs["ev_pool_scale"], np.float32).reshape(2, 4, 128).transpose(2, 0, 1)),
        "cbf": cbf, "cf": cf,
    }
    maps = []
    for c in range(8):
        b = c % nseq
        xT = np.ascontiguousarray(x[b].T.reshape(8, 128, T).transpose(1, 0, 2))
        m = dict(common)
        m["xT"] = xT
        maps.append(m)
    return maps


_NC_CACHE = {}


def run(inputs, T, layers, nseq):
    key = (T, layers)
    if key not in _NC_CACHE:
        _NC_CACHE[key] = build(T, layers)
    nc = _NC_CACHE[key]
    maps = make_in_maps(inputs, T, nseq)
    res = run_bass_kernel_spmd(nc, maps, core_ids=list(range(8)))
    outs = []
    for b in range(nseq):
        yT = np.asarray(res.results[b]["yT"])
        outs.append(yT.transpose(1, 0, 2).reshape(D, T).T)
    return np.ascontiguousarray(np.stack(outs, 0)).astype(np.float32)


def kernel(**inputs):
    return run(inputs, 4096, 4, 4)
```

```python
import contextlib
import numpy as np
import concourse.bass as bass
import concourse.mybir as mybir
from concourse.bass_utils import run_bass_kernel_spmd

F32 = mybir.dt.float32
BF16 = mybir.dt.bfloat16
ALU = mybir.AluOpType
AF = mybir.ActivationFunctionType
AX = mybir.AxisListType

D = 1024
DFF = 2816
NFB = 22
EV_COLS = 1604
NIT = 15
NEG = -30000.0
IDX_SCALE = 256 ** -0.5
WINS = (2, 4, 8, 16)
EPS = 1e-6


class Sched:
    def __init__(self, nc):
        self.nc = nc
        self.ops = []
        self.lastw = {}
        self.readers = {}
        self.pending = {}
        self.last_on = {}
        self.last_dma = {}
        self.epoch = 0

    def add(self, eng, fn, reads=(), writes=(), dma=None):
        idx = len(self.ops)
        hard, soft = set(), set()
        for k in reads:
            w = self.lastw.get(k)
            if w is not None:
                hard.add(w)
        for k in writes:
            w = self.lastw.get(k)
            if w is not None:
                hard.add(w)
            for r in self.readers.get(k, ()):
                soft.add(r)
        pb = self.pending.pop(eng, None)
        if pb:
            hard |= pb
        for k in reads:
            self.readers.setdefault(k, []).append(idx)
        for k in writes:
            self.lastw[k] = idx
            self.readers[k] = []
        self.ops.append(dict(eng=eng, fn=fn, hard=hard, soft=soft, dma=dma, sig=False, cnt=0, ep=self.epoch))
        if dma is None:
            self.last_on[eng] = idx
        else:
            self.last_dma[dma] = idx
        return idx

    def barrier(self):
        b = set(self.last_on.values()) | set(v for k, v in self.last_dma.items() if not (isinstance(k, tuple) and k[0] == "wcast"))
        self.epoch += 1
        for e in ("pe", "act", "dve", "pool", "sp"):
            self.pending[e] = set(b) | self.pending.get(e, set())

    def emit(self, final_dma):
        nc = self.nc
        ops = self.ops
        need = [None] * len(ops)
        for i, op in enumerate(ops):
            deps = set()
            for d in op["hard"]:
                od = ops[d]
                if od["dma"] is None and op["dma"] is None and od["eng"] == op["eng"] == "pe":
                    continue
                deps.add(d)
            for d in op["soft"]:
                od = ops[d]
                if od["dma"] is None and op["dma"] is None and od["eng"] == op["eng"]:
                    continue
                deps.add(d)
            need[i] = deps
            for d in deps:
                ops[d]["sig"] = True
        ecount = {}
        dcount = {}
        for op in ops:
            if op["dma"] is not None:
                dcount[op["dma"]] = dcount.get(op["dma"], 0) + 16
                op["cnt"] = dcount[op["dma"]]
            elif op["sig"]:
                ek = (op["eng"], op["ep"])
                ecount[ek] = ecount.get(ek, 0) + 1
                op["cnt"] = ecount[ek]
        with contextlib.ExitStack() as st:
            esem = {ek: st.enter_context(nc.semaphore("es_%s_%d" % ek)) for ek in sorted(ecount)}
            dsem = {k: st.enter_context(nc.semaphore("ds_%d" % n)) for n, k in enumerate(sorted(dcount, key=str))}
            block = st.enter_context(nc.Block())
            streams = {e: [] for e in ("pe", "act", "dve", "pool", "sp")}
            for i, op in enumerate(ops):
                streams[op["eng"]].append(i)

            def run(e, ename):
                waited = {}
                for i in streams[ename]:
                    op = ops[i]
                    tgt = {}
                    for d in need[i]:
                        od = ops[d]
                        s = dsem[od["dma"]] if od["dma"] is not None else esem[(od["eng"], od["ep"])]
                        key = id(s)
                        if od["cnt"] > tgt.get(key, (None, 0))[1]:
                            tgt[key] = (s, od["cnt"])
                    for key, (s, v) in tgt.items():
                        if waited.get(key, 0) < v:
                            e.wait_ge(s, v)
                            waited[key] = v
                    ins = op["fn"](e)
                    if op["dma"] is not None:
                        ins.then_inc(dsem[op["dma"]], 16)
                    elif op["sig"]:
                        ins.then_inc(esem[(ename, op["ep"])], 1)
                if ename == "sp":
                    for k in final_dma:
                        e.wait_ge(dsem[k], dcount[k])

            block.tensor(lambda e: run(e, "pe"))
            block.scalar(lambda e: run(e, "act"))
            block.vector(lambda e: run(e, "dve"))
            block.gpsimd(lambda e: run(e, "pool"))
            block.sync(lambda e: run(e, "sp"))


def build(T, layers):
    NCH = T // 512
    NKB = T // 128
    nc = bass.Bass("TRN2", target_bir_lowering=False)

    def din(name, shape, dt=F32):
        return nc.dram_tensor(name, list(shape), dt, kind="ExternalInput").ap()

    xT_in = din("xT", [128, 8, T])
    gmix = din("gmix", [128, 4, 8])
    gffn = din("gffn", [128, 4, 8])
    ev_w_in = din("ev_w_in", [2, D, EV_COLS])
    ev_w_out = din("ev_w_out", [2, D, D])
    od_w_qkv = din("od_w_qkv", [2, D, 3 * D])
    od_w_out = din("od_w_out", [2, D, D])
    w1 = din("ffn_w1", [4, D, DFF])
    w3 = din("ffn_w3", [4, D, DFF])
    w2 = din("ffn_w2", [4, DFF, D])
    qg_in = din("qg", [128, 2])
    kg_in = din("kg", [128, 2])
    poolw_in = din("pool_w", [2, 4, 128, 128])
    pscale_in = din("pscale", [128, 2, 4])
    cbf_in = din("cbf", [128, 2048 + 5 * 128])
    cf_in = din("cf", [128, 2048 + 64 + NIT])
    yT = nc.dram_tensor("yT", [128, 8, T], F32, kind="ExternalOutput").ap()

    XT_d = nc.dram_tensor("XT_d", [128, 8, T], F32).ap()
    QT_d = nc.dram_tensor("QT_d", [D, T], BF16).ap()
    KT_d = nc.dram_tensor("KT_d", [D, T], BF16).ap()
    V_d = nc.dram_tensor("V_d", [T, D], BF16).ap()
    VA_d = nc.dram_tensor("VA_d", [T, 130], BF16).ap()
    QI_d = nc.dram_tensor("QI_d", [256, T], BF16).ap()
    KI_d = nc.dram_tensor("KI_d", [64, T], BF16).ap()
    WI_d = nc.dram_tensor("WI_d", [T, 4], F32).ap()
    OT_d = nc.dram_tensor("OT_d", [D, T], BF16).ap()

    def wscr(name, ap):
        return nc.dram_tensor(name, list(ap.shape), BF16).ap()

    ev_w_in_b = wscr("ev_w_in_b", ev_w_in)
    ev_w_out_b = wscr("ev_w_out_b", ev_w_out)
    od_w_qkv_b = wscr("od_w_qkv_b", od_w_qkv)
    od_w_out_b = wscr("od_w_out_b", od_w_out)
    w1_b = wscr("w1_b", w1)
    w3_b = wscr("w3_b", w3)
    w2_b = wscr("w2_b", w2)

    S = Sched(nc)
    with contextlib.ExitStack() as st:
        def sb(name, shape, dt):
            return st.enter_context(nc.sbuf_tensor(name, list(shape), dt))

        xTt = sb("xTt", [128, 8, 512], F32)
        hT = sb("hT", [128, 8, 512], BF16)
        sq = sb("sq", [128, 8, 512], BF16)
        cbf = sb("cbf_s", [128, 2048 + 5 * 128], BF16)
        cf = sb("cf_s", [128, 2048 + 64 + NIT], F32)
        gm = sb("gm", [128, 4, 8], F32)
        gf = sb("gf", [128, 4, 8], F32)
        qg = sb("qg_s", [128, 2], F32)
        kg = sb("kg_s", [128, 2], F32)
        psc = sb("psc", [128, 2, 4], F32)
        poolw = sb("poolw", [128, 2, 4, 128], BF16)
        cst = sb("cst", [128, 4], F32)
        t32a = sb("t32a", [128, 512], F32)
        t32b = sb("t32b", [128, 512], F32)
        t32c = sb("t32c", [128, 512], F32)
        ARENA = 67584
        arena = sb("arena", [128, ARENA], BF16)
        psall = st.enter_context(nc.psum_tensor("psall", [128, 4096], F32))
        banks = [psall[:, i * 512:(i + 1) * 512] for i in range(8)]

        maskS = cbf[:, 0:2048].rearrange("p (a b) -> p a b", a=4)
        negtri = cbf[:, 2048:2176]
        negones = cbf[:, 2176:2304]
        ident = cbf[:, 2304:2432]
        blockones = cbf[:, 2432:2560]
        ones = cbf[:, 2560:2688]
        maskQ = cf[:, 0:2048].rearrange("p (a b) -> p a b", a=4)
        rc16 = cf[:, 2048:2112].rearrange("p (a b) -> p a b", a=4)
        pow2 = cf[:, 2112:2112 + NIT]

        apos = [0]

        def carve(n_el, dt, shape=None):
            nb = n_el * (4 if dt == F32 else 2)
            nb = (nb + 63) // 64 * 64
            o = apos[0]
            assert o + nb <= ARENA * 2, (o, nb)
            apos[0] = o + nb
            v = arena[:, o // 2:(o + nb) // 2]
            if dt == F32:
                v = v.bitcast(F32)
            v = v[:, 0:n_el]
            if shape is not None:
                names = " ".join("a%d" % i for i in range(len(shape)))
                kw = {"a%d" % i: s for i, s in enumerate(shape[:-1])}
                v = v.rearrange("p (%s) -> p %s" % (names, names), **kw)
            return v

        def mm(out, lhsT, rhs, start, stop, reads, writes):
            S.add("pe", lambda e: e.matmul(out, lhsT, rhs, start=start, stop=stop), reads, writes)

        def act(out, in_, func, reads, writes, bias=None, scale=None):
            kw = {}
            if bias is not None:
                kw["bias"] = bias
            if scale is not None:
                kw["scale"] = scale
            S.add("act", lambda e: e.activation(out=out, in_=in_, func=func, **kw), reads, writes)

        def dma(out, in_, reads, writes, slot, eng="sp"):
            S.add(eng, lambda e: e.dma_start(out=out, in_=in_), reads, writes, dma=slot)

        def tt(out, in0, in1, op, reads, writes, eng="dve"):
            S.add(eng, lambda e: e.tensor_tensor(out=out, in0=in0, in1=in1, op=op), reads, writes)

        def ts(out, in0, s1, s2, op0, op1, reads, writes, accum=None, eng="dve"):
            if accum is None:
                S.add(eng, lambda e: e.tensor_scalar(out=out, in0=in0, scalar1=s1, scalar2=s2, op0=op0, op1=op1),
                      reads, writes)
            else:
                S.add(eng, lambda e: e.tensor_scalar(out=out, in0=in0, scalar1=s1, scalar2=s2, op0=op0, op1=op1,
                                                     accum_out=accum), reads, writes)

        def stt(out, in0, scalar, in1, op0, op1, reads, writes, eng="dve"):
            S.add(eng, lambda e: e.scalar_tensor_tensor(out=out, in0=in0, scalar=scalar, in1=in1, op0=op0, op1=op1),
                  reads, writes)

        def memset(ap, v, writes, eng="pool"):
            S.add(eng, lambda e: e.memset(ap, v), (), writes)

        def wview(w_ap):
            return w_ap.rearrange("(k p) n -> p k n", p=128)

        dma(cbf[:], cbf_in[:, :], (), ["cbf"], "csw", eng="pool")
        dma(cf[:], cf_in[:, :], (), ["cf"], "c")
        dma(gm[:], gmix[:, :, :], (), ["gm"], "c")
        dma(gf[:], gffn[:, :, :], (), ["gf"], "c")
        dma(qg[:], qg_in[:, :], (), ["qg"], "c")
        dma(kg[:], kg_in[:, :], (), ["kg"], "c")
        dma(psc[:], pscale_in[:, :, :], (), ["psc"], "c")
        for i in range(2):
            dma(poolw[:, i, :, :], poolw_in[i].rearrange("g c d -> c g d"), (), ["poolw%d" % i], "csw", eng="pool")
        memset(cst[:, 0:1], EPS, ["cst"])
        memset(cst[:, 1:2], 1.0, ["cst"])
        memset(cst[:, 2:3], 64 * EPS, ["cst"])
        memset(cst[:, 3:4], 0.0, ["cst"])

        bk = ["ps%d" % i for i in range(8)]

        def cast_w(src, dstb, l, grp):
            rows = src.shape[0]
            for r0 in range(0, rows, 128):
                dma(dstb[r0:r0 + 128, :], src[r0:r0 + 128, :], [], [("wb", l, grp)], ("wcast", l, grp), eng="pool")

        def cast_group(l_, grp):
            i_ = l_ // 2
            if grp == 0:
                if l_ % 2 == 0:
                    cast_w(ev_w_in[i_], ev_w_in_b[i_], l_, 0)
                else:
                    cast_w(od_w_qkv[i_], od_w_qkv_b[i_], l_, 0)
            else:
                cast_w((ev_w_out if l_ % 2 == 0 else od_w_out)[i_], (ev_w_out_b if l_ % 2 == 0 else od_w_out_b)[i_], l_, 1)
                cast_w(w1[l_], w1_b[l_], l_, 1)
                cast_w(w3[l_], w3_b[l_], l_, 1)
                cast_w(w2[l_], w2_b[l_], l_, 1)

        cast_group(0, 0)

        def norm(gcol_t, l, gkey, xTt=xTt, xkey="xT"):
            act(sq[:], xTt[:], AF.Square, [xkey], ["sq"])
            for k in range(8):
                mm(banks[7][:], ones, sq[:, k, :], k == 0, k == 7, ["sq", "cbf"], [bk[7]])
            act(t32a[:], banks[7][:], AF.Ln, [bk[7], "cst"], ["t32a"], bias=cst[:, 0:1], scale=1.0 / D)
            act(t32b[:], t32a[:], AF.Exp, ["t32a"], ["t32b"], scale=-0.5)
            for k in range(8):
                stt(hT[:, k, :], xTt[:, k, :], gcol_t[:, l, k:k + 1], t32b[:], ALU.mult, ALU.mult,
                    [xkey, "t32b", gkey], [("hT", k)])

        hkeys = [("hT", k) for k in range(8)]

        for l in range(layers):
            i = l // 2
            even = (l % 2 == 0)
            src = xT_in if l == 0 else XT_d
            dst = yT if l == layers - 1 else XT_d
            srck = "xin" if l == 0 else "XT"
            dstk = "yT" if l == layers - 1 else "XT"

            S.barrier()
            apos[0] = 0
            ring = [carve(11520, BF16) for _ in range(3)]
            stg = [carve(512, BF16) for _ in range(4)]
            rct = [0]
            sct = [0]

            def ring_load(src_ap, shape3, key):
                r = rct[0] % 3
                rct[0] += 1
                n = shape3[1] * shape3[2]
                v = ring[r][:, 0:n].rearrange("p (a b) -> p a b", a=shape3[1])
                dma(v, src_ap, [key], [("ring", r)], ("ring", r), eng="pool")
                return v, ("ring", r)

            def stage_out(ps_ap, dram_ap, psk, dkey, scale=None, nparts=128):
                s = sct[0] % 4
                sct[0] += 1
                o = stg[s][0:nparts, :]
                if scale is None:
                    act(o, ps_ap, AF.Copy, [psk], [("stg", s)])
                else:
                    act(o, ps_ap, AF.Copy, [psk, "psc"], [("stg", s)], scale=scale)
                dma(dram_ap, o, [("stg", s)], [dkey], ("stgo", s))

            if even:
                uext = carve(4 * 528, F32, [4, 528])
                pa = carve(528, F32)
                pb = carve(528, F32)
                mixb = carve(512, BF16)
                vstage = carve(4 * 130, BF16, [4, 130])
                wist = carve(16, F32, [4, 4])
                t16 = carve(16, F32)
                memset(uext[:, :, 0:16], 0.0, ["uext"])
                memset(vstage[:], 1.0, ["vstage"])

            dma(xTt[:], src[:, :, 0:512], [(srck, 0)], ["xT"], "xld")
            for j in range(NCH):
                cs = slice(j * 512, (j + 1) * 512)
                norm(gm, l, "gm")
                if j + 1 < NCH:
                    dma(xTt[:], src[:, :, (j + 1) * 512:(j + 2) * 512], [(srck, j + 1)], ["xT"], "xld")
                bi = [0]

                def nb_bank():
                    b = bi[0] % 4
                    bi[0] += 1
                    return b

                if not even:
                    wq = wview(od_w_qkv_b[i])
                    for piece in range(3):
                        W, wk = ring_load(wq[:, :, piece * D:(piece + 1) * D], [128, 8, D], ("wb", l, 0))
                        if piece < 2:
                            tgt = QT_d if piece == 0 else KT_d
                            tk = "QT" if piece == 0 else "KT"
                            for nb in range(8):
                                b = nb_bank()
                                for k in range(8):
                                    mm(banks[b][:], W[:, k, nb * 128:(nb + 1) * 128], hT[:, k, :], k == 0, k == 7,
                                       [wk, ("hT", k)], [bk[b]])
                                stage_out(banks[b][:], tgt[nb * 128:(nb + 1) * 128, cs], bk[b], (tk, j),
                                          scale=(0.125 if piece == 0 else None))
                        else:
                            for tb in range(4):
                                for nh in range(2):
                                    b = nb_bank()
                                    for k in range(8):
                                        mm(banks[b][:], hT[:, k, tb * 128:(tb + 1) * 128], W[:, k, nh * 512:(nh + 1) * 512],
                                           k == 0, k == 7, [wk, ("hT", k)], [bk[b]])
                                    stage_out(banks[b][:], V_d[j * 512 + tb * 128:j * 512 + (tb + 1) * 128, nh * 512:(nh + 1) * 512],
                                              bk[b], ("V", j))
                else:
                    wv = wview(ev_w_in_b[i])
                    WA, wak = ring_load(wv[:, :, 0:1092], [128, 8, 1092], ("wb", l, 0))
                    WB, wbk = ring_load(wv[:, :, 1092:1604], [128, 8, 512], ("wb", l, 0))

                    def qkproc(c0, gcol, gkey, lnscale, lnbias, dram_ap, dkey):
                        b = nb_bank()
                        for k in range(8):
                            mm(banks[b][:], WA[:, k, c0:c0 + 128], hT[:, k, :], k == 0, k == 7, [wak, ("hT", k)], [bk[b]])
                        act(t32c[:], banks[b][:], AF.Copy, [bk[b]], ["t32c"])
                        act(sq[:, 0, :], banks[b][:], AF.Square, [bk[b]], ["sq"])
                        mm(banks[6][:], blockones, sq[:, 0, :], True, True, ["sq", "cbf"], [bk[6]])
                        act(t32a[:], banks[6][:], AF.Ln, [bk[6], "cst"], ["t32a"], bias=lnbias, scale=lnscale)
                        act(t32b[:], t32a[:], AF.Exp, ["t32a"], ["t32b"], scale=-0.5)
                        s = sct[0] % 4
                        sct[0] += 1
                        stt(stg[s][:], t32c[:], gcol, t32b[:], ALU.mult, ALU.mult, ["t32c", "t32b", gkey], [("stg", s)])
                        dma(dram_ap, stg[s][:], [("stg", s)], [dkey], ("stgo", s))

                    for nb in range(4):
                        qkproc(nb * 128, qg[:, i:i + 1], "qg", 1.0, cst[:, 2:3], QT_d[nb * 128:(nb + 1) * 128, cs], ("QT", j))
                    qkproc(512, kg[:, i:i + 1], "kg", 1.0 / 64, cst[:, 0:1], KT_d[0:128, cs], ("KT", j))
                    for tb in range(4):
                        b = nb_bank()
                        for k in range(8):
                            mm(banks[b][:, 0:128], hT[:, k, tb * 128:(tb + 1) * 128], WA[:, k, 640:768], k == 0, k == 7,
                               [wak, ("hT", k)], [bk[b]])
                        for k in range(8):
                            mm(banks[b][:, 128:132], hT[:, k, tb * 128:(tb + 1) * 128], WA[:, k, 1088:1092], k == 0, k == 7,
                               [wak, ("hT", k)], [bk[b]])
                        for g in range(2):
                            act(vstage[:, tb, g * 65:g * 65 + 64], banks[b][:, g * 64:(g + 1) * 64], AF.Copy, [bk[b]], ["vstage"])
                        act(wist[:, tb, :], banks[b][:, 128:132], AF.Copy, [bk[b]], ["wist"], scale=IDX_SCALE)
                    dma(VA_d[cs, :].rearrange("(tb s) c -> s tb c", s=128), vstage[:], ["vstage"], [("VA", j)], "vao")
                    dma(WI_d[cs, :].rearrange("(tb s) c -> s tb c", s=128), wist[:], ["wist"], [("WI", j)], "wio")
                    for nb in range(2):
                        b = nb_bank()
                        for k in range(8):
                            mm(banks[b][:], WA[:, k, 768 + nb * 128:768 + (nb + 1) * 128], hT[:, k, :], k == 0, k == 7,
                               [wak, ("hT", k)], [bk[b]])
                        stage_out(banks[b][:], QI_d[nb * 128:(nb + 1) * 128, cs], bk[b], ("QI", j))
                    b = nb_bank()
                    for k in range(8):
                        mm(banks[b][0:64, :], WA[:, k, 1024:1088], hT[:, k, :], k == 0, k == 7, [wak, ("hT", k)], [bk[b]])
                    stage_out(banks[b][0:64, :], KI_d[0:64, cs], bk[b], ("KI", j), nparts=64)
                    for g in range(4):
                        b = nb_bank()
                        for k in range(8):
                            mm(banks[b][:], WB[:, k, g * 128:(g + 1) * 128], hT[:, k, :], k == 0, k == 7, [wbk, ("hT", k)], [bk[b]])
                        act(uext[:, g, 16:528], banks[b][:], AF.Copy, [bk[b]], ["uext"])
                        a_ap, a_key = uext[:, g, :], "uext"
                        dsh = 1
                        tgl = 0
                        for _ in range(g + 1):
                            o_ap, o_key = (pa, "pa") if tgl == 0 else (pb, "pb")
                            lo_ = 2 * dsh - 1
                            tt(o_ap[:, lo_:528], a_ap[:, lo_:528], a_ap[:, lo_ - dsh:528 - dsh], ALU.add,
                               [a_key], [o_key], eng="pool")
                            a_ap, a_key = o_ap, o_key
                            dsh *= 2
                            tgl ^= 1
                        w = WINS[g]
                        stt(mixb[:], a_ap[:, 16:528], 1.0 / w, uext[:, g, 16:528], ALU.mult, ALU.subtract,
                            [a_key, "uext"], ["mixb"])
                        if j == 0:
                            tt(t16[:], a_ap[:, 16:32], rc16[:, g, :], ALU.mult, [a_key, "cf"], ["t16"])
                            tt(mixb[:, 0:16], t16[:], uext[:, g, 16:32], ALU.subtract, ["t16", "uext", "mixb"], ["mixb"])
                        b2 = nb_bank()
                        mm(banks[b2][:], poolw[:, i, g, :], mixb[:], True, True, ["mixb", "poolw%d" % i], [bk[b2]])
                        stage_out(banks[b2][:], OT_d[(4 + g) * 128:(5 + g) * 128, cs], bk[b2], ("OT", j),
                                  scale=psc[:, i, g:g + 1])
                    S.add("pool", (lambda o_, i_: (lambda e: e.tensor_copy(out=o_, in_=i_)))(uext[:, :, 0:16], uext[:, :, 512:528]),
                          ["uext", "pa", "pb", "mixb"], ["uext"])

            S.barrier()
            apos[0] = 0
            allk = lambda nm, jj: [(nm, c) for c in range(jj + 1)]
            if not even:
                KTs = [carve(T, BF16) for _ in range(2)]
                Vz = [[carve(NKB * 128, BF16, [NKB, 128]) for _ in range(2)] for _ in range(2)]
                QTc = [carve(512, BF16) for _ in range(2)]
                Eb = [carve(1024, F32) for _ in range(3)]
                spb = [carve(1024, BF16) for _ in range(3)]
                Gb = [carve(1024, F32) for _ in range(2)]
                aTb = [carve(1024, BF16) for _ in range(2)]
                Rb = [carve(1024, BF16) for _ in range(3)]
                oTs = [carve(512, BF16) for _ in range(2)]
                for ks in range(2):
                    memset(Vz[ks][0][:, :, 64:128], 0.0, [("Vz", ks, 0)])
                    memset(Vz[ks][1][:, :, 0:64], 0.0, [("Vz", ks, 1)])
                cast_group(l, 1)
                if l + 1 < layers:
                    cast_group(l + 1, 0)
                Vv = V_d.rearrange("(kb s) c -> s kb c", s=128)
                AD = [psall[:, 0:1024], psall[:, 1024:2048]]
                ADk = [["ps0", "ps1"], ["ps2", "ps3"]]
                BD = psall[:, 2048:3072]
                BDk = ["ps4", "ps5"]

                def load_pair(hp):
                    ks = hp % 2
                    dma(KTs[ks][:, :], KT_d[hp * 128:(hp + 1) * 128, :], [], [("KTs", ks)], ("ktl", ks))
                    dma(Vz[ks][0][:, :, 0:64], Vv[:, :, hp * 128:hp * 128 + 64], [], [("Vz", ks, 0)], ("vz0", ks))
                    dma(Vz[ks][1][:, :, 64:128], Vv[:, :, hp * 128 + 64:hp * 128 + 128], [], [("Vz", ks, 1)], ("vz1", ks))

                steps = []
                gi = 0
                for hp in range(8):
                    for j in range(NCH):
                        nkb = 4 * j + 4
                        for n_, kb in enumerate(range(4 * j + 3, -1, -1)):
                            steps.append(dict(hp=hp, j=j, kb=kb, first=(n_ == 0), last=(kb == 0), g=gi, n=len(steps)))
                        gi += 1
                load_pair(0)
                NS = len(steps)

                def S1(t):
                    sp_ = steps[t]
                    hp, j, kb, x = sp_["hp"], sp_["j"], sp_["kb"], t % 2
                    ks, qs = hp % 2, sp_["g"] % 2
                    if j == 0 and kb == 1 and hp + 1 < 8:
                        load_pair(hp + 1)
                    if sp_["first"]:
                        dma(QTc[qs][:, :], QT_d[hp * 128:(hp + 1) * 128, j * 512:(j + 1) * 512], [], [("QTc", qs)], ("qtl", qs))
                    diag = kb >= 4 * j
                    ksl = slice(kb * 128, (kb + 1) * 128)
                    for e_ in range(2):
                        ps_ = slice(e_ * 64, (e_ + 1) * 64)
                        o_ = AD[x][:, e_ * 512:(e_ + 1) * 512]
                        mm(o_, KTs[ks][ps_, ksl], QTc[qs][ps_, :], True, not diag, [("KTs", ks), ("QTc", qs)], [ADk[x][e_]])
                        if diag:
                            mm(o_, ident, maskS[:, kb - 4 * j, :], False, True, ["cbf"], [ADk[x][e_]])
                    xe = t % 3
                    act(Eb[xe][:], AD[x], AF.Exp, ADk[x], [("Eb", xe)])
                    act(spb[xe][:], Eb[xe][:], AF.Ln, [("Eb", xe), "cst"], [("spb", xe)], bias=cst[:, 1:2], scale=1.0)
                    if not sp_["last"]:
                        if sp_["first"]:
                            S.add("dve", (lambda o, i_: (lambda e: e.tensor_copy(out=o, in_=i_)))(Rb[xe][:], spb[xe][:]),
                                  [("spb", xe)], [("Rb", xe)])
                        else:
                            rp = (t - 1) % 3
                            tt(Rb[xe][:], Rb[rp][:], spb[xe][:], ALU.add, [("Rb", rp), ("spb", xe)], [("Rb", xe)])

                def S4(t):
                    sp_ = steps[t]
                    x = t % 2
                    xe = t % 3
                    Rprev = None if sp_["first"] else (t - 1) % 3
                    for e_ in range(2):
                        hs = slice(e_ * 512, (e_ + 1) * 512)
                        mm(BD[:, hs], negtri, spb[xe][:, hs], True, Rprev is None, ["cbf", ("spb", xe)], [BDk[e_]])
                        if Rprev is not None:
                            mm(BD[:, hs], negones, Rb[Rprev][:, hs], False, True, ["cbf", ("Rb", Rprev)], [BDk[e_]])
                    act(Gb[x][:], BD, AF.Exp, BDk, [("Gb", x)])
                    tt(aTb[x][:], Eb[xe][:], Gb[x][:], ALU.mult, [("Eb", xe), ("Gb", x)], [("aTb", x)])

                def S7(t):
                    sp_ = steps[t]
                    hp, j, kb, x = sp_["hp"], sp_["j"], sp_["kb"], t % 2
                    ks, qs = hp % 2, sp_["g"] % 2
                    bo = 6 + qs
                    for e_ in range(2):
                        hs = slice(e_ * 512, (e_ + 1) * 512)
                        mm(banks[bo][:], Vz[ks][e_][:, kb, :], aTb[x][:, hs], sp_["first"] and e_ == 0, sp_["last"] and e_ == 1,
                           [("Vz", ks, e_), ("aTb", x)], [bk[bo]])
                    if sp_["last"]:
                        act(oTs[qs][:], banks[bo][:], AF.Copy, [bk[bo]], [("oTs", qs)])
                        dma(OT_d[hp * 128:(hp + 1) * 128, j * 512:(j + 1) * 512], oTs[qs][:], [("oTs", qs)], [("OT", j)], ("oto", qs))

                for t in range(NS + 2):
                    if t < NS:
                        S1(t)
                    if 0 <= t - 1 < NS:
                        S4(t - 1)
                    if 0 <= t - 2 < NS:
                        S7(t - 2)
            else:
                KTs = carve(T, BF16)
                KI2 = carve(T, BF16)
                VAs = carve(NKB * 130, BF16, [NKB, 130])
                sc = carve(T, F32)
                mbb = [[carve(T, BF16) for _ in range(4)] for _ in range(2)]
                QTcs = [carve(4 * 512, BF16, [4, 512]) for _ in range(2)]
                QIcs = [carve(2 * 512, BF16, [2, 512]) for _ in range(2)]
                wics = [carve(16, F32, [4, 4]) for _ in range(2)]
                rtmp = [carve(512, F32) for _ in range(2)]
                PT = [carve(512, BF16) for _ in range(2)]
                otok = carve(4 * 512, BF16, [4, 512])
                oTc = carve(4 * 512, BF16, [4, 512])
                sm = carve(8 + NIT, F32)
                rd = carve(4, F32)
                dma(KTs[:, :], KT_d[0:128, :], [], ["KTs"], "ktl")
                dma(KI2[0:64, :], KI_d[:, :], [], ["KI2"], "kil")
                dma(KI2[64:128, :], KI_d[:, :], [], ["KI2"], "kil")
                dma(VAs[:, :, :], VA_d.rearrange("(kb s) c -> s kb c", s=128), [], ["VAs"], "val")
                tic = [0]
                cast_group(l, 1)
                if l + 1 < layers:
                    cast_group(l + 1, 0)

                def loads(j):
                    cs = slice(j * 512, (j + 1) * 512)
                    p = j % 2
                    dma(QTcs[p][0:64, :, :], QT_d[0:256, cs].rearrange("(h d) t -> d h t", d=64), [], [("QTc", p)], ("qtl", p))
                    dma(QTcs[p][64:128, :, :], QT_d[256:512, cs].rearrange("(h d) t -> d h t", d=64), [], [("QTc", p)], ("qtl", p))
                    dma(QIcs[p][:, :, :], QI_d[:, cs].rearrange("(b p) t -> p b t", p=128), [], [("QIc", p)], ("qil", p))
                    dma(wics[p][:, :, :], WI_d[cs, :].rearrange("(tb s) c -> s tb c", s=128), [], [("wic", p)], ("wil", p))

                def search(j, qb):
                    p = j % 2
                    L = (j + 1) * 512
                    QIc, wic, mbq = QIcs[p], wics[p], mbb[p][qb]
                    for sg in range(j + 1):
                        ssl = slice(sg * 512, (sg + 1) * 512)
                        for hi in range(4):
                            x = tic[0] % 2
                            tic[0] += 1
                            hp_ = slice((hi % 2) * 64, (hi % 2) * 64 + 64)
                            mm(banks[x][:], QIc[hp_, hi // 2, qb * 128:(qb + 1) * 128], KI2[hp_, ssl], True, True,
                               [("QIc", p), "KI2"], [bk[x]])
                            act(rtmp[x][:], banks[x][:], AF.Relu, [bk[x]], [("rtmp", x)])
                            if hi == 0:
                                ts(sc[:, ssl], rtmp[x][:], wic[:, qb, 0:1], None, ALU.mult, ALU.bypass,
                                   [("rtmp", x), ("wic", p)], ["sc"])
                            else:
                                stt(sc[:, ssl], rtmp[x][:], wic[:, qb, hi:hi + 1], sc[:, ssl], ALU.mult, ALU.add,
                                    [("rtmp", x), ("wic", p), "sc"], ["sc"])
                    S.add("dve", (lambda o_, i_: (lambda e: e.tensor_reduce(out=o_, in_=i_, axis=AX.X, op=ALU.max)))(sm[:, 0:1], sc[:, 0:L]),
                          ["sc"], ["sm"])
                    S.add("dve", (lambda o_, i_: (lambda e: e.tensor_reduce(out=o_, in_=i_, axis=AX.X, op=ALU.min)))(sm[:, 1:2], sc[:, 0:L]),
                          ["sc"], ["sm"])
                    tt(sc[:, j * 512:(j + 1) * 512], sc[:, j * 512:(j + 1) * 512], maskQ[:, qb, :], ALU.add,
                       ["sc", "cf"], ["sc"])
                    if j == 0 and qb < 2:
                        thr = sm[:, 1:2]
                    else:
                        tt(sm[:, 2:3], sm[:, 0:1], sm[:, 1:2], ALU.subtract, ["sm"], ["sm"])
                        ts(sm[:, 8:8 + NIT], pow2, sm[:, 2:3], None, ALU.mult, ALU.bypass, ["sm", "cf"], ["sm"])
                        tt(sm[:, 4:5], sm[:, 1:2], sm[:, 8:9], ALU.add, ["sm"], ["sm"])
                        for it in range(NIT):
                            ts(mbq[:, 0:L], sc[:, 0:L], sm[:, 4:5], None, ALU.is_ge, ALU.add, ["sm", "sc"],
                               [("mb", p, qb), "sm"], accum=sm[:, 5:6])
                            ts(sm[:, 6:7], sm[:, 5:6], 256.0, 0.5, ALU.is_ge, ALU.subtract, ["sm"], ["sm"])
                            stt(sm[:, 4:5], sm[:, 6:7], sm[:, 8 + it:9 + it], sm[:, 4:5], ALU.mult, ALU.add, ["sm"], ["sm"])
                        stt(sm[:, 3:4], sm[:, 8 + NIT - 1:8 + NIT], -0.5, sm[:, 4:5], ALU.mult, ALU.add, ["sm"], ["sm"])
                        thr = sm[:, 3:4]
                    ts(mbq[:, 0:L], sc[:, 0:L], thr, NEG, ALU.is_lt, ALU.mult, ["sm", "sc"], [("mb", p, qb)])

                def att_part(j, heads):
                    p = j % 2
                    QTc, mbp = QTcs[p], mbb[p]
                    tiles = [(h, kb) for h in heads for kb in range(4 * j + 4)]

                    def E1(n_):
                        h, kb = tiles[n_]
                        g = h // 4
                        gp = slice(g * 64, (g + 1) * 64)
                        x = n_ % 2
                        ksl = slice(kb * 128, (kb + 1) * 128)
                        mm(banks[2 + x][:], KTs[gp, ksl], QTc[gp, h % 4, :], True, False, ["KTs", ("QTc", p)], [bk[2 + x]])
                        for qb in range(4):
                            mm(banks[2 + x][:, qb * 128:(qb + 1) * 128], mbp[qb][:, ksl], ident, False, qb == 3,
                               [("mb", p, qb), "cbf"], [bk[2 + x]])
                        act(PT[x][:], banks[2 + x][:], AF.Exp, [bk[2 + x]], [("PT", x)])

                    def E3(n_):
                        h, kb = tiles[n_]
                        g = h // 4
                        x = n_ % 2
                        bo = 4 + (h % 2)
                        for qb in range(4):
                            if kb <= 4 * j + qb:
                                mm(banks[bo][:, qb * 65:(qb + 1) * 65], PT[x][:, qb * 128:(qb + 1) * 128],
                                   VAs[:, kb, g * 65:(g + 1) * 65], (kb == 0 and qb == 0), kb == 4 * j + qb,
                                   [("PT", x), "VAs"], [bk[bo]])
                        if kb == 4 * j + 3:
                            bov = banks[bo][:, 0:260].rearrange("p (a b) -> p a b", a=4)
                            S.add("dve", (lambda o_, bv: (lambda e: e.reciprocal(out=o_, in_=bv)))(rd[:, :], bov[:, :, 64]), [bk[bo]], ["rd"])
                            for qb in range(4):
                                ts(otok[:, qb, h * 64:(h + 1) * 64], banks[bo][:, qb * 65:qb * 65 + 64], rd[:, qb:qb + 1], None,
                                   ALU.mult, ALU.bypass, [bk[bo], "rd"], ["otok"])

                    for n_ in range(len(tiles) + 1):
                        if n_ < len(tiles):
                            E1(n_)
                        if n_ >= 1:
                            E3(n_ - 1)

                loads(0)
                for qb in range(4):
                    search(0, qb)
                for j in range(NCH):
                    cs = slice(j * 512, (j + 1) * 512)
                    if j + 1 < NCH:
                        loads(j + 1)
                    for qb in range(4):
                        if j + 1 < NCH:
                            search(j + 1, qb)
                        att_part(j, (2 * qb, 2 * qb + 1))
                    for cb in range(4):
                        x = tic[0] % 2
                        tic[0] += 1
                        for qb in range(4):
                            mm(banks[x][:, qb * 128:(qb + 1) * 128], otok[:, qb, cb * 128:(cb + 1) * 128], ident, True, True,
                               ["otok", "cbf"], [bk[x]])
                        act(oTc[:, cb, :], banks[x][:], AF.Copy, [bk[x]], ["oTc"])
                    dma(OT_d[0:512, cs].rearrange("(c p) t -> p c t", p=128), oTc[:, :, :], ["oTc"], [("OT", j)], "oto")

            S.barrier()
            apos[0] = 0
            ring = [carve(11520, BF16) for _ in range(3)]
            oTl = carve(8 * 512, BF16, [8, 512])
            gT = carve(NFB * 512, BF16, [NFB, 512])
            rct = [0]
            w_out = wview(od_w_out_b[i] if not even else ev_w_out_b[i])
            w1v = wview(w1_b[l])
            w3v = wview(w3_b[l])
            w2v = w2_b[l].rearrange("(f p) n -> p f n", p=128)
            xT2 = carve(8 * 512, F32, [8, 512])
            oTl2 = carve(8 * 512, BF16, [8, 512])
            xb = [xTt, xT2]
            xk = ["xT", "xT2"]
            oTls = [oTl, oTl2]
            pbk = [0]

            def nbank():
                b = pbk[0] % 6
                pbk[0] += 1
                return b

            def p3_head(j):
                p = j % 2
                cs = slice(j * 512, (j + 1) * 512)
                dma(xb[p][:], src[:, :, cs], [(srck, j)], [xk[p]], ("xld", p))
                dma(oTls[p][:, :, :], OT_d[:, cs].rearrange("(c p) t -> p c t", p=128), [("OT", j)], [("oTl", p)], ("otl", p))
                Wo, wok = ring_load(w_out[:, :, :], [128, 8, D], ("wb", l, 1))
                for nb in range(8):
                    b = nbank()
                    for c in range(8):
                        mm(banks[b][:], Wo[:, c, nb * 128:(nb + 1) * 128], oTls[p][:, c, :], c == 0, c == 7, [wok, ("oTl", p)], [bk[b]])
                    tt(xb[p][:, nb, :], xb[p][:, nb, :], banks[b][:], ALU.add, [xk[p], bk[b]], [xk[p]])
                norm(gf, l, "gf", xTt=xb[p], xkey=xk[p])

            def p3_ffn13(j):
                for half in range(2):
                    fs = slice(half * 1408, (half + 1) * 1408)
                    W1p, k1 = ring_load(w1v[:, :, fs], [128, 8, 1408], ("wb", l, 1))
                    W3p, k3 = ring_load(w3v[:, :, fs], [128, 8, 1408], ("wb", l, 1))
                    for fb in range(11):
                        f = half * 11 + fb
                        ba = nbank()
                        bb = nbank()
                        for k in range(8):
                            mm(banks[ba][:], W1p[:, k, fb * 128:(fb + 1) * 128], hT[:, k, :], k == 0, k == 7, [k1, ("hT", k)], [bk[ba]])
                        for k in range(8):
                            mm(banks[bb][:], W3p[:, k, fb * 128:(fb + 1) * 128], hT[:, k, :], k == 0, k == 7, [k3, ("hT", k)], [bk[bb]])
                        tsel = (t32a, "t32a") if f % 2 == 0 else (t32c, "t32c")
                        act(tsel[0][:], banks[ba][:], AF.Silu, [bk[ba]], [tsel[1]])
                        tt(gT[:, f, :], tsel[0][:], banks[bb][:], ALU.mult, [tsel[1], bk[bb]], [("gT", f)])

            def p3_ffn2(j):
                p = j % 2
                cs = slice(j * 512, (j + 1) * 512)
                for nh in range(2):
                    W2p, k2 = ring_load(w2v[:, :, nh * 512:(nh + 1) * 512], [128, NFB, 512], ("wb", l, 1))
                    for nbl in range(4):
                        nb = nh * 4 + nbl
                        b = nbank()
                        for f in range(NFB):
                            mm(banks[b][:], W2p[:, f, nbl * 128:(nbl + 1) * 128], gT[:, f, :], f == 0, f == NFB - 1,
                               [k2, ("gT", f)], [bk[b]])
                        tt(xb[p][:, nb, :], xb[p][:, nb, :], banks[b][:], ALU.add, [xk[p], bk[b]], [xk[p]])
                dma(dst[:, :, cs], xb[p][:], [xk[p]], [(dstk, j)], ("xst", p))

            p3_head(0)
            for j in range(NCH):
                p3_ffn13(j)
                if j + 1 < NCH:
                    p3_head(j + 1)
                p3_ffn2(j)

        S.emit(final_dma=[("xst", 0), ("xst", 1)])
    return nc


def host_consts():
    s = np.arange(128)[:, None]
    t = np.arange(512)[None, :]
    maskS = np.zeros((128, 4, 512), np.float32)
    maskQ = np.zeros((128, 4, 512), np.float32)
    for a in range(4):
        maskS[:, a, :] = np.where(a * 128 + s < t, 0.0, NEG)
        maskQ[:, a, :] = np.where(t <= a * 128 + s, 0.0, -3.0e38)
    jj = np.arange(128)[:, None]
    ss = np.arange(128)[None, :]
    negtri = np.where(jj >= ss, -1.0, 0.0).astype(np.float32)
    negones = -np.ones((128, 128), np.float32)
    ident = np.eye(128, dtype=np.float32)
    blockones = (jj // 64 == ss // 64).astype(np.float32)
    ones = np.ones((128, 128), np.float32)
    cbf = np.concatenate([maskS.reshape(128, 2048), negtri, negones, ident, blockones, ones], axis=1)
    rc16 = np.zeros((128, 4, 16), np.float32)
    for g, w in enumerate(WINS):
        rc16[:, g, :] = 1.0 / np.minimum(np.arange(16) + 1, w)
    pow2 = np.tile((0.5 ** (np.arange(NIT) + 1)).astype(np.float32)[None, :], (128, 1))
    cf = np.concatenate([maskQ.reshape(128, 2048), rc16.reshape(128, 64), pow2], axis=1)
    return np.ascontiguousarray(cbf, np.float32), np.ascontiguousarray(cf, np.float32)


def make_in_maps(inputs, T, nseq):
    cbf, cf = host_consts()
    f = lambda a: np.ascontiguousarray(np.asarray(a, dtype=np.float32))
    x = np.asarray(inputs["x"], dtype=np.float32)

    def gl(g):
        return np.ascontiguousarray(np.asarray(g, np.float32).reshape(4, 8, 128).transpose(2, 0, 1))

    common = {
        "gmix": gl(inputs["norm_mix_g"]), "gffn": gl(inputs["norm_ffn_g"]),
        "ev_w_in": f(inputs["ev_w_in"]), "ev_w_out": f(inputs["ev_w_out"]),
        "od_w_qkv": f(inputs["od_w_qkv"]), "od_w_out": f(inputs["od_w_out"]),
        "ffn_w1": f(inputs["ffn_w1"]), "ffn_w3": f(inputs["ffn_w3"]), "ffn_w2": f(inputs["ffn_w2"]),
        "qg": np.ascontiguousarray(np.tile(np.asarray(inputs["ev_q_norm_g"], np.float32), (1, 2)).T),
        "kg": np.ascontiguousarray(np.tile(np.asarray(inputs["ev_k_norm_g"], np.float32), (1, 2)).T),
        "pool_w": f(inputs["ev_pool_w"]),
        "pscale": np.ascontiguousarray(np.asarray(inputs["ev_pool_scale"], np.float32).reshape(2, 4, 128).transpose(2, 0, 1)),
        "cbf": cbf, "cf": cf,
    }
    maps = []
    for c in range(8):
        b = c % nseq
        xT = np.ascontiguousarray(x[b].T.reshape(8, 128, T).transpose(1, 0, 2))
        m = dict(common)
        m["xT"] = xT
        maps.append(m)
    return maps


_NC_CACHE = {}


def run(inputs, T, layers, nseq):
    key = (T, layers)
    if key not in _NC_CACHE:
        _NC_CACHE[key] = build(T, layers)
    nc = _NC_CACHE[key]
    maps = make_in_maps(inputs, T, nseq)
    res = run_bass_kernel_spmd(nc, maps, core_ids=list(range(8)))
    outs = []
    for b in range(nseq):
        yT = np.asarray(res.results[b]["yT"])
        outs.append(yT.transpose(1, 0, 2).reshape(D, T).T)
    return np.ascontiguousarray(np.stack(outs, 0)).astype(np.float32)


def kernel(**inputs):
    return run(inputs, 4096, 4, 4)
```
